# Optimizing a Trainium2 kernel written in Bass

```python
import math
import jax, jax.numpy as jnp
from jax import lax
import numpy as np


D_MODEL = 1024
BATCH = 8
SEQ = 4096
DEPTH = 4

CHUNK = 64
Q_BLOCK = 128
DA_HEADS = D_MODEL // 256
DA_HEAD_DIM = 64
DA_WIDTH = DA_HEADS * 2 * DA_HEAD_DIM
ML_HEADS = D_MODEL // 256
ML_HEAD_DIM = 128
ML_WIDTH = ML_HEADS * ML_HEAD_DIM
ML_CONV = 4
FX_HEADS = D_MODEL // 64
FX_HEAD_DIM = 64
FX_WIDTH = FX_HEADS * FX_HEAD_DIM
FFN_HIDDEN = ((8 * D_MODEL + 3 * 256 - 1) // (3 * 256)) * 256
AB_IN = 3 * DA_WIDTH + 4 * ML_WIDTH + 2 * ML_HEADS
AB_OUT = DA_WIDTH + ML_WIDTH
FX_IN = 3 * FX_WIDTH + FX_HEADS
N_EVEN = (DEPTH + 1) // 2
N_ODD = DEPTH // 2
EPS = 1e-6

kernel_name = "hybrid_diffattn_mlstm_fox_block"


def rms_norm(x, g):
    xf = x.astype(jnp.float32)
    y = xf * lax.rsqrt(jnp.mean(xf * xf, axis=-1, keepdims=True) + EPS)
    return (y * g.astype(jnp.float32)).astype(x.dtype)


def modulate(h, shift, scale):
    return h * (1.0 + scale[:, None, :]) + shift[:, None, :]


def causal_conv(x, w, b):
    K = w.shape[0]
    S = x.shape[1]
    xp = jnp.pad(x, ((0, 0), (K - 1, 0), (0, 0)))
    return sum(xp[:, j:j + S] * w[j] for j in range(K)) + b


def alibi_slopes(n):
    return 2.0 ** (-8.0 * jnp.arange(1, n + 1, dtype=jnp.float32) / n)


def diff_attention(q, k, v, q_g, k_g, lam_p, subln_g, layer_idx):
    B, S, _ = q.shape
    q = rms_norm(q.reshape(B, S, DA_HEADS, 2, DA_HEAD_DIM).transpose(0, 2, 3, 1, 4), q_g)
    k = rms_norm(k.reshape(B, S, DA_HEADS, 2, DA_HEAD_DIM).transpose(0, 2, 3, 1, 4), k_g)
    v = v.reshape(B, S, DA_HEADS, 2 * DA_HEAD_DIM).transpose(0, 2, 1, 3)
    lambda_init = 0.8 - 0.6 * math.exp(-0.3 * layer_idx)
    lp = lam_p.astype(jnp.float32)
    lam = jnp.exp(jnp.sum(lp[0] * lp[1])) - jnp.exp(jnp.sum(lp[2] * lp[3])) + lambda_init
    slopes = alibi_slopes(DA_HEADS)[None, :, None, None, None]
    k_pos = jnp.arange(S)
    scale = DA_HEAD_DIM ** -0.5

    def block(i):
        start = i * Q_BLOCK
        qb = lax.dynamic_slice_in_dim(q, start, Q_BLOCK, axis=3)
        q_pos = start + jnp.arange(Q_BLOCK)
        logits = jnp.einsum('bhmqd,bhmkd->bhmqk', qb, k, preferred_element_type=jnp.float32) * scale
        dist = jnp.abs(q_pos[:, None] - k_pos[None, :]).astype(jnp.float32)
        allowed = (k_pos[None, :] // CHUNK) <= (q_pos[:, None] // CHUNK)
        logits = jnp.where(allowed, logits - slopes * dist, -jnp.inf)
        p = jax.nn.softmax(logits, axis=-1)
        p_diff = p[:, :, 0] - lam * p[:, :, 1]
        return jnp.einsum('bhqk,bhkd->bhqd', p_diff.astype(v.dtype), v)

    out = lax.map(block, jnp.arange(S // Q_BLOCK))
    out = out.transpose(1, 2, 0, 3, 4).reshape(B, DA_HEADS, S, 2 * DA_HEAD_DIM)
    out = rms_norm(out, subln_g) * (1.0 - lambda_init)
    return out.transpose(0, 2, 1, 3).reshape(B, S, DA_WIDTH)


def mlstm(q, k, v, i_pre, f_pre):
    B, S, _ = q.shape
    H, d, L = ML_HEADS, ML_HEAD_DIM, CHUNK
    NC = S // L
    f32 = jnp.float32

    def heads(t):
        return t.astype(f32).reshape(B, NC, L, H, d).transpose(0, 3, 1, 2, 4)

    def gates(t):
        return t.astype(f32).reshape(B, NC, L, H).transpose(0, 3, 1, 2)

    q, k, v = heads(q), heads(k) * (d ** -0.5), heads(v)
    ig = gates(i_pre)
    a = jnp.cumsum(jax.nn.log_sigmoid(gates(f_pre)), axis=-1)
    a_last = a[..., -1]
    causal = jnp.tril(jnp.ones((L, L), dtype=bool))
    log_d = jnp.where(causal, a[..., :, None] - a[..., None, :] + ig[..., None, :], -jnp.inf)
    g = a_last[..., None] - a + ig
    m_loc = jnp.max(g, axis=-1)
    w = jnp.exp(g - m_loc[..., None])
    dC = jnp.einsum('bhcl,bhcld,bhcle->bhcde', w, v, k)
    dn = jnp.einsum('bhcl,bhcle->bhce', w, k)

    def step(carry, inp):
        C, n, m = carry
        dC_c, dn_c, m_loc_c, a_last_c = inp
        m_new = jnp.maximum(a_last_c + m, m_loc_c)
        decay = jnp.exp(a_last_c + m - m_new)
        s_loc = jnp.exp(m_loc_c - m_new)
        C_new = decay[..., None, None] * C + s_loc[..., None, None] * dC_c
        n_new = decay[..., None] * n + s_loc[..., None] * dn_c
        return (C_new, n_new, m_new), (C, n, m)

    init = (jnp.zeros((B, H, d, d), f32), jnp.zeros((B, H, d), f32), jnp.zeros((B, H), f32))
    xs = (jnp.moveaxis(dC, 2, 0), jnp.moveaxis(dn, 2, 0), jnp.moveaxis(m_loc, 2, 0), jnp.moveaxis(a_last, 2, 0))
    _, (C_prev, n_prev, m_prev) = lax.scan(step, init, xs)
    C_prev = jnp.moveaxis(C_prev, 0, 2)
    n_prev = jnp.moveaxis(n_prev, 0, 2)
    m_prev = jnp.moveaxis(m_prev, 0, 2)

    m_t = jnp.maximum(a + m_prev[..., None], jnp.max(log_d, axis=-1))
    inter_w = jnp.exp(a + m_prev[..., None] - m_t)
    qk = jnp.einsum('bhcld,bhcsd->bhcls', q, k) * jnp.exp(log_d - m_t[..., None])
    num = inter_w[..., None] * jnp.einsum('bhcde,bhcle->bhcld', C_prev, q) + jnp.einsum('bhcls,bhcsd->bhcld', qk, v)
    den = inter_w * jnp.einsum('bhce,bhcle->bhcl', n_prev, q) + jnp.sum(qk, axis=-1)
    h = num / jnp.maximum(jnp.abs(den), jnp.exp(-m_t))[..., None]
    return h.transpose(0, 2, 3, 1, 4).reshape(B, S, H * d)


def forgetting_attention(q, k, v, f_pre, q_g, k_g):
    B, S, _ = q.shape

    def heads(t):
        return t.reshape(B, S, FX_HEADS, FX_HEAD_DIM).transpose(0, 2, 1, 3)

    q = rms_norm(heads(q), q_g)
    k = rms_norm(heads(k), k_g)
    v = heads(v)
    cum_logf = jnp.cumsum(jax.nn.log_sigmoid(f_pre.astype(jnp.float32)), axis=1).transpose(0, 2, 1)
    k_pos = jnp.arange(S)
    scale = FX_HEAD_DIM ** -0.5

    def block(i):
        start = i * Q_BLOCK
        qb = lax.dynamic_slice_in_dim(q, start, Q_BLOCK, axis=2)
        fq = lax.dynamic_slice_in_dim(cum_logf, start, Q_BLOCK, axis=2)
        q_pos = start + jnp.arange(Q_BLOCK)
        logits = jnp.einsum('bhqd,bhkd->bhqk', qb, k, preferred_element_type=jnp.float32) * scale
        logits = logits + fq[..., :, None] - cum_logf[..., None, :]
        logits = jnp.where(k_pos[None, :] <= q_pos[:, None], logits, -jnp.inf)
        p = jax.nn.softmax(logits, axis=-1)
        return jnp.einsum('bhqk,bhkd->bhqd', p.astype(v.dtype), v)

    out = lax.map(block, jnp.arange(S // Q_BLOCK))
    return out.transpose(1, 0, 3, 2, 4).reshape(B, S, FX_WIDTH)


def setup_inputs(seed: int = 0) -> dict:
    key = jax.random.key(seed)
    ks = jax.random.split(key, 24)
    f32 = jnp.float32

    def nrm(k, shape, s):
        return jax.random.normal(k, shape, f32) * s

    return {
        'x': nrm(ks[0], (BATCH, SEQ, D_MODEL), 1.0),
        'c': nrm(ks[1], (BATCH, D_MODEL), 1.0),
        'ada_w': nrm(ks[2], (DEPTH, D_MODEL, 6 * D_MODEL), 0.5 * D_MODEL ** -0.5),
        'ada_b': nrm(ks[3], (DEPTH, 6 * D_MODEL), 0.02),
        'norm_mix_g': 1.0 + nrm(ks[4], (DEPTH, D_MODEL), 0.02),
        'norm_ffn_g': 1.0 + nrm(ks[5], (DEPTH, D_MODEL), 0.02),
        'ab_w_in': nrm(ks[6], (N_EVEN, D_MODEL, AB_IN), D_MODEL ** -0.5),
        'ml_b_i': nrm(ks[7], (N_EVEN, ML_HEADS), 0.1),
        'ml_b_f': jnp.linspace(3.0, 6.0, ML_HEADS, dtype=f32)[None, :] + nrm(ks[8], (N_EVEN, ML_HEADS), 0.1),
        'ml_conv_w': nrm(ks[9], (N_EVEN, ML_CONV, 2 * ML_WIDTH), ML_CONV ** -0.5),
        'ml_conv_b': nrm(ks[10], (N_EVEN, 2 * ML_WIDTH), 0.02),
        'da_q_g': 1.0 + nrm(ks[11], (N_EVEN, DA_HEAD_DIM), 0.02),
        'da_k_g': 1.0 + nrm(ks[12], (N_EVEN, DA_HEAD_DIM), 0.02),
        'da_lambda': nrm(ks[13], (N_EVEN, 4, DA_HEAD_DIM), 0.1),
        'da_subln_g': 1.0 + nrm(ks[14], (N_EVEN, 2 * DA_HEAD_DIM), 0.02),
        'ab_w_out': nrm(ks[15], (N_EVEN, AB_OUT, D_MODEL), AB_OUT ** -0.5),
        'fx_w_in': nrm(ks[16], (N_ODD, D_MODEL, FX_IN), D_MODEL ** -0.5),
        'fx_b_f': jnp.linspace(1.0, 6.0, FX_HEADS, dtype=f32)[None, :] + nrm(ks[17], (N_ODD, FX_HEADS), 0.1),
        'fx_q_g': 1.0 + nrm(ks[18], (N_ODD, FX_HEAD_DIM), 0.02),
        'fx_k_g': 1.0 + nrm(ks[19], (N_ODD, FX_HEAD_DIM), 0.02),
        'fx_w_out': nrm(ks[20], (N_ODD, FX_WIDTH, D_MODEL), FX_WIDTH ** -0.5),
        'ffn_w1': nrm(ks[21], (DEPTH, D_MODEL, FFN_HIDDEN), D_MODEL ** -0.5),
        'ffn_w3': nrm(ks[22], (DEPTH, D_MODEL, FFN_HIDDEN), D_MODEL ** -0.5),
        'ffn_w2': nrm(ks[23], (DEPTH, FFN_HIDDEN, D_MODEL), FFN_HIDDEN ** -0.5),
    }


def reference(x, c, ada_w, ada_b, norm_mix_g, norm_ffn_g, ab_w_in, ml_b_i, ml_b_f, ml_conv_w, ml_conv_b,
              da_q_g, da_k_g, da_lambda, da_subln_g, ab_w_out, fx_w_in, fx_b_f, fx_q_g, fx_k_g, fx_w_out,
              ffn_w1, ffn_w3, ffn_w2):
    c_act = jax.nn.silu(c)
    ab_sizes = [DA_WIDTH] * 3 + [ML_WIDTH] * 4 + [ML_HEADS] * 2
    ab_split = np.cumsum(ab_sizes)[:-1].tolist()
    for l in range(DEPTH):
        mod = c_act @ ada_w[l] + ada_b[l]
        sh_m, sc_m, g_m, sh_f, sc_f, g_f = jnp.split(mod, 6, axis=-1)
        h = modulate(rms_norm(x, norm_mix_g[l]), sh_m, sc_m)
        j = l // 2
        if l % 2 == 0:
            z = h @ ab_w_in[j]
            da_q, da_k, da_v, ml_q, ml_k, ml_v, ml_o, ml_i, ml_f = jnp.split(z, ab_split, axis=-1)
            ml_qk = jax.nn.silu(causal_conv(jnp.concatenate([ml_q, ml_k], axis=-1), ml_conv_w[j], ml_conv_b[j]))
            ml_q, ml_k = jnp.split(ml_qk, 2, axis=-1)
            y_da = diff_attention(da_q, da_k, da_v, da_q_g[j], da_k_g[j], da_lambda[j], da_subln_g[j], l)
            y_ml = jax.nn.sigmoid(ml_o) * mlstm(ml_q, ml_k, ml_v, ml_i + ml_b_i[j], ml_f + ml_b_f[j])
            y = jnp.concatenate([y_da, y_ml.astype(h.dtype)], axis=-1) @ ab_w_out[j]
        else:
            z = h @ fx_w_in[j]
            fq, fk, fv, ff = jnp.split(z, [FX_WIDTH, 2 * FX_WIDTH, 3 * FX_WIDTH], axis=-1)
            y = forgetting_attention(fq, fk, fv, ff + fx_b_f[j], fx_q_g[j], fx_k_g[j]) @ fx_w_out[j]
        x = x + g_m[:, None, :] * y
        h = modulate(rms_norm(x, norm_ffn_g[l]), sh_f, sc_f)
        ffn = (jax.nn.silu(h @ ffn_w1[l]) * (h @ ffn_w3[l])) @ ffn_w2[l]
        x = x + g_f[:, None, :] * ffn
    return x
```

```python
import contextlib
import math
import numpy as np
import concourse.bass as bass
import concourse.mybir as mybir
from concourse.bass_utils import run_bass_kernel_spmd

F32 = mybir.dt.float32
BF16 = mybir.dt.bfloat16
ALU = mybir.AluOpType
AF = mybir.ActivationFunctionType

S = 4096
D = 1024
DEPTH = 4
TT = 512
NTT = S // TT
KC = 8
FFN = 2816
NF = FFN // 128
AB_IN = 3592
FX_IN = 3088
EPS = 1e-6
NEG = -30000.0

ENGS = ("pe", "act", "dve", "pool", "sp")
NDMA_SEM = 12


class Buf:
    __slots__ = ("name", "w", "r", "rd")

    def __init__(self, name=""):
        self.name = name
        self.w = None
        self.r = {}
        self.rd = []


class Op:
    __slots__ = ("eng", "fn", "deps", "idx", "ms", "cnt", "dsem", "dval", "isdma")

    def __init__(self, eng, fn, isdma):
        self.eng = eng
        self.fn = fn
        self.deps = []
        self.idx = -1
        self.ms = False
        self.cnt = 0
        self.dsem = None
        self.dval = 0
        self.isdma = isdma


class Prog:
    def __init__(self, nc, es):
        self.nc = nc
        self.esem = {e: es.enter_context(nc.semaphore("s_" + e)) for e in ENGS}
        self.dsem = {}
        for e in ("sp", "pool", "act"):
            for k in range(NDMA_SEM):
                self.dsem[(e, k)] = es.enter_context(nc.semaphore("d_%s%d" % (e, k)))
        self.base = {e: 0 for e in ENGS}
        self.dma_rr = {e: 0 for e in ENGS}
        self.dma_val = {}
        self.dma_last = {}
        self.streams = {e: [] for e in ENGS}
        self.touched = set()
        self.barrier = []
        self.total_ops = 0
        self.total_waits = 0

    def _add(self, eng, fn, reads, writes, isdma):
        op = Op(eng, fn, isdma)
        deps = op.deps
        for b in reads:
            if b.w is not None:
                deps.append(b.w)
        for b in writes:
            if b.w is not None:
                deps.append(b.w)
            deps.extend(b.r.values())
            deps.extend(b.rd)
        for b in reads:
            if isdma:
                b.rd.append(op)
            else:
                b.r[eng] = op
            self.touched.add(b)
        for b in writes:
            b.w = op
            b.r = {}
            b.rd = []
            self.touched.add(b)
        op.idx = len(self.streams[eng])
        self.streams[eng].append(op)
        return op

    def op(self, eng, fn, reads=(), writes=()):
        return self._add(eng, fn, reads, writes, False)

    def dma(self, eng, fn, reads=(), writes=()):
        op = self._add(eng, fn, reads, writes, True)
        k = self.dma_rr[eng]
        self.dma_rr[eng] = (k + 1) % NDMA_SEM
        key = (eng, k)
        prev = self.dma_last.get(key)
        if prev is not None:
            op.deps.append(prev)
        v = self.dma_val.get(key, 0) + 16
        self.dma_val[key] = v
        self.dma_last[key] = op
        op.dsem = key
        op.dval = v
        return op

    def flush(self, final=False):
        nc = self.nc
        waits = {e: [] for e in ENGS}
        for e in ENGS:
            maxidx = {}
            dwaited = {}
            for op in self.streams[e]:
                best = {}
                for d in op.deps:
                    if d.isdma:
                        if dwaited.get(d.dsem, 0) < d.dval:
                            cur = best.get(("d", d.dsem))
                            if cur is None or cur.dval < d.dval:
                                best[("d", d.dsem)] = d
                    else:
                        if d.eng == e and e == "pe":
                            continue
                        if maxidx.get(d.eng, -1) < d.idx:
                            cur = best.get(("e", d.eng))
                            if cur is None or cur.idx < d.idx:
                                best[("e", d.eng)] = d
                wl = []
                for key, d in best.items():
                    if key[0] == "d":
                        dwaited[d.dsem] = d.dval
                    else:
                        maxidx[d.eng] = d.idx
                        d.ms = True
                    wl.append(d)
                waits[e].append(wl)
        for e in ENGS:
            if self.streams[e]:
                last = self.streams[e][-1]
                if not last.isdma:
                    last.ms = True
        for e in ENGS:
            c = self.base[e]
            for op in self.streams[e]:
                if op.ms:
                    c += 1
                    op.cnt = c
            self.base[e] = c
        esem, dsem = self.esem, self.dsem
        barrier = self.barrier
        new_barrier = [("e", e, self.base[e]) for e in ENGS if self.base[e] > 0]
        new_barrier += [("d", key, v) for key, v in self.dma_val.items()]

        def emit(e, engobj):
            first = True
            for op, wl in zip(self.streams[e], waits[e]):
                if first:
                    first = False
                    for kind, key, v in barrier:
                        if kind == "e":
                            if key != e:
                                engobj.wait_ge(esem[key], v)
                        else:
                            engobj.wait_ge(dsem[key], v)
                for d in wl:
                    if d.isdma:
                        engobj.wait_ge(dsem[d.dsem], d.dval)
                    else:
                        engobj.wait_ge(esem[d.eng], d.cnt)
                ins = op.fn(engobj)
                if op.isdma:
                    ins.then_inc(dsem[op.dsem], 16)
                elif op.ms:
                    ins.then_inc(esem[e], 1)
            if final and e == "sp":
                for kind, key, v in new_barrier:
                    if kind == "e":
                        if key != e:
                            engobj.wait_ge(esem[key], v)
                    else:
                        engobj.wait_ge(dsem[key], v)

        with nc.Block() as block:
            @block.tensor
            def _(eng):
                emit("pe", eng)

            @block.scalar
            def _(eng):
                emit("act", eng)

            @block.vector
            def _(eng):
                emit("dve", eng)

            @block.gpsimd
            def _(eng):
                emit("pool", eng)

            @block.sync
            def _(eng):
                emit("sp", eng)

        for e in ENGS:
            self.total_ops += len(self.streams[e])
            self.total_waits += sum(len(w) for w in waits[e])
        self.barrier = new_barrier
        self.streams = {e: [] for e in ENGS}
        self.dma_last = {}
        for b in self.touched:
            b.w = None
            b.r = {}
            b.rd = []
        self.touched = set()


def MM(out, lhsT, rhs, start, stop):
    return lambda e: e.matmul(out, lhsT=lhsT, rhs=rhs, start=start, stop=stop, skip_group_check=True)


def ACT(out, in_, func, bias=None, scale=None):
    kw = {}
    if bias is not None:
        kw["bias"] = bias
    if scale is not None:
        kw["scale"] = scale
    return lambda e: e.activation(out=out, in_=in_, func=func, **kw)


def TT_(out, in0, in1, op):
    return lambda e: e.tensor_tensor(out=out, in0=in0, in1=in1, op=op)


def TS(out, in0, s1, s2, op0, op1=None):
    if op1 is None:
        return lambda e: e.tensor_scalar(out=out, in0=in0, scalar1=s1, scalar2=None, op0=op0)
    return lambda e: e.tensor_scalar(out=out, in0=in0, scalar1=s1, scalar2=s2, op0=op0, op1=op1)


def STT(out, in0, scalar, in1, op0, op1):
    return lambda e: e.scalar_tensor_tensor(out=out, in0=in0, scalar=scalar, in1=in1, op0=op0, op1=op1)


def CP(out, in_):
    return lambda e: e.tensor_copy(out=out, in_=in_)


def MS(ap, v):
    return lambda e: e.memset(ap, v)


def RCP(out, in_):
    return lambda e: e.reciprocal(out=out, in_=in_)


def DMA(out, in_):
    return lambda e: e.dma_start(out=out, in_=in_)


def SCAN(out, d0, d1, init, op0, op1):
    return lambda e: e.tensor_tensor_scan(out=out, data0=d0, data1=d1, initial=init, op0=op0, op1=op1)


def lambda_init(l):
    return 0.8 - 0.6 * math.exp(-0.3 * l)


def build(layers, x_in_name="xT", dbg=False):
    nc = bass.Bass("TRN2", target_bir_lowering=False)

    def din(name, shape):
        return nc.dram_tensor(name, list(shape), F32, kind="ExternalInput").ap()

    xT_d = din("xT", [D, S])
    cT_d = din("cT", [128, KC])
    ada_w_d = din("ada_w", [DEPTH, D, 6 * D])
    ada_bT_d = din("ada_bT", [DEPTH, 128, 48])
    gmixT_d = din("gmixT", [DEPTH, 128, KC])
    gffnT_d = din("gffnT", [DEPTH, 128, KC])
    ab_w_in_d = din("ab_w_in", [2, D, AB_IN])
    ab_w_out_d = din("ab_w_out", [2, D, D])
    fx_w_in_d = din("fx_w_in", [2, D, FX_IN])
    fx_w_out_d = din("fx_w_out", [2, D, D])
    w1_d = din("ffn_w1", [DEPTH, D, FFN])
    w3_d = din("ffn_w3", [DEPTH, D, FFN])
    w2_d = din("ffn_w2", [DEPTH, FFN, D])
    convw_d = din("convwT", [2, 128, 8, 4])
    convb_d = din("convbT", [2, 128, 8])
    gbe_d = din("gb_even", [2, 128, 256])
    gbo_d = din("gb_odd", [2, 128, 512])
    qkg_d = din("qkg", [DEPTH, 128, 2])
    subg_d = din("subg", [2, 128, 1])
    lpT_d = din("lpT", [2, 64, 4])
    cstf_d = din("cstf", [128, 3 * 128])
    cstb_d = din("cstb", [128, 8 * 128])
    datab_d = din("datab", [128, 4 * 35])
    sel_d = din("sel", [68, 4 * 128])
    daq_d = din("daqaug", [16, S])
    outT_d = nc.dram_tensor("outT", [D, S], F32, kind="ExternalOutput").ap()

    def dscr(name, shape, dt=BF16):
        kind = "ExternalOutput" if dbg else "Internal"
        return nc.dram_tensor(name, list(shape), dt, kind=kind).ap()

    qT_o = dscr("qT_o", [16, 68, S])
    kT_o = dscr("kT_o", [16, 68, S])
    V_o = dscr("V_o", [S, 16, 128])
    qT_da = dscr("qT_da", [8, 66, S])
    kT_da = dscr("kT_da", [8, 66, S])
    qT_ml = dscr("qT_ml", [4, 128, S])
    kT_ml = dscr("kT_ml", [4, 128, S])
    V_e = dscr("V_e", [S, 8, 128])
    osig = dscr("osig", [4, 128, S])
    yT_s = dscr("yT_s", [D, S])
    adaS = nc.dram_tensor("adaS", [12, 128, KC, 512], BF16, kind="Internal").ap()
    winA = nc.dram_tensor("winA", [20, 128, KC, 128], BF16, kind="Internal").ap()
    winB = nc.dram_tensor("winB", [2, 128, KC, 512], BF16, kind="Internal").ap()
    winG = nc.dram_tensor("winG", [128, KC, 16], BF16, kind="Internal").ap()
    woutS = nc.dram_tensor("woutS", [128, KC, D], BF16, kind="Internal").ap()
    w13s = nc.dram_tensor("w13s", [NF, 128, KC, 2, 128], BF16, kind="Internal").ap()
    w2s = nc.dram_tensor("w2s", [KC, 128, NF, 128], BF16, kind="Internal").ap()

    with contextlib.ExitStack() as es:
        P = Prog(nc, es)

        uid = [0]

        def sb(name, shape, dt, st=es):
            uid[0] += 1
            return st.enter_context(nc.sbuf_tensor("%s_%d" % (name, uid[0]), list(shape), dt))

        def ps(name, st, shape=(128, 512), dt=F32):
            uid[0] += 1
            return st.enter_context(nc.psum_tensor("%s_%d" % (name, uid[0]), list(shape), dt))

        X = sb("X", [128, KC, S], F32)
        XB = [[Buf("X%d_%d" % (c, t)) for t in range(NTT)] for c in range(KC)]
        cst = sb("cst_sb", [128, 3 * 128], F32)
        cstb = sb("cstb_sb", [128, 8 * 128], BF16)
        ident_b = cstb[:, 0:128]
        ones_b = cstb[:, 128:256]
        bones_b = cstb[:, 256:384]
        cm8_b = cstb[:, 384:512]
        dab8_b = [cstb[:, 512 + 128 * h: 640 + 128 * h] for h in range(4)]
        ident_f = cst[:, 0:128]
        ones_f = cst[:, 128:256]
        U_f = cst[:, 256:384]
        datab = sb("datab_sb", [128, 4 * 35], F32)
        selb = sb("selb", [68, 4 * 128], BF16)
        modT = sb("modT", [128, 48], F32)
        cols = sb("cols", [128, 64], F32)
        B_const = Buf("const")
        B_mod = Buf("mod")
        B_cols = Buf("cols")
        GSM, SHM, GM, GSF, SHF, GF = 0, 8, 16, 24, 32, 40
        QG, KG, SUBG, NLAM = 48, 49, 50, 51
        kbias = sb("kbias", [128, 16, 32], F32)
        B_kbias = Buf("kbias")
        graw = None
        grawB = Buf("graw")

        with contextlib.ExitStack() as ph:
            stage = sb("su_stage", [128, 8 * 128], F32, ph)
            stage2 = sb("su_stage2", [68, 4 * 128], F32, ph)
            Bs2 = Buf()
            rowsf = sb("su_rowsf", [16, S], F32, ph)
            rowsb = sb("su_rowsb", [16, S], BF16, ph)
            onesr = sb("su_onesr", [16, S], BF16, ph)
            Bs, Brf, Brb, Bor = Buf(), Buf(), Buf(), Buf()
            P.dma("sp", DMA(cst[:], cstf_d), writes=[B_const])
            P.dma("sp", DMA(stage[:], cstb_d), writes=[Bs])
            P.op("dve", CP(cstb[:], stage[:]), reads=[Bs], writes=[B_const])
            P.dma("sp", DMA(datab[:], datab_d), writes=[B_const])
            P.dma("sp", DMA(stage2[:], sel_d), writes=[Bs2])
            P.op("dve", CP(selb[:], stage2[:]), reads=[Bs2], writes=[B_const])
            for c in range(KC):
                P.dma("sp", DMA(X[:, c, :], xT_d[c * 128:(c + 1) * 128, :]),
                      writes=[XB[c][t] for t in range(NTT)])
            P.op("pool", MS(onesr[:], 1.0), writes=[Bor])
            P.dma("sp", DMA(kT_o[:, 64, :], onesr[:]), reads=[Bor])
            for r_ in (65, 66, 67):
                P.dma("sp", DMA(qT_o[:, r_, :], onesr[:]), reads=[Bor])
            P.dma("sp", DMA(kT_da[:, 64, :], onesr[0:8, :]), reads=[Bor])
            P.dma("sp", DMA(kT_da[:, 65, :], onesr[0:8, :]), reads=[Bor])
            P.dma("sp", DMA(rowsf[:], daq_d), writes=[Brf])
            P.op("dve", CP(rowsb[:], rowsf[:]), reads=[Brf], writes=[Brb])
            P.dma("sp", DMA(qT_da[:, 64, :], rowsb[0:8, :]), reads=[Brb])
            P.dma("sp", DMA(qT_da[:, 65, :], rowsb[8:16, :]), reads=[Brb])
            P.flush()

        def load_w_cast(ph_stage, stage_bufs, dst_ap, src_ap, dst_buf, n_free, state, engs=("pool",)):
            k = state[0] % len(stage_bufs)
            eng = engs[state[0] % len(engs)]
            state[0] += 1
            st_tile, st_buf = ph_stage[k], stage_bufs[k]
            P.dma("sp", DMA(st_tile[:, 0:n_free], src_ap), writes=[st_buf])
            if eng == "act":
                P.op("act", ACT(dst_ap, st_tile[:, 0:n_free], AF.Copy), reads=[st_buf], writes=[dst_buf])
            else:
                P.op(eng, CP(dst_ap, st_tile[:, 0:n_free]), reads=[st_buf], writes=[dst_buf])

        class PiecePipe:
            def __init__(self, pieces, ahead):
                self.p = pieces
                self.ia = 0
                self.ib = 0
                self.ahead = ahead

            def step(self, n=1):
                for _ in range(n):
                    while self.ia < len(self.p) and self.ia <= self.ib + self.ahead:
                        self.p[self.ia][0]()
                        self.ia += 1
                    if self.ib < len(self.p):
                        self.p[self.ib][1]()
                        self.ib += 1

            def drain(self):
                while self.ib < len(self.p):
                    self.step()

            def __len__(self):
                return len(self.p)

        def fm_groups(l):
            if l % 2 == 0:
                return [(0, 1024), (1536, 1024), (3072, 512)]
            return [(0, 1024), (1024, 1024)]

        def tm_groups(l):
            if l % 2 == 0:
                return [1024, 2560], (3584, 8)
            return [2048, 2560], (3072, 16)

        def precast_pieces(l, stf, stfB, stb, stbB, engs):
            even = (l % 2 == 0)
            j = l // 2
            win = (ab_w_in_d if even else fx_w_in_d)[j].rearrange("(kc p) n -> p kc n", p=128)
            wout = (ab_w_out_d if even else fx_w_out_d)[j].rearrange("(kc p) n -> p kc n", p=128)
            wada = ada_w_d[l].rearrange("(kc p) n -> p kc n", p=128)
            NS = len(stf)
            ctr = [0]
            pieces = []

            def mk(src, ncol, dst_fn, view=None):
                slot = [0]

                def fa():
                    k = ctr[0] % NS
                    eng = engs[ctr[0] % len(engs)]
                    ctr[0] += 1
                    slot[0] = k
                    P.dma("sp", DMA(stf[k][:, 0:ncol], src), writes=[stfB[k]])
                    if eng == "act":
                        P.op("act", ACT(stb[k][:, 0:ncol], stf[k][:, 0:ncol], AF.Copy), reads=[stfB[k]], writes=[stbB[k]])
                    else:
                        P.op(eng, CP(stb[k][:, 0:ncol], stf[k][:, 0:ncol]), reads=[stfB[k]], writes=[stbB[k]])

                def fb():
                    k = slot[0]
                    srcv = stb[k][:, 0:ncol]
                    if view is not None:
                        srcv = srcv.rearrange(view, n=128)
                    P.dma("sp", DMA(dst_fn(), srcv), reads=[stbB[k]])
                return (fa, fb)

            ch = 0
            for (c0, n) in fm_groups(l):
                for cc0 in range(0, n, 512):
                    for kc in range(KC):
                        def dst(ch_=ch + cc0 // 128, kc_=kc):
                            return winA[ch_:ch_ + 4, :, kc_, :].rearrange("c p n -> p c n")
                        pieces.append(mk(win[:, kc, c0 + cc0:c0 + cc0 + 512], 512, dst, "p (c n) -> p c n"))
                ch += n // 128
            tmc, (g0, ng) = tm_groups(l)
            for gi, c0 in enumerate(tmc):
                for kc in range(KC):
                    pieces.append(mk(win[:, kc, c0:c0 + 512], 512, lambda gi_=gi, kc_=kc: winB[gi_, :, kc_, :]))
            for kc in range(KC):
                pieces.append(mk(win[:, kc, g0:g0 + ng], ng, lambda kc_=kc, ng_=ng: winG[:, kc_, 0:ng_]))
            for kc in range(KC):
                for c0 in (0, 512):
                    pieces.append(mk(wout[:, kc, c0:c0 + 512], 512, lambda kc_=kc, c0_=c0: woutS[:, kc_, c0_:c0_ + 512]))
            for blk in range(12):
                for kc in range(KC):
                    pieces.append(mk(wada[:, kc, blk * 512:(blk + 1) * 512], 512, lambda b_=blk, kc_=kc: adaS[b_, :, kc_, :]))
            return pieces

        def phase_precast(l):
            with contextlib.ExitStack() as ph:
                NS = 10
                stf = [sb("pc_f%d" % i, [128, 512], F32, ph) for i in range(NS)]
                stfB = [Buf() for _ in range(NS)]
                stb = [sb("pc_b%d" % i, [128, 512], BF16, ph) for i in range(NS)]
                stbB = [Buf() for _ in range(NS)]
                pp_ = PiecePipe(precast_pieces(l, stf, stfB, stb, stbB, ("pool", "dve", "act")), NS - 2)
                pp_.drain()
                P.flush()

        def phase_ada(l):
            with contextlib.ExitStack() as ph:
                cT = sb("ad_cT", [128, KC], F32, ph)
                scb = sb("ad_scb", [128, KC], BF16, ph)
                abT = sb("ad_abT", [128, 48], F32, ph)
                gm = sb("ad_gm", [128, 2 * KC], F32, ph)
                wb = [sb("ad_wb%d" % i, [128, KC, 512], BF16, ph) for i in range(3)]
                wbB = [Buf() for _ in range(3)]
                pm = ps("ad_pm", ph, (128, 48))
                BcT, Bsc, Bab, Bgm, Bpm = Buf(), Buf(), Buf(), Buf(), Buf()
                st = [0]
                P.dma("sp", DMA(cT[:], cT_d), writes=[BcT])
                P.dma("sp", DMA(abT[:], ada_bT_d[l]), writes=[Bab])
                P.dma("sp", DMA(gm[:, 0:KC], gmixT_d[l]), writes=[Bgm])
                P.dma("sp", DMA(gm[:, KC:2 * KC], gffnT_d[l]), writes=[Bgm])
                P.dma("sp", DMA(cols[:, QG:QG + 2], qkg_d[l]), writes=[B_cols])
                P.op("act", ACT(scb[:], cT[:], AF.Silu), reads=[BcT], writes=[Bsc])
                wv = ada_w_d[l].rearrange("(kc p) n -> p kc n", p=128)
                def ld_ada(blk):
                    P.dma("sp", DMA(wb[blk % 3][:].rearrange("p kc n -> p (kc n)"),
                                    adaS[blk].rearrange("p kc n -> p (kc n)")), writes=[wbB[blk % 3]])
                ld_ada(0)
                ld_ada(1)
                for blk in range(12):
                    b = blk % 3
                    if blk + 2 < 12:
                        ld_ada(blk + 2)
                    for cc in range(4):
                        j = blk * 4 + cc
                        for kc in range(KC):
                            P.op("pe", MM(pm[:, j:j + 1], wb[b][:, kc, cc * 128:(cc + 1) * 128],
                                          scb[:, kc:kc + 1], kc == 0, kc == KC - 1),
                                 reads=[wbB[b], Bsc], writes=[Bpm])
                P.op("dve", TT_(modT[:], pm[:], abT[:], ALU.add), reads=[Bpm, Bab], writes=[B_mod])
                for (dst, g0, sc0, sh0, ga0) in ((GSM, 0, 8, 0, 16), (GSF, KC, 32, 24, 40)):
                    P.op("dve", STT(cols[:, dst:dst + 8], modT[:, sc0:sc0 + 8], 1.0, gm[:, g0:g0 + 8],
                                    ALU.add, ALU.mult), reads=[B_mod, Bgm], writes=[B_cols])
                    P.op("dve", CP(cols[:, dst + 8:dst + 16], modT[:, sh0:sh0 + 8]), reads=[B_mod], writes=[B_cols])
                    P.op("dve", CP(cols[:, dst + 16:dst + 24], modT[:, ga0:ga0 + 8]), reads=[B_mod], writes=[B_cols])
                if l % 2 == 0:
                    j = l // 2
                    lp = sb("ad_lp", [64, 4], F32, ph)
                    pr = sb("ad_pr", [64, 2], F32, ph)
                    ee = sb("ad_ee", [128, 2], F32, ph)
                    pl = ps("ad_pl", ph, (128, 2))
                    Blp, Bpr, Bee, Bpl = Buf(), Buf(), Buf(), Buf()
                    P.dma("sp", DMA(lp[:], lpT_d[j]), writes=[Blp])
                    P.dma("sp", DMA(cols[:, SUBG:SUBG + 1], subg_d[j]), writes=[B_cols])
                    P.op("dve", TT_(pr[:, 0:1], lp[:, 0:1], lp[:, 1:2], ALU.mult), reads=[Blp], writes=[Bpr])
                    P.op("dve", TT_(pr[:, 1:2], lp[:, 2:3], lp[:, 3:4], ALU.mult), reads=[Blp], writes=[Bpr])
                    P.op("pe", MM(pl[:], ones_f[0:64, :], pr[:], True, True), reads=[Bpr, B_const], writes=[Bpl])
                    P.op("act", ACT(ee[:], pl[:], AF.Exp), reads=[Bpl], writes=[Bee])
                    P.op("dve", TT_(cols[:, NLAM:NLAM + 1], ee[:, 1:2], ee[:, 0:1], ALU.subtract),
                         reads=[Bee], writes=[B_cols])
                    P.op("dve", TS(cols[:, NLAM:NLAM + 1], cols[:, NLAM:NLAM + 1], -lambda_init(l), None, ALU.add),
                         reads=[B_cols], writes=[B_cols])
                P.flush()

        def emit_rstd(ph, tt, rstd_ap, sqs, sqB, pss, pssB, rstdB):
            for c in range(KC):
                k = c % len(sqs)
                P.op("act", ACT(sqs[k][:], X[:, c, tt * TT:(tt + 1) * TT], AF.Square),
                     reads=[XB[c][tt]], writes=[sqB[k]])
                P.op("pe", MM(pss[:], ones_b, sqs[k][:], c == 0, c == KC - 1),
                     reads=[sqB[k], B_const], writes=[pssB])
            P.op("act", ACT(rstd_ap, pss[:], AF.Ln, bias=EPS, scale=1.0 / D), reads=[pssB], writes=[rstdB])
            P.op("act", ACT(rstd_ap, rstd_ap, AF.Exp, scale=-0.5), reads=[rstdB], writes=[rstdB])

        def emit_hT(tt, hT, hTB, rstd_ap, rstdB, tmpf, tmpfB, gs0, sh0):
            for c in range(KC):
                k = c % len(tmpf)
                P.op("dve", STT(tmpf[k][:], X[:, c, tt * TT:(tt + 1) * TT], cols[:, gs0 + c:gs0 + c + 1], rstd_ap,
                                ALU.mult, ALU.mult), reads=[XB[c][tt], B_cols, rstdB], writes=[tmpfB[k]])
                P.op("act", ACT(hT[:, c, :], tmpf[k][:], AF.Identity, bias=cols[:, sh0 + c:sh0 + c + 1]),
                     reads=[tmpfB[k], B_cols], writes=[hTB])

        def phase_mixin(l):
            even = (l % 2 == 0)
            j = l // 2
            with contextlib.ExitStack() as ph:
                rstd = [sb("mi_rstd%d" % i, [128, TT], F32, ph) for i in range(2)]
                rstdB = [Buf(), Buf()]
                hT = [sb("mi_hT%d" % i, [128, KC, TT], BF16, ph) for i in range(2)]
                hTB = [Buf(), Buf()]
                sqs = [sb("mi_sq%d" % i, [128, TT], BF16, ph) for i in range(2)]
                sqB = [Buf(), Buf()]
                tmpf = [sb("mi_tmpf%d" % i, [128, TT], F32, ph) for i in range(2)]
                tmpfB = [Buf(), Buf()]
                qsq = [sb("mi_qsq%d" % i, [128, TT], BF16, ph) for i in range(2)]
                qsqB = [Buf(), Buf()]
                qrs = [sb("mi_qrs%d" % i, [128, TT], F32, ph) for i in range(2)]
                qrsB = [Buf(), Buf()]
                wa = [sb("mi_wa%d" % i, [128, KC, 128], BF16, ph) for i in range(4)]
                waB = [Buf() for _ in range(4)]
                wbt = [sb("mi_wb%d" % i, [128, KC, 512], BF16, ph) for i in range(2)]
                wbtB = [Buf(), Buf()]
                wgt = sb("mi_wgt", [128, KC, 16], BF16, ph)
                wgtB = Buf()
                stg = [sb("mi_stg%d" % i, [128, TT], BF16, ph) for i in range(3)]
                stgB = [Buf() for _ in range(3)]
                vst = [sb("mi_vst%d" % i, [128, 8, 128], BF16, ph) for i in range(2)]
                vstB = [Buf(), Buf()]
                pss = ps("mi_pss", ph)
                pssB = Buf()
                pz = [ps("mi_pz%d" % i, ph) for i in range(3)]
                pzB = [Buf() for _ in range(3)]
                pn = [ps("mi_pn%d" % i, ph) for i in range(2)]
                pnB = [Buf(), Buf()]
                pg = ps("mi_pg", ph)
                pgB = Buf()
                cnt = {"z": 0, "stg": 0, "vst": 0, "q": 0, "wa": 0}
                if even:
                    raw = sb("mi_raw", [128, TT + 3], F32, ph)
                    rawB = Buf()
                    halo = sb("mi_halo", [128, 8, 3], F32, ph)
                    haloB = Buf()
                    acc = sb("mi_acc", [128, TT], F32, ph)
                    accB = Buf()
                    cw = sb("mi_cw", [128, 8, 4], F32, ph)
                    cb = sb("mi_cb", [128, 8], F32, ph)
                    BcwB = Buf()
                    P.dma("sp", DMA(cw[:], convw_d[j]), writes=[BcwB])
                    P.dma("sp", DMA(cb[:], convb_d[j]), writes=[BcwB])
                else:
                    for i in range(2):
                        P.op("pool", MS(vst[i][:], 1.0), writes=[vstB[i]])
                ng = 8 if even else 16
                P.dma("sp", DMA(wgt[:, :, 0:ng], winG[:, :, 0:ng]), writes=[wgtB])
                chunks = []
                if even:
                    for cc in range(8):
                        chunks.append(("qk", cc, ("da", cc)))
                    for cc in range(8):
                        chunks.append(("conv", 8 + cc, cc))
                    for cc in range(4):
                        chunks.append(("sig", 16 + cc, cc))
                    vaux = [0, 4]
                else:
                    for cc in range(8):
                        chunks.append(("qk", cc, ("fq", cc)))
                    for cc in range(8):
                        chunks.append(("qk", 8 + cc, ("fk", cc)))
                    vaux = [0, 8]
                NCH = len(chunks)
                total = NTT * NCH

                def load_wa(gi):
                    ch = chunks[gi % NCH][1]
                    k = gi % 4
                    P.dma("sp", DMA(wa[k][:].rearrange("p kc n -> p (kc n)"), winA[ch].rearrange("p kc n -> p (kc n)")),
                          writes=[waB[k]])

                def load_wb(gi):
                    k = gi % 2
                    P.dma("sp", DMA(wbt[k][:].rearrange("p kc n -> p (kc n)"), winB[gi % 2].rearrange("p kc n -> p (kc n)")),
                          writes=[wbtB[k]])

                def norm_tile(tt):
                    k = tt % 2
                    emit_rstd(ph, tt, rstd[k][:], sqs, sqB, pss, pssB, rstdB[k])
                    emit_hT(tt, hT[k], hTB[k], rstd[k][:], rstdB[k], tmpf, tmpfB, GSM, SHM)

                load_wa(0)
                load_wa(1)
                load_wa(2)
                norm_tile(0)
                for tt in range(NTT):
                    tsl = slice(tt * TT, (tt + 1) * TT)
                    h_, h_B = hT[tt % 2], hTB[tt % 2]
                    load_wb(2 * tt)
                    load_wb(2 * tt + 1)
                    for ci, (kind, ch, aux) in enumerate(chunks):
                        gi = tt * NCH + ci
                        if gi + 3 < total:
                            load_wa(gi + 3)
                        if ci == NCH // 2 and tt + 1 < NTT:
                            norm_tile(tt + 1)
                        wt, wtB = wa[gi % 4], waB[gi % 4]
                        z = pz[cnt["z"] % 3]
                        zB = pzB[cnt["z"] % 3]
                        cnt["z"] += 1
                        for kc in range(KC):
                            P.op("pe", MM(z[:], wt[:, kc, :], h_[:, kc, :], kc == 0, kc == KC - 1),
                                 reads=[wtB, h_B], writes=[zB])
                        sg = stg[cnt["stg"] % 3]
                        sgB = stgB[cnt["stg"] % 3]
                        cnt["stg"] += 1
                        if kind == "qk":
                            qi = cnt["q"] % 2
                            cnt["q"] += 1
                            P.op("act", ACT(qsq[qi][:], z[:], AF.Square), reads=[zB], writes=[qsqB[qi]])
                            P.op("pe", MM(pn[qi][:], bones_b, qsq[qi][:], True, True),
                                 reads=[qsqB[qi], B_const], writes=[pnB[qi]])
                            P.op("act", ACT(qrs[qi][:], pn[qi][:], AF.Ln, bias=EPS, scale=1.0 / 64), reads=[pnB[qi]], writes=[qrsB[qi]])
                            P.op("act", ACT(qrs[qi][:], qrs[qi][:], AF.Exp, scale=-0.5), reads=[qrsB[qi]], writes=[qrsB[qi]])
                            typ, cc = aux
                            if typ == "da":
                                isq = cc < 4
                                dst = qT_da if isq else kT_da
                                u0 = 2 * (cc % 4)
                            else:
                                isq = (typ == "fq")
                                dst = qT_o if isq else kT_o
                                u0 = 2 * cc
                            gcol = cols[:, QG:QG + 1] if isq else cols[:, KG:KG + 1]
                            P.op("dve", STT(sg[:], z[:], gcol, qrs[qi][:], ALU.mult, ALU.mult),
                                 reads=[zB, B_cols, qrsB[qi]], writes=[sgB])
                            P.dma("sp", DMA(dst[u0, 0:64, tsl], sg[0:64, :]), reads=[sgB])
                            P.dma("sp", DMA(dst[u0 + 1, 0:64, tsl], sg[64:128, :]), reads=[sgB])
                        elif kind == "conv":
                            cc = aux
                            if tt == 0:
                                P.op("pool", MS(raw[:, 0:3], 0.0), writes=[rawB])
                            else:
                                P.op("pool", CP(raw[:, 0:3], halo[:, cc, :]), reads=[haloB], writes=[rawB])
                            P.op("act", ACT(raw[:, 3:TT + 3], z[:], AF.Copy), reads=[zB], writes=[rawB])
                            P.op("pool", CP(halo[:, cc, :], raw[:, TT:TT + 3]), reads=[rawB], writes=[haloB])
                            P.op("dve", TS(acc[:], raw[:, 0:TT], cw[:, cc, 0:1], cb[:, cc:cc + 1], ALU.mult, ALU.add),
                                 reads=[rawB, BcwB], writes=[accB])
                            for tap in range(1, 4):
                                P.op("dve", STT(acc[:], raw[:, tap:tap + TT], cw[:, cc, tap:tap + 1], acc[:],
                                                ALU.mult, ALU.add), reads=[rawB, BcwB, accB], writes=[accB])
                            P.op("act", ACT(sg[:], acc[:], AF.Silu), reads=[accB], writes=[sgB])
                            dst = qT_ml if cc < 4 else kT_ml
                            P.dma("sp", DMA(dst[cc % 4, :, tsl], sg[:]), reads=[sgB])
                        else:
                            cc = aux
                            P.op("act", ACT(sg[:], z[:], AF.Sigmoid), reads=[zB], writes=[sgB])
                            P.dma("sp", DMA(osig[cc, :, tsl], sg[:]), reads=[sgB])
                    for gi2 in range(2):
                        wt, wtB = wbt[gi2], wbtB[gi2]
                        for sub in range(4):
                            z = pz[cnt["z"] % 3]
                            zB = pzB[cnt["z"] % 3]
                            cnt["z"] += 1
                            for kc in range(KC):
                                P.op("pe", MM(z[:], h_[:, kc, sub * 128:(sub + 1) * 128], wt[:, kc, :],
                                              kc == 0, kc == KC - 1), reads=[wtB, h_B], writes=[zB])
                            vs = vst[cnt["vst"] % 2]
                            vsB = vstB[cnt["vst"] % 2]
                            cnt["vst"] += 1
                            t0 = tt * TT + sub * 128
                            if even:
                                P.op("act", ACT(vs[:, 0:4, :], z[:].rearrange("p (h d) -> p h d", d=128), AF.Copy),
                                     reads=[zB], writes=[vsB])
                                P.dma("sp", DMA(V_e[t0:t0 + 128, vaux[gi2]:vaux[gi2] + 4, :], vs[:, 0:4, :]), reads=[vsB])
                            else:
                                P.op("act", ACT(vs[:, :, 0:64], z[:].rearrange("p (h d) -> p h d", d=64), AF.Copy),
                                     reads=[zB], writes=[vsB])
                                P.dma("sp", DMA(V_o[t0:t0 + 128, vaux[gi2]:vaux[gi2] + 8, :], vs[:]), reads=[vsB])
                    for sub in range(4):
                        ti = tt * 4 + sub
                        for kc in range(KC):
                            P.op("pe", MM(pg[:, ti * ng:(ti + 1) * ng], h_[:, kc, sub * 128:(sub + 1) * 128],
                                          wgt[:, kc, 0:ng], kc == 0, kc == KC - 1),
                                 reads=[wgtB, h_B], writes=[pgB])
                P.op("dve", CP(graw[:, 0:32 * ng], pg[:, 0:32 * ng]), reads=[pgB], writes=[grawB])
                P.flush()

        def phase_gates(l):
            even = (l % 2 == 0)
            j = l // 2
            with contextlib.ExitStack() as ph:
                ng = 8 if even else 16
                nh = 4 if even else 16
                gb = sb("mi_gb", [128, 512], F32, ph)
                gbB = Buf()
                graw2 = sb("mi_graw2", [128, 32 * ng], F32, ph)
                graw2B = Buf()
                spt = sb("mi_spt", [128, 16, 32], F32, ph)
                sptB = Buf()
                tot = sb("mi_tot", [128, 16, 32], F32, ph)
                totB = Buf()
                inc = sb("mi_inc", [128, 16, 32], F32, ph)
                incB = Buf()
                fpos = sb("mi_fpos", [128, 16, 32], F32, ph)
                fposB = Buf()
                onesc = sb("mi_onesc", [128, 32], F32, ph)
                onescB = Buf()
                pc = ps("mi_pc", ph)
                pcB = Buf()
                pt = ps("mi_pt", ph)
                ptB = Buf()
                P.dma("sp", DMA(gb[:, 0:32 * ng], (gbe_d if even else gbo_d)[j]), writes=[gbB])
                P.op("pool", MS(onesc[:], 1.0), writes=[onescB])
                P.op("dve", TT_(graw2[:, 0:32 * ng], graw[:, 0:32 * ng], gb[:, 0:32 * ng], ALU.add),
                     reads=[grawB, gbB], writes=[graw2B])
                gv = graw2[:].rearrange("p (t g) -> p g t", g=ng)
                f0 = 4 if even else 0
                P.op("act", ACT(spt[:, 0:nh, :], gv[:, f0:f0 + nh, :], AF.Exp, scale=-1.0), reads=[graw2B], writes=[sptB])
                P.op("act", ACT(spt[:, 0:nh, :], spt[:, 0:nh, :], AF.Ln, bias=1.0), reads=[sptB], writes=[sptB])
                spf = spt[:, 0:nh, :].rearrange("p h t -> p (h t)")
                P.op("pe", MM(pc[:, 0:nh * 32], U_f, spf, True, True), reads=[sptB, B_const], writes=[pcB])
                P.op("pe", MM(pt[:, 0:nh * 32], ones_f, spf, True, True), reads=[sptB, B_const], writes=[ptB])
                P.op("dve", CP(tot[:, 0:nh, :].rearrange("p h t -> p (h t)"), pt[:, 0:nh * 32]), reads=[ptB], writes=[totB])
                for h in range(nh):
                    P.op("dve", SCAN(inc[:, h, :], onesc[:], tot[:, h, :], 0.0, ALU.mult, ALU.add),
                         reads=[totB, onescB], writes=[incB])
                P.op("dve", TT_(inc[:, 0:nh, :], inc[:, 0:nh, :], tot[:, 0:nh, :], ALU.subtract), reads=[incB, totB], writes=[incB])
                P.op("dve", TT_(fpos[:, 0:nh, :].rearrange("p h t -> p (h t)"), pc[:, 0:nh * 32],
                                inc[:, 0:nh, :].rearrange("p h t -> p (h t)"), ALU.add), reads=[pcB, incB], writes=[fposB])
                if even:
                    P.op("dve", STT(kbias[:, 0:4, :], gv[:, 0:4, :], math.log(128 ** -0.5), fpos[:, 0:4, :], ALU.add, ALU.add),
                         reads=[graw2B, fposB], writes=[B_kbias])
                else:
                    P.op("dve", CP(kbias[:, :, :], fpos[:, :, :]), reads=[fposB], writes=[B_kbias])
                prow = [ps("mi_prow%d" % i, ph, (16, 2048)) for i in range(1)]
                prowB = Buf()
                frow = sb("mi_frow", [16, S], F32, ph)
                frowB = Buf()
                fposT = sb("mi_fposT", [128, 32, 16], F32, ph)
                fposTB = Buf()
                P.op("dve", CP(fposT[:, :, 0:nh].rearrange("p t h -> p h t"), fpos[:, 0:nh, :]), reads=[fposB], writes=[fposTB])
                for half in range(2):
                    for t8 in range(16):
                        ti = half * 16 + t8
                        P.op("pe", MM(prow[0][0:nh, t8 * 128:(t8 + 1) * 128], fposT[:, ti, 0:nh], ident_f, True, True),
                             reads=[fposTB, B_const], writes=[prowB])
                    P.op("act", ACT(frow[0:nh, half * 2048:(half + 1) * 2048], prow[0][0:nh, :], AF.Copy,
                                    scale=(-1.0 if even else -8.0)), reads=[prowB], writes=[frowB])
                if even:
                    fsp = sb("mi_fsp", [68, S], BF16, ph)
                    fspB = Buf()
                    r1 = sb("mi_r1", [4, S], F32, ph)
                    r1B = Buf()
                    hb = sb("mi_hb", [4, S], BF16, ph)
                    hbB = Buf()
                    P.op("pool", MS(fsp[:], 0.0), writes=[fspB])
                    P.op("act", ACT(fsp[0:4, :], frow[0:4, :], AF.Copy), reads=[frowB, fspB], writes=[fspB])
                    P.op("dve", TT_(r1[:], frow[0:4, :], fsp[0:4, :], ALU.subtract), reads=[frowB, fspB], writes=[r1B])
                    P.op("act", ACT(hb[:], r1[:], AF.Copy), reads=[r1B], writes=[hbB])
                    P.op("act", ACT(fsp[32:36, :], hb[:], AF.Copy), reads=[hbB, fspB], writes=[fspB])
                    P.op("dve", TT_(r1[:], r1[:], hb[:], ALU.subtract), reads=[r1B, hbB], writes=[r1B])
                    P.op("act", ACT(fsp[64:68, :], r1[:], AF.Copy), reads=[r1B, fspB], writes=[fspB])
                    P.dma("sp", DMA(fsplit_d[:, :], fsp[:]), reads=[fspB])
                else:
                    frb = sb("mi_frb", [16, S], BF16, ph)
                    frbB = Buf()
                    P.op("act", ACT(frb[:], frow[:], AF.Copy), reads=[frowB], writes=[frbB])
                    P.dma("sp", DMA(qT_o[:, 64, :], frb[:]), reads=[frbB])
                    kh = [sb("mi_kh%d" % i, [16, S], BF16, ph) for i in range(3)]
                    khB = [Buf() for _ in range(3)]
                    P.op("dve", TS(frow[:], frow[:], -1.0, None, ALU.mult), reads=[frowB, frbB], writes=[frowB])
                    for i in range(3):
                        P.op("act", ACT(kh[i][:], frow[:], AF.Copy), reads=[frowB], writes=[khB[i]])
                        if i < 2:
                            P.op("dve", TT_(frow[:], frow[:], kh[i][:], ALU.subtract), reads=[frowB, khB[i]], writes=[frowB])
                        P.dma("sp", DMA(kT_o[:, 65 + i, :], kh[i][:]), reads=[khB[i]])
                P.flush()

        fsplit_d = dscr("fsplit", [68, S])

        def phase_attn(l):
            even = (l % 2 == 0)
            with contextlib.ExitStack() as ph:
                kT = [sb("at_kT%d" % i, [128, S], BF16, ph) for i in range(2)]
                kTB = [Buf(), Buf()]
                Vt = [sb("at_V%d" % i, [128, 32, 128], BF16, ph) for i in range(2)]
                VB = [Buf(), Buf()]
                qT = [sb("at_qT%d" % i, [128, TT], BF16, ph) for i in range(2)]
                qTB = [Buf(), Buf()]
                NP = 4 if even else 6
                pT = [sb("at_pT%d" % i, [128, TT], BF16, ph) for i in range(NP)]
                pTB = [Buf() for _ in range(NP)]
                ysg = [sb("at_ysg%d" % i, [128, TT], BF16, ph) for i in range(2)]
                ysgB = [Buf(), Buf()]
                tA = sb("at_tA", [128, TT], F32, ph)
                tAB = Buf()
                tB_ = sb("at_tB", [128, TT], F32, ph)
                tBB = Buf()
                NSB = 4 if even else 6
                bank = [ps("at_bk%d" % i, ph) for i in range(NSB)]
                bankB = [Buf() for _ in range(NSB)]
                p_o = [ps("at_po%d" % i, ph) for i in range(2)]
                p_oB = [Buf(), Buf()]
                NWC = 2 if even else 4
                wcf = [sb("at_wcf%d" % i, [128, 512], F32, ph) for i in range(NWC)]
                wcfB = [Buf() for _ in range(NWC)]
                wcb = [sb("at_wcb%d" % i, [128, 512], BF16, ph) for i in range(NWC)]
                wcbB = [Buf() for _ in range(NWC)]
                cnt = {"s": 0, "p": 0, "y": 0, "d": 0}
                if even:
                    dT = [sb("at_dT%d" % i, [128, TT], BF16, ph) for i in range(2)]
                    dTB = [Buf() for _ in range(2)]
                    negF = [sb("at_negF%d" % i, [128, TT], F32, ph) for i in range(2)]
                    negFB = [Buf(), Buf()]
                    dtmp = sb("at_dtmp", [128, 128], F32, ph)
                    dtmpB = Buf()
                    zacc1 = sb("at_zacc", [128, TT], F32, ph)
                    zacc1B = Buf()
                    zacc = [zacc1, zacc1]
                    zaccB = [zacc1B, zacc1B]
                    r0 = sb("at_r0", [128, TT], F32, ph)
                    r0B = Buf()
                    sqb = sb("at_sqb", [128, TT], BF16, ph)
                    sqbB = Buf()
                    fsp = sb("at_fsp", [68, S], BF16, ph)
                    fspB = Buf()
                    og = [sb("at_og%d" % i, [128, TT], BF16, ph) for i in range(2)]
                    ogB = [Buf(), Buf()]
                    p_z = [ps("at_pz%d" % i, ph) for i in range(2)]
                    p_zB = [Buf(), Buf()]
                    P.dma("sp", DMA(fsp[:], fsplit_d[:, :]), writes=[fspB])
                    items = [("da", h) for h in range(4)] + [("ml", h) for h in range(4)]
                else:
                    items = [("fx", h) for h in range(16)]

                def vview(src, h):
                    return src[:, h, :].rearrange("(t p) d -> p t d", p=128)

                def item_bufs(ii):
                    kind, h = items[ii]
                    if kind == "da":
                        return (0, 1), h % 2
                    return (ii % 2,), ii % 2

                def load_item(ii):
                    kind, h = items[ii]
                    kb, vb = item_bufs(ii)
                    if kind == "fx":
                        P.dma("sp", DMA(kT[kb[0]][0:68, :], kT_o[h]), writes=[kTB[kb[0]]])
                        P.dma("sp", DMA(Vt[vb][:], vview(V_o, h)), writes=[VB[vb]])
                    elif kind == "da":
                        for m in range(2):
                            P.dma("sp", DMA(kT[m][0:66, :], kT_da[2 * h + m]), writes=[kTB[m]])
                        P.dma("sp", DMA(Vt[vb][:], vview(V_e, h)), writes=[VB[vb]])
                    else:
                        P.dma("sp", DMA(kT[kb[0]][:], kT_ml[h]), writes=[kTB[kb[0]]])
                        P.dma("sp", DMA(Vt[vb][:], vview(V_e, 4 + h)), writes=[VB[vb]])

                steps = []
                for ii, (kind, h) in enumerate(items):
                    for jq in range(NTT):
                        for m in range(2 if kind == "da" else 1):
                            steps.append((ii, kind, h, m, jq))

                def load_q(si):
                    ii, kind, h, m, jq = steps[si]
                    t0 = jq * TT
                    qq, qqB = qT[si % 2], qTB[si % 2]
                    if kind == "fx":
                        P.dma("sp", DMA(qq[0:68, :], qT_o[h][:, t0:t0 + TT]), writes=[qqB])
                    elif kind == "da":
                        P.dma("sp", DMA(qq[0:66, :], qT_da[2 * h + m][:, t0:t0 + TT]), writes=[qqB])
                    else:
                        P.dma("sp", DMA(qq[:, :], qT_ml[h][:, t0:t0 + TT]), writes=[qqB])
                        P.dma("sp", DMA(og[si % 2][:], osig[h, :, t0:t0 + TT]), writes=[ogB[si % 2]])

                def step_blocks(si):
                    jq = steps[si][4]
                    blocks = [(kt, 0, TT, False) for kt in range(4 * jq)]
                    for r in range(4):
                        blocks.append((4 * jq + r, 128 * r, 128 * r + 128, True))
                        if r < 3:
                            blocks.append((4 * jq + r, 128 * (r + 1), TT, False))
                    return blocks

                sblocks = [step_blocks(si) for si in range(len(steps))]
                touched = {}
                loaded_items = set()
                loaded_q = set()

                def ensure_loaded(si):
                    ii = steps[si][0]
                    if ii not in loaded_items:
                        load_item(ii)
                        loaded_items.add(ii)
                    if si not in loaded_q:
                        load_q(si)
                        loaded_q.add(si)

                def step_ctx(si):
                    ii, kind, h, m, jq = steps[si]
                    kb, vb = item_bufs(ii)
                    if kind == "da":
                        kk, kkB = kT[m], kTB[m]
                    else:
                        kk, kkB = kT[kb[0]], kTB[kb[0]]
                    return kind, h, m, jq, kk, kkB, Vt[vb], VB[vb], qT[si % 2], qTB[si % 2]

                def issue_scores(si, bi):
                    kind, h, m, jq, kk, kkB, vv, vvB, qq, qqB = step_ctx(si)
                    t0 = jq * TT
                    kt, c0, c1, diag = sblocks[si][bi]
                    nsb = NSB
                    si_ = cnt["s"] % nsb
                    cnt["s"] += 1
                    s_t, s_B = bank[si_], bankB[si_]
                    ks = slice(kt * 128, kt * 128 + 128)
                    if kind == "fx":
                        P.op("pe", MM(s_t[:, c0:c1], kk[0:68, ks], qq[0:68, c0:c1], True, not diag),
                             reads=[kkB, qqB], writes=[s_B])
                        if diag:
                            P.op("pe", MM(s_t[:, c0:c1], ident_b, cm8_b, False, True), reads=[B_const], writes=[s_B])
                        return (s_t, s_B, None, None)
                    if kind == "da":
                        if diag:
                            P.op("pe", MM(s_t[:, c0:c1], kk[0:64, ks], qq[0:64, c0:c1], True, False),
                                 reads=[kkB, qqB], writes=[s_B])
                            P.op("pe", MM(s_t[:, c0:c1], ident_b, dab8_b[h], False, True), reads=[B_const], writes=[s_B])
                        else:
                            P.op("pe", MM(s_t[:, c0:c1], kk[0:66, ks], qq[0:66, c0:c1], True, True),
                                 reads=[kkB, qqB], writes=[s_B])
                        return (s_t, s_B, None, None)
                    P.op("pe", MM(s_t[:, c0:c1], kk[:, ks], qq[:, c0:c1], True, True), reads=[kkB, qqB], writes=[s_B])
                    return (s_t, s_B, None, None)

                def finish_block(si, bi, sc):
                    kind, h, m, jq, kk, kkB, vv, vvB, qq, qqB = step_ctx(si)
                    kt, c0, c1, diag = sblocks[si][bi]
                    s_t, s_B, e_t, e_B = sc
                    pi = cnt["p"] % NP
                    cnt["p"] += 1
                    pp, ppB = pT[pi], pTB[pi]
                    po, poB = p_o[si % 2], p_oB[si % 2]
                    if kind == "fx":
                        P.op("act", ACT(pp[:, c0:c1], s_t[:, c0:c1], AF.Exp, scale=0.125),
                             reads=[s_B], writes=[ppB])
                    elif kind == "da":
                        if diag:
                            P.op("act", ACT(pp[:, c0:c1], s_t[:, c0:c1], AF.Exp, scale=0.125), reads=[s_B], writes=[ppB])
                        else:
                            dd = 4 * jq - kt + 3
                            P.op("act", ACT(pp[:, c0:c1], s_t[:, c0:c1], AF.Exp, bias=datab[:, h * 35 + dd:h * 35 + dd + 1],
                                            scale=0.125), reads=[s_B, B_const], writes=[ppB])
                    else:
                        di = cnt["d"] % 2
                        cnt["d"] += 1
                        nf, nfB = negF[si % 2], negFB[si % 2]
                        if diag:
                            P.op("dve", TT_(dtmp[:], nf[:, c0:c1], cm8_b, ALU.add), reads=[nfB, B_const], writes=[dtmpB])
                            P.op("act", ACT(dT[di][:, c0:c1], dtmp[:], AF.Exp, bias=kbias[:, h, kt:kt + 1]),
                                 reads=[dtmpB, B_kbias], writes=[dTB[di]])
                        else:
                            P.op("act", ACT(dT[di][:, c0:c1], nf[:, c0:c1], AF.Exp, bias=kbias[:, h, kt:kt + 1]),
                                 reads=[nfB, B_kbias], writes=[dTB[di]])
                        P.op("dve", TT_(pp[:, c0:c1], s_t[:, c0:c1], dT[di][:, c0:c1], ALU.mult),
                             reads=[s_B, dTB[di]], writes=[ppB])
                    tch = touched.setdefault(si, [False] * 4)
                    first = not tch[c0 // 128]
                    for qi in range(c0 // 128, c1 // 128):
                        tch[qi] = True
                    P.op("pe", MM(po[:, c0:c1], vv[:, kt, :], pp[:, c0:c1], first, diag),
                         reads=[vvB, ppB], writes=[poB])
                    if kind == "ml":
                        P.op("pe", MM(p_z[si % 2][:, c0:c1], ones_b, pp[:, c0:c1], first, diag),
                             reads=[B_const, ppB], writes=[p_zB[si % 2]])
                    elif kind == "da":
                        za, zaB = zacc[si % 2], zaccB[si % 2]
                        if first:
                            P.op("dve", CP(za[:, c0:c1], pp[:, c0:c1]), reads=[ppB], writes=[zaB])
                        else:
                            P.op("dve", TT_(za[:, c0:c1], za[:, c0:c1], pp[:, c0:c1], ALU.add), reads=[ppB, zaB], writes=[zaB])

                def finalize(si):
                    kind, h, m, jq, kk, kkB, vv, vvB, qq, qqB = step_ctx(si)
                    t0 = jq * TT
                    tsl = slice(t0, t0 + TT)
                    po, poB = p_o[si % 2], p_oB[si % 2]
                    if kind == "fx":
                        yi = cnt["y"] % 2
                        cnt["y"] += 1
                        P.op("dve", RCP(tA[0:64, :], po[64:128, :]), reads=[poB], writes=[tAB])
                        P.op("dve", TT_(ysg[yi][0:64, :], po[0:64, :], tA[0:64, :], ALU.mult),
                             reads=[poB, tAB], writes=[ysgB[yi]])
                        P.dma("sp", DMA(yT_s[h * 64:(h + 1) * 64, tsl], ysg[yi][0:64, :]), reads=[ysgB[yi]])
                        return
                    pz, pzB = p_z[si % 2], p_zB[si % 2]
                    if kind == "da":
                        P.op("pe", MM(pz[:], ones_f, zacc[si % 2][:], True, True), reads=[B_const, zaccB[si % 2]], writes=[pzB])
                        P.op("dve", RCP(tA[:], pz[:]), reads=[pzB], writes=[tAB])
                        if m == 0:
                            P.op("dve", TT_(r0[:], po[:], tA[:], ALU.mult), reads=[poB, tAB], writes=[r0B])
                        else:
                            yi = cnt["y"] % 2
                            cnt["y"] += 1
                            P.op("dve", TT_(tB_[:], po[:], tA[:], ALU.mult), reads=[poB, tAB], writes=[tBB])
                            P.op("dve", STT(tB_[:], tB_[:], cols[:, NLAM:NLAM + 1], r0[:], ALU.mult, ALU.add),
                                 reads=[tBB, B_cols, r0B], writes=[tBB])
                            P.op("act", ACT(sqb[:], tB_[:], AF.Square), reads=[tBB], writes=[sqbB])
                            P.op("pe", MM(pz[:], ones_b, sqb[:], True, True), reads=[sqbB, B_const], writes=[pzB])
                            P.op("act", ACT(tA[:], pz[:], AF.Ln, bias=EPS, scale=1.0 / 128), reads=[pzB], writes=[tAB])
                            P.op("act", ACT(tA[:], tA[:], AF.Exp, scale=-0.5), reads=[tAB], writes=[tAB])
                            P.op("dve", STT(tB_[:], tB_[:], cols[:, SUBG:SUBG + 1], tA[:], ALU.mult, ALU.mult),
                                 reads=[tBB, B_cols, tAB], writes=[tBB])
                            P.op("act", ACT(ysg[yi][:], tB_[:], AF.Copy, scale=1.0 - lambda_init(l)),
                                 reads=[tBB], writes=[ysgB[yi]])
                            P.dma("sp", DMA(yT_s[h * 128:(h + 1) * 128, tsl], ysg[yi][:]), reads=[ysgB[yi]])
                    else:
                        yi = cnt["y"] % 2
                        cnt["y"] += 1
                        ogt, ogtB = og[si % 2], ogB[si % 2]
                        P.op("act", ACT(tA[:], pz[:], AF.Abs), reads=[pzB], writes=[tAB])
                        P.op("dve", TS(tA[:], tA[:], 1.0, None, ALU.max), reads=[tAB], writes=[tAB])
                        P.op("dve", RCP(tA[:], tA[:]), reads=[tAB], writes=[tAB])
                        P.op("dve", TT_(tB_[:], po[:], tA[:], ALU.mult), reads=[poB, tAB], writes=[tBB])
                        P.op("dve", TT_(ysg[yi][:], tB_[:], ogt[:], ALU.mult), reads=[tBB, ogtB], writes=[ysgB[yi]])
                        P.dma("sp", DMA(yT_s[512 + h * 128:512 + (h + 1) * 128, tsl], ysg[yi][:]), reads=[ysgB[yi]])

                flat = [(si, bi) for si in range(len(steps)) for bi in range(len(sblocks[si]))]
                wpipe = PiecePipe(wcast_pieces(l, wcf, wcfB, wcb, wcbB, ("pool",)), max(1, NWC - 2))
                wper = -(-len(wpipe) // len(steps))
                pend = []
                nxt = 0
                for idx, (si, bi) in enumerate(flat):
                    if bi == 0:
                        ensure_loaded(si)
                        if steps[si][1] == "ml":
                            h_ = steps[si][2]
                            t0_ = steps[si][4] * TT
                            pzz, pzzB = p_z[si % 2], p_zB[si % 2]
                            P.op("pe", MM(pzz[:], selb[:, h_ * 128:(h_ + 1) * 128], fsp[:, t0_:t0_ + TT], True, True),
                                 reads=[B_const, fspB], writes=[pzzB])
                            P.op("act", ACT(negF[si % 2][:], pzz[:], AF.Copy), reads=[pzzB], writes=[negFB[si % 2]])
                        ii = steps[si][0]
                        if ii + 1 < len(items) and (ii + 1) not in loaded_items:
                            kb0, vb0 = item_bufs(ii)
                            kb1, vb1 = item_bufs(ii + 1)
                            if not (set(kb0) & set(kb1)) and vb0 != vb1:
                                load_item(ii + 1)
                                loaded_items.add(ii + 1)
                        if si + 1 < len(steps) and steps[si + 1][0] in loaded_items:
                            ensure_loaded(si + 1)
                        wpipe.step(wper)
                    la = NSB - 1
                    while nxt < len(flat) and nxt <= idx + la:
                        nsi, nbi = flat[nxt]
                        ensure_loaded(nsi)
                        pend.append(issue_scores(nsi, nbi))
                        nxt += 1
                    finish_block(si, bi, pend.pop(0))
                    if bi == len(sblocks[si]) - 1:
                        finalize(si)
                wpipe.drain()
                P.flush()

        def phase_out(l):
            even = (l % 2 == 0)
            j = l // 2
            w_d = (ab_w_out_d if even else fx_w_out_d)[j].rearrange("(kc p) n -> p kc n", p=128)
            with contextlib.ExitStack() as ph:
                wo = sb("ou_wo", [128, KC, D], BF16, ph)
                woB = Buf()
                yt = [sb("ou_yt%d" % i, [128, KC, TT], BF16, ph) for i in range(2)]
                ytB = [Buf(), Buf()]
                pp = [ps("ou_p%d" % i, ph) for i in range(2)]
                ppB = [Buf(), Buf()]
                P.dma("sp", DMA(wo[:].rearrange("p kc n -> p (kc n)"), woutS.rearrange("p kc n -> p (kc n)")), writes=[woB])
                yv = yT_s.rearrange("(c p) t -> p c t", p=128)
                n = 0
                P.dma("sp", DMA(yt[0][:], yv[:, :, 0:TT]), writes=[ytB[0]])
                for tt in range(NTT):
                    if tt + 1 < NTT:
                        P.dma("sp", DMA(yt[(tt + 1) % 2][:], yv[:, :, (tt + 1) * TT:(tt + 2) * TT]), writes=[ytB[(tt + 1) % 2]])
                    y, yB = yt[tt % 2], ytB[tt % 2]
                    for co in range(KC):
                        p_, pB = pp[n % 2], ppB[n % 2]
                        n += 1
                        for kc in range(KC):
                            P.op("pe", MM(p_[:], wo[:, kc, co * 128:(co + 1) * 128], y[:, kc, :], kc == 0, kc == KC - 1),
                                 reads=[woB, yB], writes=[pB])
                        xs = X[:, co, tt * TT:(tt + 1) * TT]
                        P.op("dve", STT(xs, p_[:], cols[:, GM + co:GM + co + 1], xs, ALU.mult, ALU.add),
                             reads=[pB, B_cols, XB[co][tt]], writes=[XB[co][tt]])
                P.flush()

        def wcast_pieces(l, stf, stfB, stb, stbB, engs):
            w1v = w1_d[l].rearrange("(kc p) n -> p kc n", p=128)
            w3v = w3_d[l].rearrange("(kc p) n -> p kc n", p=128)
            w2v = w2_d[l].rearrange("(f p) n -> p f n", p=128)
            NS = len(stf)
            pieces = []
            ctr = [0]

            def mk13(wi, wv_, kc, c0, ncol):
                slot = [0]

                def fa():
                    k = ctr[0] % NS
                    eng = engs[ctr[0] % len(engs)]
                    ctr[0] += 1
                    slot[0] = k
                    P.dma("sp", DMA(stf[k][:, 0:ncol], wv_[:, kc, c0:c0 + ncol]), writes=[stfB[k]])
                    if eng == "act":
                        P.op("act", ACT(stb[k][:, 0:ncol], stf[k][:, 0:ncol], AF.Copy), reads=[stfB[k]], writes=[stbB[k]])
                    else:
                        P.op(eng, CP(stb[k][:, 0:ncol], stf[k][:, 0:ncol]), reads=[stfB[k]], writes=[stbB[k]])

                def fb():
                    k = slot[0]
                    nfc = ncol // 128
                    f0 = c0 // 128
                    P.dma("sp", DMA(w13s[f0:f0 + nfc, :, kc, wi, :].rearrange("f p n -> p f n"),
                                    stb[k][:, 0:ncol].rearrange("p (f n) -> p f n", n=128)), reads=[stbB[k]])
                return (fa, fb)

            def mk2(f_, c0):
                slot = [0]

                def fa():
                    k = ctr[0] % NS
                    eng = engs[ctr[0] % len(engs)]
                    ctr[0] += 1
                    slot[0] = k
                    P.dma("sp", DMA(stf[k][:, 0:512], w2v[:, f_, c0:c0 + 512]), writes=[stfB[k]])
                    if eng == "act":
                        P.op("act", ACT(stb[k][:, 0:512], stf[k][:, 0:512], AF.Copy), reads=[stfB[k]], writes=[stbB[k]])
                    else:
                        P.op(eng, CP(stb[k][:, 0:512], stf[k][:, 0:512]), reads=[stfB[k]], writes=[stbB[k]])

                def fb():
                    k = slot[0]
                    co0 = c0 // 128
                    P.dma("sp", DMA(w2s[co0:co0 + 4, :, f_, :].rearrange("c p n -> p c n"),
                                    stb[k][:, 0:512].rearrange("p (c n) -> p c n", n=128)), reads=[stbB[k]])
                return (fa, fb)

            for wi, wv_ in enumerate((w1v, w3v)):
                for kc in range(KC):
                    for c0 in range(0, FFN, 512):
                        pieces.append(mk13(wi, wv_, kc, c0, min(512, FFN - c0)))
            for f_ in range(NF):
                for c0 in (0, 512):
                    pieces.append(mk2(f_, c0))
            return pieces

        def phase_ffn(l, next_l=None):
            w1v = w1_d[l].rearrange("(kc p) n -> p kc n", p=128)
            w3v = w3_d[l].rearrange("(kc p) n -> p kc n", p=128)
            w2v = w2_d[l].rearrange("(f p) n -> p f n", p=128)
            with contextlib.ExitStack() as ph:
                hT = sb("ff_hT", [128, KC, TT], BF16, ph)
                hTB = Buf()
                g = sb("ff_g", [128, NF, TT], BF16, ph)
                gB = [Buf() for _ in range(NF)]
                sqs = [sb("ff_sq%d" % i, [128, TT], BF16, ph) for i in range(2)]
                sqB = [Buf(), Buf()]
                rstd = sb("ff_rstd", [128, TT], F32, ph)
                rstdB = Buf()
                tmpf = [sb("ff_tmpf0", [128, TT], F32, ph)]
                tmpfB = [Buf()]
                sT = [sb("ff_sT0", [128, TT], BF16, ph)]
                sTB = [Buf()]
                w13 = [sb("ff_w13_%d" % i, [128, KC, 256], BF16, ph) for i in range(3)]
                w13B = [Buf() for _ in range(3)]
                w2t = [sb("ff_w2_%d" % i, [128, NF, 128], BF16, ph) for i in range(2)]
                w2B = [Buf() for _ in range(2)]
                pss = ps("ff_pss", ph)
                pssB = Buf()
                pu1 = [ps("ff_pu1_%d" % i, ph) for i in range(2)]
                pu1B = [Buf(), Buf()]
                pu3 = [ps("ff_pu3_%d" % i, ph) for i in range(2)]
                pu3B = [Buf(), Buf()]
                py = [ps("ff_py%d" % i, ph) for i in range(2)]
                pyB = [Buf(), Buf()]
                st = [0]
                nw2 = 0
                ny = 0
                pcs = None
                if next_l is not None:
                    pcf = [sb("ff_pcf%d" % i, [128, 512], F32, ph) for i in range(3)]
                    pcfB = [Buf() for _ in range(3)]
                    pcb = [sb("ff_pcb%d" % i, [128, 512], BF16, ph) for i in range(3)]
                    pcbB = [Buf() for _ in range(3)]
                    pcs = PiecePipe(precast_pieces(next_l, pcf, pcfB, pcb, pcbB, ("pool",)), 1)
                pc_per = -(-len(pcs) // (NTT * (NF + KC))) if pcs is not None else 0

                def do_pc():
                    if pcs is not None:
                        pcs.step(pc_per)

                for tt in range(NTT):
                    tsl = slice(tt * TT, (tt + 1) * TT)
                    emit_rstd(ph, tt, rstd[:], sqs, sqB, pss, pssB, rstdB)
                    emit_hT(tt, hT, hTB, rstd[:], rstdB, tmpf, tmpfB, GSF, SHF)

                    def load13(f):
                        k = f % 3
                        P.dma("sp", DMA(w13[k][:].rearrange("p kc n -> p (kc n)"),
                                        w13s[f].rearrange("p kc w n -> p (kc w n)")), writes=[w13B[k]])

                    load13(0)
                    load13(1)
                    for f in range(NF):
                        if f + 2 < NF:
                            load13(f + 2)
                        do_pc()
                        k = f % 2
                        kw = f % 3
                        for kc in range(KC):
                            P.op("pe", MM(pu1[k][:], w13[kw][:, kc, 0:128], hT[:, kc, :], kc == 0, kc == KC - 1),
                                 reads=[w13B[kw], hTB], writes=[pu1B[k]])
                        for kc in range(KC):
                            P.op("pe", MM(pu3[k][:], w13[kw][:, kc, 128:256], hT[:, kc, :], kc == 0, kc == KC - 1),
                                 reads=[w13B[kw], hTB], writes=[pu3B[k]])
                        P.op("act", ACT(sT[0][:], pu1[k][:], AF.Silu), reads=[pu1B[k]], writes=[sTB[0]])
                        P.op("dve", TT_(g[:, f, :], pu3[k][:], sT[0][:], ALU.mult), reads=[pu3B[k], sTB[0]], writes=[gB[f]])

                    def load2(co):
                        nonlocal nw2
                        k = nw2 % 2
                        nw2 += 1
                        P.dma("sp", DMA(w2t[k][:].rearrange("p f n -> p (f n)"),
                                        w2s[co].rearrange("p f n -> p (f n)")), writes=[w2B[k]])
                        return k

                    nxt = load2(0)
                    for co in range(KC):
                        cur = nxt
                        if co + 1 < KC:
                            nxt = load2(co + 1)
                        do_pc()
                        p_, pB = py[ny % 2], pyB[ny % 2]
                        ny += 1
                        for f in range(NF):
                            wk = cur
                            P.op("pe", MM(p_[:], w2t[wk][:, f, :], g[:, f, :], f == 0, f == NF - 1),
                                 reads=[w2B[wk], gB[f]], writes=[pB])
                        xs = X[:, co, tsl]
                        P.op("dve", STT(xs, p_[:], cols[:, GF + co:GF + co + 1], xs, ALU.mult, ALU.add),
                             reads=[pB, B_cols, XB[co][tt]], writes=[XB[co][tt]])
                if pcs is not None:
                    pcs.drain()
                P.flush()

        phase_precast(layers[0])
        for li_, l in enumerate(layers):
            next_l = layers[li_ + 1] if li_ + 1 < len(layers) else None
            phase_ada(l)
            with contextlib.ExitStack() as phg:
                graw = sb("graw", [128, 512], F32, phg)
                phase_mixin(l)
                phase_gates(l)
            phase_attn(l)
            phase_out(l)
            phase_ffn(l, next_l)

        for c in range(KC):
            P.dma("sp", DMA(outT_d[c * 128:(c + 1) * 128, :], X[:, c, :]), reads=[XB[c][t] for t in range(NTT)])
        P.flush(final=True)
        print("ops", P.total_ops, "waits", P.total_waits, flush=True)
    return nc


def _consts():
    ident = np.eye(128, dtype=np.float32)
    ones = np.ones((128, 128), np.float32)
    bones = np.zeros((128, 128), np.float32)
    bones[:64, :64] = 1.0
    bones[64:, 64:] = 1.0
    s = np.arange(128)[:, None]
    t = np.arange(128)[None, :]
    cm8 = np.where(s <= t, 0.0, NEG * 8).astype(np.float32)
    slopes = 2.0 ** (-8.0 * np.arange(1, 5) / 4)
    dabs = []
    for h in range(4):
        allowed = (s // 64) <= (t // 64)
        dabs.append(np.where(allowed, -slopes[h] * np.abs(t - s) * 8.0, NEG * 8).astype(np.float32))
    U = (s <= t).astype(np.float32)
    cstf = np.concatenate([ident, ones, U], axis=1).astype(np.float32)
    cstb = np.concatenate([ident, ones, bones, cm8] + dabs, axis=1).astype(np.float32)
    datab = np.zeros((128, 4 * 35), np.float32)
    p = np.arange(128)
    for h in range(4):
        for dd in range(35):
            d = dd - 3
            datab[:, h * 35 + dd] = -slopes[h] * 128.0 * d + slopes[h] * p
    sel = np.zeros((68, 4 * 128), np.float32)
    for h in range(4):
        for r in (0, 32, 64):
            sel[r + h, h * 128:(h + 1) * 128] = 1.0
    import ml_dtypes
    tm = (np.arange(S) % TT).astype(np.float32)
    daq = np.zeros((16, S), np.float32)
    for h in range(4):
        v = (-slopes[h] * tm * 8.0).astype(np.float32)
        hi = v.astype(ml_dtypes.bfloat16).astype(np.float32)
        lo = v - hi
        for m in range(2):
            daq[2 * h + m] = hi
            daq[8 + 2 * h + m] = lo
    return cstf, cstb, datab, sel, daq


def _host_inputs(inp):
    f = lambda a: np.ascontiguousarray(np.asarray(a, dtype=np.float32))
    cstf, cstb, datab, sel, daq = _consts()
    col = lambda v: f(np.asarray(v).reshape(-1, 128).T)
    shared = {
        "ada_w": f(inp["ada_w"]),
        "ada_bT": f(np.stack([col(inp["ada_b"][l]) for l in range(DEPTH)])),
        "gmixT": f(np.stack([col(inp["norm_mix_g"][l]) for l in range(DEPTH)])),
        "gffnT": f(np.stack([col(inp["norm_ffn_g"][l]) for l in range(DEPTH)])),
        "ab_w_in": f(inp["ab_w_in"]), "ab_w_out": f(inp["ab_w_out"]),
        "fx_w_in": f(inp["fx_w_in"]), "fx_w_out": f(inp["fx_w_out"]),
        "ffn_w1": f(inp["ffn_w1"]), "ffn_w3": f(inp["ffn_w3"]), "ffn_w2": f(inp["ffn_w2"]),
        "cstf": cstf, "cstb": cstb, "datab": datab, "sel": sel, "daqaug": daq,
    }
    cw = np.asarray(inp["ml_conv_w"])
    shared["convwT"] = f(np.stack([cw[j].reshape(4, 8, 128).transpose(2, 1, 0) for j in range(2)]))
    shared["convbT"] = f(np.stack([col(inp["ml_conv_b"][j]) for j in range(2)]))
    gbe = np.stack([np.tile(np.concatenate([np.asarray(inp["ml_b_i"][j]), np.asarray(inp["ml_b_f"][j])])[None, :], (128, 32))
                    for j in range(2)])
    shared["gb_even"] = f(gbe)
    gbo = np.stack([np.tile(np.asarray(inp["fx_b_f"][j])[None, :], (128, 32)) for j in range(2)])
    shared["gb_odd"] = f(gbo)
    qkg = np.zeros((DEPTH, 128, 2), np.float32)
    for l in range(DEPTH):
        j = l // 2
        if l % 2 == 0:
            qkg[l, :, 0] = np.tile(np.asarray(inp["da_q_g"][j]), 2)
            qkg[l, :, 1] = np.tile(np.asarray(inp["da_k_g"][j]), 2)
        else:
            qkg[l, :, 0] = np.tile(np.asarray(inp["fx_q_g"][j]), 2)
            qkg[l, :, 1] = np.tile(np.asarray(inp["fx_k_g"][j]), 2)
    shared["qkg"] = qkg
    shared["subg"] = f(np.stack([np.asarray(inp["da_subln_g"][j]).reshape(128, 1) for j in range(2)]))
    shared["lpT"] = f(np.stack([np.asarray(inp["da_lambda"][j]).T for j in range(2)]))
    x = np.asarray(inp["x"], dtype=np.float32)
    c = np.asarray(inp["c"], dtype=np.float32)
    maps = []
    for b in range(x.shape[0]):
        m = dict(shared)
        m["xT"] = np.ascontiguousarray(x[b].T)
        m["cT"] = np.ascontiguousarray(c[b].reshape(KC, 128).T)
        maps.append(m)
    return maps


_NC_CACHE = {}


def kernel(**inputs):
    maps = _host_inputs(inputs)
    key = "full"
    if key not in _NC_CACHE:
        _NC_CACHE[key] = build(list(range(DEPTH)))
    nc = _NC_CACHE[key]
    res = run_bass_kernel_spmd(nc, maps, core_ids=list(range(8)))
    out = np.stack([np.ascontiguousarray(r["outT"].T) for r in res.results], axis=0)
    return out.astype(np.float32)
```

```python
import contextlib
import math
import numpy as np
import concourse.bass as bass
import concourse.mybir as mybir
from concourse.bass_utils import run_bass_kernel_spmd

F32 = mybir.dt.float32
BF16 = mybir.dt.bfloat16
ALU = mybir.AluOpType
AF = mybir.ActivationFunctionType

S = 4096
D = 1024
DEPTH = 4
TT = 512
NTT = S // TT
KC = 8
FFN = 2816
NF = FFN // 128
AB_IN = 3592
FX_IN = 3088
EPS = 1e-6
NEG = -30000.0

ENGS = ("pe", "act", "dve", "pool", "sp")
NDMA_SEM = 12


class Buf:
    __slots__ = ("name", "w", "r", "rd")

    def __init__(self, name=""):
        self.name = name
        self.w = None
        self.r = {}
        self.rd = []


class Op:
    __slots__ = ("eng", "fn", "deps", "idx", "ms", "cnt", "dsem", "dval", "isdma")

    def __init__(self, eng, fn, isdma):
        self.eng = eng
        self.fn = fn
        self.deps = []
        self.idx = -1
        self.ms = False
        self.cnt = 0
        self.dsem = None
        self.dval = 0
        self.isdma = isdma


class Prog:
    def __init__(self, nc, es):
        self.nc = nc
        self.esem = {e: es.enter_context(nc.semaphore("s_" + e)) for e in ENGS}
        self.dsem = {}
        for e in ("sp", "pool", "act"):
            for k in range(NDMA_SEM):
                self.dsem[(e, k)] = es.enter_context(nc.semaphore("d_%s%d" % (e, k)))
        self.base = {e: 0 for e in ENGS}
        self.dma_rr = {e: 0 for e in ENGS}
        self.dma_val = {}
        self.dma_last = {}
        self.streams = {e: [] for e in ENGS}
        self.touched = set()
        self.barrier = []
        self.total_ops = 0
        self.total_waits = 0

    def _add(self, eng, fn, reads, writes, isdma):
        op = Op(eng, fn, isdma)
        deps = op.deps
        for b in reads:
            if b.w is not None:
                deps.append(b.w)
        for b in writes:
            if b.w is not None:
                deps.append(b.w)
            deps.extend(b.r.values())
            deps.extend(b.rd)
        for b in reads:
            if isdma:
                b.rd.append(op)
            else:
                b.r[eng] = op
            self.touched.add(b)
        for b in writes:
            b.w = op
            b.r = {}
            b.rd = []
            self.touched.add(b)
        op.idx = len(self.streams[eng])
        self.streams[eng].append(op)
        return op

    def op(self, eng, fn, reads=(), writes=()):
        return self._add(eng, fn, reads, writes, False)

    def dma(self, eng, fn, reads=(), writes=()):
        op = self._add(eng, fn, reads, writes, True)
        k = self.dma_rr[eng]
        self.dma_rr[eng] = (k + 1) % NDMA_SEM
        key = (eng, k)
        prev = self.dma_last.get(key)
        if prev is not None:
            op.deps.append(prev)
        v = self.dma_val.get(key, 0) + 16
        self.dma_val[key] = v
        self.dma_last[key] = op
        op.dsem = key
        op.dval = v
        return op

    def flush(self, final=False):
        nc = self.nc
        waits = {e: [] for e in ENGS}
        for e in ENGS:
            maxidx = {}
            dwaited = {}
            for op in self.streams[e]:
                best = {}
                for d in op.deps:
                    if d.isdma:
                        if dwaited.get(d.dsem, 0) < d.dval:
                            cur = best.get(("d", d.dsem))
                            if cur is None or cur.dval < d.dval:
                                best[("d", d.dsem)] = d
                    else:
                        if d.eng == e and e == "pe":
                            continue
                        if maxidx.get(d.eng, -1) < d.idx:
                            cur = best.get(("e", d.eng))
                            if cur is None or cur.idx < d.idx:
                                best[("e", d.eng)] = d
                wl = []
                for key, d in best.items():
                    if key[0] == "d":
                        dwaited[d.dsem] = d.dval
                    else:
                        maxidx[d.eng] = d.idx
                        d.ms = True
                    wl.append(d)
                waits[e].append(wl)
        for e in ENGS:
            if self.streams[e]:
                last = self.streams[e][-1]
                if not last.isdma:
                    last.ms = True
        for e in ENGS:
            c = self.base[e]
            for op in self.streams[e]:
                if op.ms:
                    c += 1
                    op.cnt = c
            self.base[e] = c
        esem, dsem = self.esem, self.dsem
        barrier = self.barrier
        new_barrier = [("e", e, self.base[e]) for e in ENGS if self.base[e] > 0]
        new_barrier += [("d", key, v) for key, v in self.dma_val.items()]

        def emit(e, engobj):
            first = True
            for op, wl in zip(self.streams[e], waits[e]):
                if first:
                    first = False
                    for kind, key, v in barrier:
                        if kind == "e":
                            if key != e:
                                engobj.wait_ge(esem[key], v)
                        else:
                            engobj.wait_ge(dsem[key], v)
                for d in wl:
                    if d.isdma:
                        engobj.wait_ge(dsem[d.dsem], d.dval)
                    else:
                        engobj.wait_ge(esem[d.eng], d.cnt)
                ins = op.fn(engobj)
                if op.isdma:
                    ins.then_inc(dsem[op.dsem], 16)
                elif op.ms:
                    ins.then_inc(esem[e], 1)
            if final and e == "sp":
                for kind, key, v in new_barrier:
                    if kind == "e":
                        if key != e:
                            engobj.wait_ge(esem[key], v)
                    else:
                        engobj.wait_ge(dsem[key], v)

        with nc.Block() as block:
            @block.tensor
            def _(eng):
                emit("pe", eng)

            @block.scalar
            def _(eng):
                emit("act", eng)

            @block.vector
            def _(eng):
                emit("dve", eng)

            @block.gpsimd
            def _(eng):
                emit("pool", eng)

            @block.sync
            def _(eng):
                emit("sp", eng)

        for e in ENGS:
            self.total_ops += len(self.streams[e])
            self.total_waits += sum(len(w) for w in waits[e])
        self.barrier = new_barrier
        self.streams = {e: [] for e in ENGS}
        self.dma_last = {}
        for b in self.touched:
            b.w = None
            b.r = {}
            b.rd = []
        self.touched = set()


def MM(out, lhsT, rhs, start, stop):
    return lambda e: e.matmul(out, lhsT=lhsT, rhs=rhs, start=start, stop=stop, skip_group_check=True)


def ACT(out, in_, func, bias=None, scale=None):
    kw = {}
    if bias is not None:
        kw["bias"] = bias
    if scale is not None:
        kw["scale"] = scale
    return lambda e: e.activation(out=out, in_=in_, func=func, **kw)


def TT_(out, in0, in1, op):
    return lambda e: e.tensor_tensor(out=out, in0=in0, in1=in1, op=op)


def TS(out, in0, s1, s2, op0, op1=None):
    if op1 is None:
        return lambda e: e.tensor_scalar(out=out, in0=in0, scalar1=s1, scalar2=None, op0=op0)
    return lambda e: e.tensor_scalar(out=out, in0=in0, scalar1=s1, scalar2=s2, op0=op0, op1=op1)


def STT(out, in0, scalar, in1, op0, op1):
    return lambda e: e.scalar_tensor_tensor(out=out, in0=in0, scalar=scalar, in1=in1, op0=op0, op1=op1)


def CP(out, in_):
    return lambda e: e.tensor_copy(out=out, in_=in_)


def MS(ap, v):
    return lambda e: e.memset(ap, v)


def RCP(out, in_):
    return lambda e: e.reciprocal(out=out, in_=in_)


def DMA(out, in_):
    return lambda e: e.dma_start(out=out, in_=in_)


def SCAN(out, d0, d1, init, op0, op1):
    return lambda e: e.tensor_tensor_scan(out=out, data0=d0, data1=d1, initial=init, op0=op0, op1=op1)


def lambda_init(l):
    return 0.8 - 0.6 * math.exp(-0.3 * l)


def build(layers, x_in_name="xT", dbg=False):
    nc = bass.Bass("TRN2", target_bir_lowering=False)

    def din(name, shape):
        return nc.dram_tensor(name, list(shape), F32, kind="ExternalInput").ap()

    xT_d = din("xT", [D, S])
    cT_d = din("cT", [128, KC])
    ada_w_d = din("ada_w", [DEPTH, D, 6 * D])
    ada_bT_d = din("ada_bT", [DEPTH, 128, 48])
    gmixT_d = din("gmixT", [DEPTH, 128, KC])
    gffnT_d = din("gffnT", [DEPTH, 128, KC])
    ab_w_in_d = din("ab_w_in", [2, D, AB_IN])
    ab_w_out_d = din("ab_w_out", [2, D, D])
    fx_w_in_d = din("fx_w_in", [2, D, FX_IN])
    fx_w_out_d = din("fx_w_out", [2, D, D])
    w1_d = din("ffn_w1", [DEPTH, D, FFN])
    w3_d = din("ffn_w3", [DEPTH, D, FFN])
    w2_d = din("ffn_w2", [DEPTH, FFN, D])
    convw_d = din("convwT", [2, 128, 8, 4])
    convb_d = din("convbT", [2, 128, 8])
    gbe_d = din("gb_even", [2, 128, 256])
    gbo_d = din("gb_odd", [2, 128, 512])
    qkg_d = din("qkg", [DEPTH, 128, 2])
    subg_d = din("subg", [2, 128, 1])
    lpT_d = din("lpT", [2, 64, 4])
    cstf_d = din("cstf", [128, 3 * 128])
    cstb_d = din("cstb", [128, 8 * 128])
    datab_d = din("datab", [128, 4 * 35])
    sel_d = din("sel", [68, 4 * 128])
    daq_d = din("daqaug", [16, S])
    outT_d = nc.dram_tensor("outT", [D, S], F32, kind="ExternalOutput").ap()

    def dscr(name, shape, dt=BF16):
        kind = "ExternalOutput" if dbg else "Internal"
        return nc.dram_tensor(name, list(shape), dt, kind=kind).ap()

    qT_o = dscr("qT_o", [16, 68, S])
    kT_o = dscr("kT_o", [16, 68, S])
    V_o = dscr("V_o", [S, 16, 128])
    qT_da = dscr("qT_da", [8, 66, S])
    kT_da = dscr("kT_da", [8, 66, S])
    qT_ml = dscr("qT_ml", [4, 128, S])
    kT_ml = dscr("kT_ml", [4, 128, S])
    V_e = dscr("V_e", [S, 8, 128])
    osig = dscr("osig", [4, 128, S])
    yT_s = dscr("yT_s", [D, S])
    adaS = nc.dram_tensor("adaS", [12, 128, KC, 512], BF16, kind="Internal").ap()
    winA = nc.dram_tensor("winA", [20, 128, KC, 128], BF16, kind="Internal").ap()
    winB = nc.dram_tensor("winB", [2, 128, KC, 512], BF16, kind="Internal").ap()
    winG = nc.dram_tensor("winG", [128, KC, 16], BF16, kind="Internal").ap()
    woutS = nc.dram_tensor("woutS", [128, KC, D], BF16, kind="Internal").ap()
    w13s = nc.dram_tensor("w13s", [NF, 128, KC, 2, 128], BF16, kind="Internal").ap()
    w2s = nc.dram_tensor("w2s", [KC, 128, NF, 128], BF16, kind="Internal").ap()

    with contextlib.ExitStack() as es:
        P = Prog(nc, es)

        uid = [0]

        def sb(name, shape, dt, st=es):
            uid[0] += 1
            return st.enter_context(nc.sbuf_tensor("%s_%d" % (name, uid[0]), list(shape), dt))

        def ps(name, st, shape=(128, 512), dt=F32):
            uid[0] += 1
            return st.enter_context(nc.psum_tensor("%s_%d" % (name, uid[0]), list(shape), dt))

        X = sb("X", [128, KC, S], F32)
        XB = [[Buf("X%d_%d" % (c, t)) for t in range(NTT)] for c in range(KC)]
        cst = sb("cst_sb", [128, 3 * 128], F32)
        cstb = sb("cstb_sb", [128, 8 * 128], BF16)
        ident_b = cstb[:, 0:128]
        ones_b = cstb[:, 128:256]
        bones_b = cstb[:, 256:384]
        cm8_b = cstb[:, 384:512]
        dab8_b = [cstb[:, 512 + 128 * h: 640 + 128 * h] for h in range(4)]
        ident_f = cst[:, 0:128]
        ones_f = cst[:, 128:256]
        U_f = cst[:, 256:384]
        datab = sb("datab_sb", [128, 4 * 35], F32)
        selb = sb("selb", [68, 4 * 128], BF16)
        modT = sb("modT", [128, 48], F32)
        cols = sb("cols", [128, 64], F32)
        B_const = Buf("const")
        B_mod = Buf("mod")
        B_cols = Buf("cols")
        GSM, SHM, GM, GSF, SHF, GF = 0, 8, 16, 24, 32, 40
        QG, KG, SUBG, NLAM = 48, 49, 50, 51
        kbias = sb("kbias", [128, 16, 32], F32)
        B_kbias = Buf("kbias")
        graw = None
        grawB = Buf("graw")

        with contextlib.ExitStack() as ph:
            stage = sb("su_stage", [128, 8 * 128], F32, ph)
            stage2 = sb("su_stage2", [68, 4 * 128], F32, ph)
            Bs2 = Buf()
            rowsf = sb("su_rowsf", [16, S], F32, ph)
            rowsb = sb("su_rowsb", [16, S], BF16, ph)
            onesr = sb("su_onesr", [16, S], BF16, ph)
            Bs, Brf, Brb, Bor = Buf(), Buf(), Buf(), Buf()
            P.dma("sp", DMA(cst[:], cstf_d), writes=[B_const])
            P.dma("sp", DMA(stage[:], cstb_d), writes=[Bs])
            P.op("dve", CP(cstb[:], stage[:]), reads=[Bs], writes=[B_const])
            P.dma("sp", DMA(datab[:], datab_d), writes=[B_const])
            P.dma("sp", DMA(stage2[:], sel_d), writes=[Bs2])
            P.op("dve", CP(selb[:], stage2[:]), reads=[Bs2], writes=[B_const])
            for c in range(KC):
                P.dma("sp", DMA(X[:, c, :], xT_d[c * 128:(c + 1) * 128, :]),
                      writes=[XB[c][t] for t in range(NTT)])
            P.op("pool", MS(onesr[:], 1.0), writes=[Bor])
            P.dma("sp", DMA(kT_o[:, 64, :], onesr[:]), reads=[Bor])
            for r_ in (65, 66, 67):
                P.dma("sp", DMA(qT_o[:, r_, :], onesr[:]), reads=[Bor])
            P.dma("sp", DMA(kT_da[:, 64, :], onesr[0:8, :]), reads=[Bor])
            P.dma("sp", DMA(kT_da[:, 65, :], onesr[0:8, :]), reads=[Bor])
            P.dma("sp", DMA(rowsf[:], daq_d), writes=[Brf])
            P.op("dve", CP(rowsb[:], rowsf[:]), reads=[Brf], writes=[Brb])
            P.dma("sp", DMA(qT_da[:, 64, :], rowsb[0:8, :]), reads=[Brb])
            P.dma("sp", DMA(qT_da[:, 65, :], rowsb[8:16, :]), reads=[Brb])
            P.flush()

        def load_w_cast(ph_stage, stage_bufs, dst_ap, src_ap, dst_buf, n_free, state, engs=("pool",)):
            k = state[0] % len(stage_bufs)
            eng = engs[state[0] % len(engs)]
            state[0] += 1
            st_tile, st_buf = ph_stage[k], stage_bufs[k]
            P.dma("sp", DMA(st_tile[:, 0:n_free], src_ap), writes=[st_buf])
            if eng == "act":
                P.op("act", ACT(dst_ap, st_tile[:, 0:n_free], AF.Copy), reads=[st_buf], writes=[dst_buf])
            else:
                P.op(eng, CP(dst_ap, st_tile[:, 0:n_free]), reads=[st_buf], writes=[dst_buf])

        class PiecePipe:
            def __init__(self, pieces, ahead):
                self.p = pieces
                self.ia = 0
                self.ib = 0
                self.ahead = ahead

            def step(self, n=1):
                for _ in range(n):
                    while self.ia < len(self.p) and self.ia <= self.ib + self.ahead:
                        self.p[self.ia][0]()
                        self.ia += 1
                    if self.ib < len(self.p):
                        self.p[self.ib][1]()
                        self.ib += 1

            def drain(self):
                while self.ib < len(self.p):
                    self.step()

            def __len__(self):
                return len(self.p)

        def fm_groups(l):
            if l % 2 == 0:
                return [(0, 1024), (1536, 1024), (3072, 512)]
            return [(0, 1024), (1024, 1024)]

        def tm_groups(l):
            if l % 2 == 0:
                return [1024, 2560], (3584, 8)
            return [2048, 2560], (3072, 16)

        def precast_pieces(l, stf, stfB, stb, stbB, engs):
            even = (l % 2 == 0)
            j = l // 2
            win = (ab_w_in_d if even else fx_w_in_d)[j].rearrange("(kc p) n -> p kc n", p=128)
            wout = (ab_w_out_d if even else fx_w_out_d)[j].rearrange("(kc p) n -> p kc n", p=128)
            wada = ada_w_d[l].rearrange("(kc p) n -> p kc n", p=128)
            NS = len(stf)
            ctr = [0]
            pieces = []

            def mk(src, ncol, dst_fn, view=None):
                slot = [0]

                def fa():
                    k = ctr[0] % NS
                    eng = engs[ctr[0] % len(engs)]
                    ctr[0] += 1
                    slot[0] = k
                    P.dma("sp", DMA(stf[k][:, 0:ncol], src), writes=[stfB[k]])
                    if eng == "act":
                        P.op("act", ACT(stb[k][:, 0:ncol], stf[k][:, 0:ncol], AF.Copy), reads=[stfB[k]], writes=[stbB[k]])
                    else:
                        P.op(eng, CP(stb[k][:, 0:ncol], stf[k][:, 0:ncol]), reads=[stfB[k]], writes=[stbB[k]])

                def fb():
                    k = slot[0]
                    srcv = stb[k][:, 0:ncol]
                    if view is not None:
                        srcv = srcv.rearrange(view, n=128)
                    P.dma("sp", DMA(dst_fn(), srcv), reads=[stbB[k]])
                return (fa, fb)

            ch = 0
            for (c0, n) in fm_groups(l):
                for cc0 in range(0, n, 512):
                    for kc in range(KC):
                        def dst(ch_=ch + cc0 // 128, kc_=kc):
                            return winA[ch_:ch_ + 4, :, kc_, :].rearrange("c p n -> p c n")
                        pieces.append(mk(win[:, kc, c0 + cc0:c0 + cc0 + 512], 512, dst, "p (c n) -> p c n"))
                ch += n // 128
            tmc, (g0, ng) = tm_groups(l)
            for gi, c0 in enumerate(tmc):
                for kc in range(KC):
                    pieces.append(mk(win[:, kc, c0:c0 + 512], 512, lambda gi_=gi, kc_=kc: winB[gi_, :, kc_, :]))
            for kc in range(KC):
                pieces.append(mk(win[:, kc, g0:g0 + ng], ng, lambda kc_=kc, ng_=ng: winG[:, kc_, 0:ng_]))
            for kc in range(KC):
                for c0 in (0, 512):
                    pieces.append(mk(wout[:, kc, c0:c0 + 512], 512, lambda kc_=kc, c0_=c0: woutS[:, kc_, c0_:c0_ + 512]))
            for blk in range(12):
                for kc in range(KC):
                    pieces.append(mk(wada[:, kc, blk * 512:(blk + 1) * 512], 512, lambda b_=blk, kc_=kc: adaS[b_, :, kc_, :]))
            return pieces

        def phase_precast(l):
            with contextlib.ExitStack() as ph:
                NS = 10
                stf = [sb("pc_f%d" % i, [128, 512], F32, ph) for i in range(NS)]
                stfB = [Buf() for _ in range(NS)]
                stb = [sb("pc_b%d" % i, [128, 512], BF16, ph) for i in range(NS)]
                stbB = [Buf() for _ in range(NS)]
                pp_ = PiecePipe(precast_pieces(l, stf, stfB, stb, stbB, ("pool", "dve", "act")), NS - 2)
                pp_.drain()
                P.flush()

        def phase_ada(l):
            with contextlib.ExitStack() as ph:
                cT = sb("ad_cT", [128, KC], F32, ph)
                scb = sb("ad_scb", [128, KC], BF16, ph)
                abT = sb("ad_abT", [128, 48], F32, ph)
                gm = sb("ad_gm", [128, 2 * KC], F32, ph)
                wb = [sb("ad_wb%d" % i, [128, KC, 512], BF16, ph) for i in range(3)]
                wbB = [Buf() for _ in range(3)]
                pm = ps("ad_pm", ph, (128, 48))
                BcT, Bsc, Bab, Bgm, Bpm = Buf(), Buf(), Buf(), Buf(), Buf()
                st = [0]
                P.dma("sp", DMA(cT[:], cT_d), writes=[BcT])
                P.dma("sp", DMA(abT[:], ada_bT_d[l]), writes=[Bab])
                P.dma("sp", DMA(gm[:, 0:KC], gmixT_d[l]), writes=[Bgm])
                P.dma("sp", DMA(gm[:, KC:2 * KC], gffnT_d[l]), writes=[Bgm])
                P.dma("sp", DMA(cols[:, QG:QG + 2], qkg_d[l]), writes=[B_cols])
                P.op("act", ACT(scb[:], cT[:], AF.Silu), reads=[BcT], writes=[Bsc])
                wv = ada_w_d[l].rearrange("(kc p) n -> p kc n", p=128)
                def ld_ada(blk):
                    P.dma("sp", DMA(wb[blk % 3][:].rearrange("p kc n -> p (kc n)"),
                                    adaS[blk].rearrange("p kc n -> p (kc n)")), writes=[wbB[blk % 3]])
                ld_ada(0)
                ld_ada(1)
                for blk in range(12):
                    b = blk % 3
                    if blk + 2 < 12:
                        ld_ada(blk + 2)
                    for cc in range(4):
                        j = blk * 4 + cc
                        for kc in range(KC):
                            P.op("pe", MM(pm[:, j:j + 1], wb[b][:, kc, cc * 128:(cc + 1) * 128],
                                          scb[:, kc:kc + 1], kc == 0, kc == KC - 1),
                                 reads=[wbB[b], Bsc], writes=[Bpm])
                P.op("dve", TT_(modT[:], pm[:], abT[:], ALU.add), reads=[Bpm, Bab], writes=[B_mod])
                for (dst, g0, sc0, sh0, ga0) in ((GSM, 0, 8, 0, 16), (GSF, KC, 32, 24, 40)):
                    P.op("dve", STT(cols[:, dst:dst + 8], modT[:, sc0:sc0 + 8], 1.0, gm[:, g0:g0 + 8],
                                    ALU.add, ALU.mult), reads=[B_mod, Bgm], writes=[B_cols])
                    P.op("dve", CP(cols[:, dst + 8:dst + 16], modT[:, sh0:sh0 + 8]), reads=[B_mod], writes=[B_cols])
                    P.op("dve", CP(cols[:, dst + 16:dst + 24], modT[:, ga0:ga0 + 8]), reads=[B_mod], writes=[B_cols])
                if l % 2 == 0:
                    j = l // 2
                    lp = sb("ad_lp", [64, 4], F32, ph)
                    pr = sb("ad_pr", [64, 2], F32, ph)
                    ee = sb("ad_ee", [128, 2], F32, ph)
                    pl = ps("ad_pl", ph, (128, 2))
                    Blp, Bpr, Bee, Bpl = Buf(), Buf(), Buf(), Buf()
                    P.dma("sp", DMA(lp[:], lpT_d[j]), writes=[Blp])
                    P.dma("sp", DMA(cols[:, SUBG:SUBG + 1], subg_d[j]), writes=[B_cols])
                    P.op("dve", TT_(pr[:, 0:1], lp[:, 0:1], lp[:, 1:2], ALU.mult), reads=[Blp], writes=[Bpr])
                    P.op("dve", TT_(pr[:, 1:2], lp[:, 2:3], lp[:, 3:4], ALU.mult), reads=[Blp], writes=[Bpr])
                    P.op("pe", MM(pl[:], ones_f[0:64, :], pr[:], True, True), reads=[Bpr, B_const], writes=[Bpl])
                    P.op("act", ACT(ee[:], pl[:], AF.Exp), reads=[Bpl], writes=[Bee])
                    P.op("dve", TT_(cols[:, NLAM:NLAM + 1], ee[:, 1:2], ee[:, 0:1], ALU.subtract),
                         reads=[Bee], writes=[B_cols])
                    P.op("dve", TS(cols[:, NLAM:NLAM + 1], cols[:, NLAM:NLAM + 1], -lambda_init(l), None, ALU.add),
                         reads=[B_cols], writes=[B_cols])
                P.flush()

        def emit_rstd(ph, tt, rstd_ap, sqs, sqB, pss, pssB, rstdB):
            for c in range(KC):
                k = c % len(sqs)
                P.op("act", ACT(sqs[k][:], X[:, c, tt * TT:(tt + 1) * TT], AF.Square),
                     reads=[XB[c][tt]], writes=[sqB[k]])
                P.op("pe", MM(pss[:], ones_b, sqs[k][:], c == 0, c == KC - 1),
                     reads=[sqB[k], B_const], writes=[pssB])
            P.op("act", ACT(rstd_ap, pss[:], AF.Ln, bias=EPS, scale=1.0 / D), reads=[pssB], writes=[rstdB])
            P.op("act", ACT(rstd_ap, rstd_ap, AF.Exp, scale=-0.5), reads=[rstdB], writes=[rstdB])

        def emit_hT(tt, hT, hTB, rstd_ap, rstdB, tmpf, tmpfB, gs0, sh0):
            for c in range(KC):
                k = c % len(tmpf)
                P.op("dve", STT(tmpf[k][:], X[:, c, tt * TT:(tt + 1) * TT], cols[:, gs0 + c:gs0 + c + 1], rstd_ap,
                                ALU.mult, ALU.mult), reads=[XB[c][tt], B_cols, rstdB], writes=[tmpfB[k]])
                P.op("act", ACT(hT[:, c, :], tmpf[k][:], AF.Identity, bias=cols[:, sh0 + c:sh0 + c + 1]),
                     reads=[tmpfB[k], B_cols], writes=[hTB])

        def phase_mixin(l):
            even = (l % 2 == 0)
            j = l // 2
            with contextlib.ExitStack() as ph:
                rstd = [sb("mi_rstd%d" % i, [128, TT], F32, ph) for i in range(2)]
                rstdB = [Buf(), Buf()]
                hT = [sb("mi_hT%d" % i, [128, KC, TT], BF16, ph) for i in range(2)]
                hTB = [Buf(), Buf()]
                sqs = [sb("mi_sq%d" % i, [128, TT], BF16, ph) for i in range(2)]
                sqB = [Buf(), Buf()]
                tmpf = [sb("mi_tmpf%d" % i, [128, TT], F32, ph) for i in range(2)]
                tmpfB = [Buf(), Buf()]
                qsq = [sb("mi_qsq%d" % i, [128, TT], BF16, ph) for i in range(2)]
                qsqB = [Buf(), Buf()]
                qrs = [sb("mi_qrs%d" % i, [128, TT], F32, ph) for i in range(2)]
                qrsB = [Buf(), Buf()]
                wa = [sb("mi_wa%d" % i, [128, KC, 128], BF16, ph) for i in range(4)]
                waB = [Buf() for _ in range(4)]
                wbt = [sb("mi_wb%d" % i, [128, KC, 512], BF16, ph) for i in range(2)]
                wbtB = [Buf(), Buf()]
                wgt = sb("mi_wgt", [128, KC, 16], BF16, ph)
                wgtB = Buf()
                stg = [sb("mi_stg%d" % i, [128, TT], BF16, ph) for i in range(3)]
                stgB = [Buf() for _ in range(3)]
                vst = [sb("mi_vst%d" % i, [128, 8, 128], BF16, ph) for i in range(2)]
                vstB = [Buf(), Buf()]
                pss = ps("mi_pss", ph)
                pssB = Buf()
                pz = [ps("mi_pz%d" % i, ph) for i in range(3)]
                pzB = [Buf() for _ in range(3)]
                pn = [ps("mi_pn%d" % i, ph) for i in range(2)]
                pnB = [Buf(), Buf()]
                pg = ps("mi_pg", ph)
                pgB = Buf()
                cnt = {"z": 0, "stg": 0, "vst": 0, "q": 0, "wa": 0}
                if even:
                    raw = sb("mi_raw", [128, TT + 3], F32, ph)
                    rawB = Buf()
                    halo = sb("mi_halo", [128, 8, 3], F32, ph)
                    haloB = Buf()
                    acc = sb("mi_acc", [128, TT], F32, ph)
                    accB = Buf()
                    cw = sb("mi_cw", [128, 8, 4], F32, ph)
                    cb = sb("mi_cb", [128, 8], F32, ph)
                    BcwB = Buf()
                    P.dma("sp", DMA(cw[:], convw_d[j]), writes=[BcwB])
                    P.dma("sp", DMA(cb[:], convb_d[j]), writes=[BcwB])
                else:
                    for i in range(2):
                        P.op("pool", MS(vst[i][:], 1.0), writes=[vstB[i]])
                ng = 8 if even else 16
                P.dma("sp", DMA(wgt[:, :, 0:ng], winG[:, :, 0:ng]), writes=[wgtB])
                chunks = []
                if even:
                    for cc in range(8):
                        chunks.append(("qk", cc, ("da", cc)))
                    for cc in range(8):
                        chunks.append(("conv", 8 + cc, cc))
                    for cc in range(4):
                        chunks.append(("sig", 16 + cc, cc))
                    vaux = [0, 4]
                else:
                    for cc in range(8):
                        chunks.append(("qk", cc, ("fq", cc)))
                    for cc in range(8):
                        chunks.append(("qk", 8 + cc, ("fk", cc)))
                    vaux = [0, 8]
                NCH = len(chunks)
                total = NTT * NCH

                def load_wa(gi):
                    ch = chunks[gi % NCH][1]
                    k = gi % 4
                    P.dma("sp", DMA(wa[k][:].rearrange("p kc n -> p (kc n)"), winA[ch].rearrange("p kc n -> p (kc n)")),
                          writes=[waB[k]])

                def load_wb(gi):
                    k = gi % 2
                    P.dma("sp", DMA(wbt[k][:].rearrange("p kc n -> p (kc n)"), winB[gi % 2].rearrange("p kc n -> p (kc n)")),
                          writes=[wbtB[k]])

                def norm_tile(tt):
                    k = tt % 2
                    emit_rstd(ph, tt, rstd[k][:], sqs, sqB, pss, pssB, rstdB[k])
                    emit_hT(tt, hT[k], hTB[k], rstd[k][:], rstdB[k], tmpf, tmpfB, GSM, SHM)

                load_wa(0)
                load_wa(1)
                load_wa(2)
                norm_tile(0)
                for tt in range(NTT):
                    tsl = slice(tt * TT, (tt + 1) * TT)
                    h_, h_B = hT[tt % 2], hTB[tt % 2]
                    load_wb(2 * tt)
                    load_wb(2 * tt + 1)
                    for ci, (kind, ch, aux) in enumerate(chunks):
                        gi = tt * NCH + ci
                        if gi + 3 < total:
                            load_wa(gi + 3)
                        if ci == NCH // 2 and tt + 1 < NTT:
                            norm_tile(tt + 1)
                        wt, wtB = wa[gi % 4], waB[gi % 4]
                        z = pz[cnt["z"] % 3]
                        zB = pzB[cnt["z"] % 3]
                        cnt["z"] += 1
                        for kc in range(KC):
                            P.op("pe", MM(z[:], wt[:, kc, :], h_[:, kc, :], kc == 0, kc == KC - 1),
                                 reads=[wtB, h_B], writes=[zB])
                        sg = stg[cnt["stg"] % 3]
                        sgB = stgB[cnt["stg"] % 3]
                        cnt["stg"] += 1
                        if kind == "qk":
                            qi = cnt["q"] % 2
                            cnt["q"] += 1
                            P.op("act", ACT(qsq[qi][:], z[:], AF.Square), reads=[zB], writes=[qsqB[qi]])
                            P.op("pe", MM(pn[qi][:], bones_b, qsq[qi][:], True, True),
                                 reads=[qsqB[qi], B_const], writes=[pnB[qi]])
                            P.op("act", ACT(qrs[qi][:], pn[qi][:], AF.Ln, bias=EPS, scale=1.0 / 64), reads=[pnB[qi]], writes=[qrsB[qi]])
                            P.op("act", ACT(qrs[qi][:], qrs[qi][:], AF.Exp, scale=-0.5), reads=[qrsB[qi]], writes=[qrsB[qi]])
                            typ, cc = aux
                            if typ == "da":
                                isq = cc < 4
                                dst = qT_da if isq else kT_da
                                u0 = 2 * (cc % 4)
                            else:
                                isq = (typ == "fq")
                                dst = qT_o if isq else kT_o
                                u0 = 2 * cc
                            gcol = cols[:, QG:QG + 1] if isq else cols[:, KG:KG + 1]
                            P.op("dve", STT(sg[:], z[:], gcol, qrs[qi][:], ALU.mult, ALU.mult),
                                 reads=[zB, B_cols, qrsB[qi]], writes=[sgB])
                            P.dma("sp", DMA(dst[u0, 0:64, tsl], sg[0:64, :]), reads=[sgB])
                            P.dma("sp", DMA(dst[u0 + 1, 0:64, tsl], sg[64:128, :]), reads=[sgB])
                        elif kind == "conv":
                            cc = aux
                            if tt == 0:
                                P.op("pool", MS(raw[:, 0:3], 0.0), writes=[rawB])
                            else:
                                P.op("pool", CP(raw[:, 0:3], halo[:, cc, :]), reads=[haloB], writes=[rawB])
                            P.op("act", ACT(raw[:, 3:TT + 3], z[:], AF.Copy), reads=[zB], writes=[rawB])
                            P.op("pool", CP(halo[:, cc, :], raw[:, TT:TT + 3]), reads=[rawB], writes=[haloB])
                            P.op("dve", TS(acc[:], raw[:, 0:TT], cw[:, cc, 0:1], cb[:, cc:cc + 1], ALU.mult, ALU.add),
                                 reads=[rawB, BcwB], writes=[accB])
                            for tap in range(1, 4):
                                P.op("dve", STT(acc[:], raw[:, tap:tap + TT], cw[:, cc, tap:tap + 1], acc[:],
                                                ALU.mult, ALU.add), reads=[rawB, BcwB, accB], writes=[accB])
                            P.op("act", ACT(sg[:], acc[:], AF.Silu), reads=[accB], writes=[sgB])
                            dst = qT_ml if cc < 4 else kT_ml
                            P.dma("sp", DMA(dst[cc % 4, :, tsl], sg[:]), reads=[sgB])
                        else:
                            cc = aux
                            P.op("act", ACT(sg[:], z[:], AF.Sigmoid), reads=[zB], writes=[sgB])
                            P.dma("sp", DMA(osig[cc, :, tsl], sg[:]), reads=[sgB])
                    for gi2 in range(2):
                        wt, wtB = wbt[gi2], wbtB[gi2]
                        for sub in range(4):
                            z = pz[cnt["z"] % 3]
                            zB = pzB[cnt["z"] % 3]
                            cnt["z"] += 1
                            for kc in range(KC):
                                P.op("pe", MM(z[:], h_[:, kc, sub * 128:(sub + 1) * 128], wt[:, kc, :],
                                              kc == 0, kc == KC - 1), reads=[wtB, h_B], writes=[zB])
                            vs = vst[cnt["vst"] % 2]
                            vsB = vstB[cnt["vst"] % 2]
                            cnt["vst"] += 1
                            t0 = tt * TT + sub * 128
                            if even:
                                P.op("act", ACT(vs[:, 0:4, :], z[:].rearrange("p (h d) -> p h d", d=128), AF.Copy),
                                     reads=[zB], writes=[vsB])
                                P.dma("sp", DMA(V_e[t0:t0 + 128, vaux[gi2]:vaux[gi2] + 4, :], vs[:, 0:4, :]), reads=[vsB])
                            else:
                                P.op("act", ACT(vs[:, :, 0:64], z[:].rearrange("p (h d) -> p h d", d=64), AF.Copy),
                                     reads=[zB], writes=[vsB])
                                P.dma("sp", DMA(V_o[t0:t0 + 128, vaux[gi2]:vaux[gi2] + 8, :], vs[:]), reads=[vsB])
                    for sub in range(4):
                        ti = tt * 4 + sub
                        for kc in range(KC):
                            P.op("pe", MM(pg[:, ti * ng:(ti + 1) * ng], h_[:, kc, sub * 128:(sub + 1) * 128],
                                          wgt[:, kc, 0:ng], kc == 0, kc == KC - 1),
                                 reads=[wgtB, h_B], writes=[pgB])
                P.op("dve", CP(graw[:, 0:32 * ng], pg[:, 0:32 * ng]), reads=[pgB], writes=[grawB])
                P.flush()

        def phase_gates(l):
            even = (l % 2 == 0)
            j = l // 2
            with contextlib.ExitStack() as ph:
                ng = 8 if even else 16
                nh = 4 if even else 16
                gb = sb("mi_gb", [128, 512], F32, ph)
                gbB = Buf()
                graw2 = sb("mi_graw2", [128, 32 * ng], F32, ph)
                graw2B = Buf()
                spt = sb("mi_spt", [128, 16, 32], F32, ph)
                sptB = Buf()
                tot = sb("mi_tot", [128, 16, 32], F32, ph)
                totB = Buf()
                inc = sb("mi_inc", [128, 16, 32], F32, ph)
                incB = Buf()
                fpos = sb("mi_fpos", [128, 16, 32], F32, ph)
                fposB = Buf()
                onesc = sb("mi_onesc", [128, 32], F32, ph)
                onescB = Buf()
                pc = ps("mi_pc", ph)
                pcB = Buf()
                pt = ps("mi_pt", ph)
                ptB = Buf()
                P.dma("sp", DMA(gb[:, 0:32 * ng], (gbe_d if even else gbo_d)[j]), writes=[gbB])
                P.op("pool", MS(onesc[:], 1.0), writes=[onescB])
                P.op("dve", TT_(graw2[:, 0:32 * ng], graw[:, 0:32 * ng], gb[:, 0:32 * ng], ALU.add),
                     reads=[grawB, gbB], writes=[graw2B])
                gv = graw2[:].rearrange("p (t g) -> p g t", g=ng)
                f0 = 4 if even else 0
                P.op("act", ACT(spt[:, 0:nh, :], gv[:, f0:f0 + nh, :], AF.Exp, scale=-1.0), reads=[graw2B], writes=[sptB])
                P.op("act", ACT(spt[:, 0:nh, :], spt[:, 0:nh, :], AF.Ln, bias=1.0), reads=[sptB], writes=[sptB])
                spf = spt[:, 0:nh, :].rearrange("p h t -> p (h t)")
                P.op("pe", MM(pc[:, 0:nh * 32], U_f, spf, True, True), reads=[sptB, B_const], writes=[pcB])
                P.op("pe", MM(pt[:, 0:nh * 32], ones_f, spf, True, True), reads=[sptB, B_const], writes=[ptB])
                P.op("dve", CP(tot[:, 0:nh, :].rearrange("p h t -> p (h t)"), pt[:, 0:nh * 32]), reads=[ptB], writes=[totB])
                for h in range(nh):
                    P.op("dve", SCAN(inc[:, h, :], onesc[:], tot[:, h, :], 0.0, ALU.mult, ALU.add),
                         reads=[totB, onescB], writes=[incB])
                P.op("dve", TT_(inc[:, 0:nh, :], inc[:, 0:nh, :], tot[:, 0:nh, :], ALU.subtract), reads=[incB, totB], writes=[incB])
                P.op("dve", TT_(fpos[:, 0:nh, :].rearrange("p h t -> p (h t)"), pc[:, 0:nh * 32],
                                inc[:, 0:nh, :].rearrange("p h t -> p (h t)"), ALU.add), reads=[pcB, incB], writes=[fposB])
                if even:
                    P.op("dve", STT(kbias[:, 0:4, :], gv[:, 0:4, :], math.log(128 ** -0.5), fpos[:, 0:4, :], ALU.add, ALU.add),
                         reads=[graw2B, fposB], writes=[B_kbias])
                else:
                    P.op("dve", CP(kbias[:, :, :], fpos[:, :, :]), reads=[fposB], writes=[B_kbias])
                prow = [ps("mi_prow%d" % i, ph, (16, 2048)) for i in range(1)]
                prowB = Buf()
                frow = sb("mi_frow", [16, S], F32, ph)
                frowB = Buf()
                fposT = sb("mi_fposT", [128, 32, 16], F32, ph)
                fposTB = Buf()
                P.op("dve", CP(fposT[:, :, 0:nh].rearrange("p t h -> p h t"), fpos[:, 0:nh, :]), reads=[fposB], writes=[fposTB])
                for half in range(2):
                    for t8 in range(16):
                        ti = half * 16 + t8
                        P.op("pe", MM(prow[0][0:nh, t8 * 128:(t8 + 1) * 128], fposT[:, ti, 0:nh], ident_f, True, True),
                             reads=[fposTB, B_const], writes=[prowB])
                    P.op("act", ACT(frow[0:nh, half * 2048:(half + 1) * 2048], prow[0][0:nh, :], AF.Copy,
                                    scale=(-1.0 if even else -8.0)), reads=[prowB], writes=[frowB])
                if even:
                    fsp = sb("mi_fsp", [68, S], BF16, ph)
                    fspB = Buf()
                    r1 = sb("mi_r1", [4, S], F32, ph)
                    r1B = Buf()
                    hb = sb("mi_hb", [4, S], BF16, ph)
                    hbB = Buf()
                    P.op("pool", MS(fsp[:], 0.0), writes=[fspB])
                    P.op("act", ACT(fsp[0:4, :], frow[0:4, :], AF.Copy), reads=[frowB, fspB], writes=[fspB])
                    P.op("dve", TT_(r1[:], frow[0:4, :], fsp[0:4, :], ALU.subtract), reads=[frowB, fspB], writes=[r1B])
                    P.op("act", ACT(hb[:], r1[:], AF.Copy), reads=[r1B], writes=[hbB])
                    P.op("act", ACT(fsp[32:36, :], hb[:], AF.Copy), reads=[hbB, fspB], writes=[fspB])
                    P.op("dve", TT_(r1[:], r1[:], hb[:], ALU.subtract), reads=[r1B, hbB], writes=[r1B])
                    P.op("act", ACT(fsp[64:68, :], r1[:], AF.Copy), reads=[r1B, fspB], writes=[fspB])
                    P.dma("sp", DMA(fsplit_d[:, :], fsp[:]), reads=[fspB])
                else:
                    frb = sb("mi_frb", [16, S], BF16, ph)
                    frbB = Buf()
                    P.op("act", ACT(frb[:], frow[:], AF.Copy), reads=[frowB], writes=[frbB])
                    P.dma("sp", DMA(qT_o[:, 64, :], frb[:]), reads=[frbB])
                    kh = [sb("mi_kh%d" % i, [16, S], BF16, ph) for i in range(3)]
                    khB = [Buf() for _ in range(3)]
                    P.op("dve", TS(frow[:], frow[:], -1.0, None, ALU.mult), reads=[frowB, frbB], writes=[frowB])
                    for i in range(3):
                        P.op("act", ACT(kh[i][:], frow[:], AF.Copy), reads=[frowB], writes=[khB[i]])
                        if i < 2:
                            P.op("dve", TT_(frow[:], frow[:], kh[i][:], ALU.subtract), reads=[frowB, khB[i]], writes=[frowB])
                        P.dma("sp", DMA(kT_o[:, 65 + i, :], kh[i][:]), reads=[khB[i]])
                P.flush()

        fsplit_d = dscr("fsplit", [68, S])

        def phase_attn(l):
            even = (l % 2 == 0)
            with contextlib.ExitStack() as ph:
                kT = [sb("at_kT%d" % i, [128, S], BF16, ph) for i in range(2)]
                kTB = [Buf(), Buf()]
                Vt = [sb("at_V%d" % i, [128, 32, 128], BF16, ph) for i in range(2)]
                VB = [Buf(), Buf()]
                qT = [sb("at_qT%d" % i, [128, TT], BF16, ph) for i in range(2)]
                qTB = [Buf(), Buf()]
                NP = 4 if even else 6
                pT = [sb("at_pT%d" % i, [128, TT], BF16, ph) for i in range(NP)]
                pTB = [Buf() for _ in range(NP)]
                ysg = [sb("at_ysg%d" % i, [128, TT], BF16, ph) for i in range(2)]
                ysgB = [Buf(), Buf()]
                tA = sb("at_tA", [128, TT], F32, ph)
                tAB = Buf()
                tB_ = sb("at_tB", [128, TT], F32, ph)
                tBB = Buf()
                NSB = 4 if even else 6
                bank = [ps("at_bk%d" % i, ph) for i in range(NSB)]
                bankB = [Buf() for _ in range(NSB)]
                p_o = [ps("at_po%d" % i, ph) for i in range(2)]
                p_oB = [Buf(), Buf()]
                NWC = 2 if even else 4
                wcf = [sb("at_wcf%d" % i, [128, 512], F32, ph) for i in range(NWC)]
                wcfB = [Buf() for _ in range(NWC)]
                wcb = [sb("at_wcb%d" % i, [128, 512], BF16, ph) for i in range(NWC)]
                wcbB = [Buf() for _ in range(NWC)]
                cnt = {"s": 0, "p": 0, "y": 0, "d": 0}
                if even:
                    dT = [sb("at_dT%d" % i, [128, TT], BF16, ph) for i in range(2)]
                    dTB = [Buf() for _ in range(2)]
                    negF = [sb("at_negF%d" % i, [128, TT], F32, ph) for i in range(2)]
                    negFB = [Buf(), Buf()]
                    dtmp = sb("at_dtmp", [128, 128], F32, ph)
                    dtmpB = Buf()
                    r0 = sb("at_r0", [128, TT], F32, ph)
                    r0B = Buf()
                    sqb = sb("at_sqb", [128, TT], BF16, ph)
                    sqbB = Buf()
                    fsp = sb("at_fsp", [68, S], BF16, ph)
                    fspB = Buf()
                    og = [sb("at_og%d" % i, [128, TT], BF16, ph) for i in range(2)]
                    ogB = [Buf(), Buf()]
                    p_z = [ps("at_pz%d" % i, ph) for i in range(2)]
                    p_zB = [Buf(), Buf()]
                    P.dma("sp", DMA(fsp[:], fsplit_d[:, :]), writes=[fspB])
                    items = [("da", h) for h in range(4)] + [("ml", h) for h in range(4)]
                else:
                    items = [("fx", h) for h in range(16)]

                def vview(src, h):
                    return src[:, h, :].rearrange("(t p) d -> p t d", p=128)

                def item_bufs(ii):
                    kind, h = items[ii]
                    if kind == "da":
                        return (0, 1), h % 2
                    return (ii % 2,), ii % 2

                def load_item(ii):
                    kind, h = items[ii]
                    kb, vb = item_bufs(ii)
                    if kind == "fx":
                        P.dma("sp", DMA(kT[kb[0]][0:68, :], kT_o[h]), writes=[kTB[kb[0]]])
                        P.dma("sp", DMA(Vt[vb][:], vview(V_o, h)), writes=[VB[vb]])
                    elif kind == "da":
                        for m in range(2):
                            P.dma("sp", DMA(kT[m][0:66, :], kT_da[2 * h + m]), writes=[kTB[m]])
                        P.dma("sp", DMA(Vt[vb][:], vview(V_e, h)), writes=[VB[vb]])
                    else:
                        P.dma("sp", DMA(kT[kb[0]][:], kT_ml[h]), writes=[kTB[kb[0]]])
                        P.dma("sp", DMA(Vt[vb][:], vview(V_e, 4 + h)), writes=[VB[vb]])

                steps = []
                for ii, (kind, h) in enumerate(items):
                    for jq in range(NTT):
                        for m in range(2 if kind == "da" else 1):
                            steps.append((ii, kind, h, m, jq))

                def load_q(si):
                    ii, kind, h, m, jq = steps[si]
                    t0 = jq * TT
                    qq, qqB = qT[si % 2], qTB[si % 2]
                    if kind == "fx":
                        P.dma("sp", DMA(qq[0:68, :], qT_o[h][:, t0:t0 + TT]), writes=[qqB])
                    elif kind == "da":
                        P.dma("sp", DMA(qq[0:66, :], qT_da[2 * h + m][:, t0:t0 + TT]), writes=[qqB])
                    else:
                        P.dma("sp", DMA(qq[:, :], qT_ml[h][:, t0:t0 + TT]), writes=[qqB])
                        P.dma("sp", DMA(og[si % 2][:], osig[h, :, t0:t0 + TT]), writes=[ogB[si % 2]])

                def step_blocks(si):
                    jq = steps[si][4]
                    blocks = [(kt, 0, TT, False) for kt in range(4 * jq)]
                    for r in range(4):
                        blocks.append((4 * jq + r, 128 * r, 128 * r + 128, True))
                        if r < 3:
                            blocks.append((4 * jq + r, 128 * (r + 1), TT, False))
                    return blocks

                sblocks = [step_blocks(si) for si in range(len(steps))]
                touched = {}
                loaded_items = set()
                loaded_q = set()

                def ensure_loaded(si):
                    ii = steps[si][0]
                    if ii not in loaded_items:
                        load_item(ii)
                        loaded_items.add(ii)
                    if si not in loaded_q:
                        load_q(si)
                        loaded_q.add(si)

                def step_ctx(si):
                    ii, kind, h, m, jq = steps[si]
                    kb, vb = item_bufs(ii)
                    if kind == "da":
                        kk, kkB = kT[m], kTB[m]
                    else:
                        kk, kkB = kT[kb[0]], kTB[kb[0]]
                    return kind, h, m, jq, kk, kkB, Vt[vb], VB[vb], qT[si % 2], qTB[si % 2]

                def issue_scores(si, bi):
                    kind, h, m, jq, kk, kkB, vv, vvB, qq, qqB = step_ctx(si)
                    t0 = jq * TT
                    kt, c0, c1, diag = sblocks[si][bi]
                    nsb = NSB
                    si_ = cnt["s"] % nsb
                    cnt["s"] += 1
                    s_t, s_B = bank[si_], bankB[si_]
                    ks = slice(kt * 128, kt * 128 + 128)
                    if kind == "fx":
                        P.op("pe", MM(s_t[:, c0:c1], kk[0:68, ks], qq[0:68, c0:c1], True, not diag),
                             reads=[kkB, qqB], writes=[s_B])
                        if diag:
                            P.op("pe", MM(s_t[:, c0:c1], ident_b, cm8_b, False, True), reads=[B_const], writes=[s_B])
                        return (s_t, s_B, None, None)
                    if kind == "da":
                        if diag:
                            P.op("pe", MM(s_t[:, c0:c1], kk[0:64, ks], qq[0:64, c0:c1], True, False),
                                 reads=[kkB, qqB], writes=[s_B])
                            P.op("pe", MM(s_t[:, c0:c1], ident_b, dab8_b[h], False, True), reads=[B_const], writes=[s_B])
                        else:
                            P.op("pe", MM(s_t[:, c0:c1], kk[0:66, ks], qq[0:66, c0:c1], True, True),
                                 reads=[kkB, qqB], writes=[s_B])
                        return (s_t, s_B, None, None)
                    P.op("pe", MM(s_t[:, c0:c1], kk[:, ks], qq[:, c0:c1], True, True), reads=[kkB, qqB], writes=[s_B])
                    return (s_t, s_B, None, None)

                def finish_block(si, bi, sc):
                    kind, h, m, jq, kk, kkB, vv, vvB, qq, qqB = step_ctx(si)
                    kt, c0, c1, diag = sblocks[si][bi]
                    s_t, s_B, e_t, e_B = sc
                    pi = cnt["p"] % NP
                    cnt["p"] += 1
                    pp, ppB = pT[pi], pTB[pi]
                    po, poB = p_o[si % 2], p_oB[si % 2]
                    if kind == "fx":
                        P.op("act", ACT(pp[:, c0:c1], s_t[:, c0:c1], AF.Exp, scale=0.125),
                             reads=[s_B], writes=[ppB])
                    elif kind == "da":
                        if diag:
                            P.op("act", ACT(pp[:, c0:c1], s_t[:, c0:c1], AF.Exp, scale=0.125), reads=[s_B], writes=[ppB])
                        else:
                            dd = 4 * jq - kt + 3
                            P.op("act", ACT(pp[:, c0:c1], s_t[:, c0:c1], AF.Exp, bias=datab[:, h * 35 + dd:h * 35 + dd + 1],
                                            scale=0.125), reads=[s_B, B_const], writes=[ppB])
                    else:
                        di = cnt["d"] % 2
                        cnt["d"] += 1
                        nf, nfB = negF[si % 2], negFB[si % 2]
                        if diag:
                            P.op("dve", TT_(dtmp[:], nf[:, c0:c1], cm8_b, ALU.add), reads=[nfB, B_const], writes=[dtmpB])
                            P.op("act", ACT(dT[di][:, c0:c1], dtmp[:], AF.Exp, bias=kbias[:, h, kt:kt + 1]),
                                 reads=[dtmpB, B_kbias], writes=[dTB[di]])
                        else:
                            P.op("act", ACT(dT[di][:, c0:c1], nf[:, c0:c1], AF.Exp, bias=kbias[:, h, kt:kt + 1]),
                                 reads=[nfB, B_kbias], writes=[dTB[di]])
                        P.op("dve", TT_(pp[:, c0:c1], s_t[:, c0:c1], dT[di][:, c0:c1], ALU.mult),
                             reads=[s_B, dTB[di]], writes=[ppB])
                    tch = touched.setdefault(si, [False] * 4)
                    first = not tch[c0 // 128]
                    for qi in range(c0 // 128, c1 // 128):
                        tch[qi] = True
                    P.op("pe", MM(po[:, c0:c1], vv[:, kt, :], pp[:, c0:c1], first, diag),
                         reads=[vvB, ppB], writes=[poB])
                    if kind != "fx":
                        P.op("pe", MM(p_z[si % 2][:, c0:c1], ones_b, pp[:, c0:c1], first, diag),
                             reads=[B_const, ppB], writes=[p_zB[si % 2]])

                def finalize(si):
                    kind, h, m, jq, kk, kkB, vv, vvB, qq, qqB = step_ctx(si)
                    t0 = jq * TT
                    tsl = slice(t0, t0 + TT)
                    po, poB = p_o[si % 2], p_oB[si % 2]
                    if kind == "fx":
                        yi = cnt["y"] % 2
                        cnt["y"] += 1
                        P.op("dve", RCP(tA[0:64, :], po[64:128, :]), reads=[poB], writes=[tAB])
                        P.op("dve", TT_(ysg[yi][0:64, :], po[0:64, :], tA[0:64, :], ALU.mult),
                             reads=[poB, tAB], writes=[ysgB[yi]])
                        P.dma("sp", DMA(yT_s[h * 64:(h + 1) * 64, tsl], ysg[yi][0:64, :]), reads=[ysgB[yi]])
                        return
                    pz, pzB = p_z[si % 2], p_zB[si % 2]
                    if kind == "da":
                        P.op("dve", RCP(tA[:], pz[:]), reads=[pzB], writes=[tAB])
                        if m == 0:
                            P.op("dve", TT_(r0[:], po[:], tA[:], ALU.mult), reads=[poB, tAB], writes=[r0B])
                        else:
                            yi = cnt["y"] % 2
                            cnt["y"] += 1
                            P.op("dve", TT_(tB_[:], po[:], tA[:], ALU.mult), reads=[poB, tAB], writes=[tBB])
                            P.op("dve", STT(tB_[:], tB_[:], cols[:, NLAM:NLAM + 1], r0[:], ALU.mult, ALU.add),
                                 reads=[tBB, B_cols, r0B], writes=[tBB])
                            P.op("act", ACT(sqb[:], tB_[:], AF.Square), reads=[tBB], writes=[sqbB])
                            P.op("pe", MM(pz[:], ones_b, sqb[:], True, True), reads=[sqbB, B_const], writes=[pzB])
                            P.op("act", ACT(tA[:], pz[:], AF.Ln, bias=EPS, scale=1.0 / 128), reads=[pzB], writes=[tAB])
                            P.op("act", ACT(tA[:], tA[:], AF.Exp, scale=-0.5), reads=[tAB], writes=[tAB])
                            P.op("dve", STT(tB_[:], tB_[:], cols[:, SUBG:SUBG + 1], tA[:], ALU.mult, ALU.mult),
                                 reads=[tBB, B_cols, tAB], writes=[tBB])
                            P.op("act", ACT(ysg[yi][:], tB_[:], AF.Copy, scale=1.0 - lambda_init(l)),
                                 reads=[tBB], writes=[ysgB[yi]])
                            P.dma("sp", DMA(yT_s[h * 128:(h + 1) * 128, tsl], ysg[yi][:]), reads=[ysgB[yi]])
                    else:
                        yi = cnt["y"] % 2
                        cnt["y"] += 1
                        ogt, ogtB = og[si % 2], ogB[si % 2]
                        P.op("act", ACT(tA[:], pz[:], AF.Abs), reads=[pzB], writes=[tAB])
                        P.op("dve", TS(tA[:], tA[:], 1.0, None, ALU.max), reads=[tAB], writes=[tAB])
                        P.op("dve", RCP(tA[:], tA[:]), reads=[tAB], writes=[tAB])
                        P.op("dve", TT_(tB_[:], po[:], tA[:], ALU.mult), reads=[poB, tAB], writes=[tBB])
                        P.op("dve", TT_(ysg[yi][:], tB_[:], ogt[:], ALU.mult), reads=[tBB, ogtB], writes=[ysgB[yi]])
                        P.dma("sp", DMA(yT_s[512 + h * 128:512 + (h + 1) * 128, tsl], ysg[yi][:]), reads=[ysgB[yi]])

                flat = [(si, bi) for si in range(len(steps)) for bi in range(len(sblocks[si]))]
                wpipe = PiecePipe(wcast_pieces(l, wcf, wcfB, wcb, wcbB, ("pool",)), max(1, NWC - 2))
                wper = -(-len(wpipe) // len(steps))
                pend = []
                nxt = 0
                for idx, (si, bi) in enumerate(flat):
                    if bi == 0:
                        ensure_loaded(si)
                        if steps[si][1] == "ml":
                            h_ = steps[si][2]
                            t0_ = steps[si][4] * TT
                            pzz, pzzB = p_z[si % 2], p_zB[si % 2]
                            P.op("pe", MM(pzz[:], selb[:, h_ * 128:(h_ + 1) * 128], fsp[:, t0_:t0_ + TT], True, True),
                                 reads=[B_const, fspB], writes=[pzzB])
                            P.op("act", ACT(negF[si % 2][:], pzz[:], AF.Copy), reads=[pzzB], writes=[negFB[si % 2]])
                        ii = steps[si][0]
                        if ii + 1 < len(items) and (ii + 1) not in loaded_items:
                            kb0, vb0 = item_bufs(ii)
                            kb1, vb1 = item_bufs(ii + 1)
                            if not (set(kb0) & set(kb1)) and vb0 != vb1:
                                load_item(ii + 1)
                                loaded_items.add(ii + 1)
                        if si + 1 < len(steps) and steps[si + 1][0] in loaded_items:
                            ensure_loaded(si + 1)
                        wpipe.step(wper)
                    la = NSB - 1
                    while nxt < len(flat) and nxt <= idx + la:
                        nsi, nbi = flat[nxt]
                        ensure_loaded(nsi)
                        pend.append(issue_scores(nsi, nbi))
                        nxt += 1
                    finish_block(si, bi, pend.pop(0))
                    if bi == len(sblocks[si]) - 1:
                        finalize(si)
                wpipe.drain()
                P.flush()

        def phase_out(l):
            even = (l % 2 == 0)
            j = l // 2
            w_d = (ab_w_out_d if even else fx_w_out_d)[j].rearrange("(kc p) n -> p kc n", p=128)
            with contextlib.ExitStack() as ph:
                wo = sb("ou_wo", [128, KC, D], BF16, ph)
                woB = Buf()
                yt = [sb("ou_yt%d" % i, [128, KC, TT], BF16, ph) for i in range(2)]
                ytB = [Buf(), Buf()]
                pp = [ps("ou_p%d" % i, ph) for i in range(2)]
                ppB = [Buf(), Buf()]
                P.dma("sp", DMA(wo[:].rearrange("p kc n -> p (kc n)"), woutS.rearrange("p kc n -> p (kc n)")), writes=[woB])
                yv = yT_s.rearrange("(c p) t -> p c t", p=128)
                n = 0
                P.dma("sp", DMA(yt[0][:], yv[:, :, 0:TT]), writes=[ytB[0]])
                for tt in range(NTT):
                    if tt + 1 < NTT:
                        P.dma("sp", DMA(yt[(tt + 1) % 2][:], yv[:, :, (tt + 1) * TT:(tt + 2) * TT]), writes=[ytB[(tt + 1) % 2]])
                    y, yB = yt[tt % 2], ytB[tt % 2]
                    for co in range(KC):
                        p_, pB = pp[n % 2], ppB[n % 2]
                        n += 1
                        for kc in range(KC):
                            P.op("pe", MM(p_[:], wo[:, kc, co * 128:(co + 1) * 128], y[:, kc, :], kc == 0, kc == KC - 1),
                                 reads=[woB, yB], writes=[pB])
                        xs = X[:, co, tt * TT:(tt + 1) * TT]
                        P.op("dve", STT(xs, p_[:], cols[:, GM + co:GM + co + 1], xs, ALU.mult, ALU.add),
                             reads=[pB, B_cols, XB[co][tt]], writes=[XB[co][tt]])
                P.flush()

        def wcast_pieces(l, stf, stfB, stb, stbB, engs):
            w1v = w1_d[l].rearrange("(kc p) n -> p kc n", p=128)
            w3v = w3_d[l].rearrange("(kc p) n -> p kc n", p=128)
            w2v = w2_d[l].rearrange("(f p) n -> p f n", p=128)
            NS = len(stf)
            pieces = []
            ctr = [0]

            def mk13(wi, wv_, kc, c0, ncol):
                slot = [0]

                def fa():
                    k = ctr[0] % NS
                    eng = engs[ctr[0] % len(engs)]
                    ctr[0] += 1
                    slot[0] = k
                    P.dma("sp", DMA(stf[k][:, 0:ncol], wv_[:, kc, c0:c0 + ncol]), writes=[stfB[k]])
                    if eng == "act":
                        P.op("act", ACT(stb[k][:, 0:ncol], stf[k][:, 0:ncol], AF.Copy), reads=[stfB[k]], writes=[stbB[k]])
                    else:
                        P.op(eng, CP(stb[k][:, 0:ncol], stf[k][:, 0:ncol]), reads=[stfB[k]], writes=[stbB[k]])

                def fb():
                    k = slot[0]
                    nfc = ncol // 128
                    f0 = c0 // 128
                    P.dma("sp", DMA(w13s[f0:f0 + nfc, :, kc, wi, :].rearrange("f p n -> p f n"),
                                    stb[k][:, 0:ncol].rearrange("p (f n) -> p f n", n=128)), reads=[stbB[k]])
                return (fa, fb)

            def mk2(f_, c0):
                slot = [0]

                def fa():
                    k = ctr[0] % NS
                    eng = engs[ctr[0] % len(engs)]
                    ctr[0] += 1
                    slot[0] = k
                    P.dma("sp", DMA(stf[k][:, 0:512], w2v[:, f_, c0:c0 + 512]), writes=[stfB[k]])
                    if eng == "act":
                        P.op("act", ACT(stb[k][:, 0:512], stf[k][:, 0:512], AF.Copy), reads=[stfB[k]], writes=[stbB[k]])
                    else:
                        P.op(eng, CP(stb[k][:, 0:512], stf[k][:, 0:512]), reads=[stfB[k]], writes=[stbB[k]])

                def fb():
                    k = slot[0]
                    co0 = c0 // 128
                    P.dma("sp", DMA(w2s[co0:co0 + 4, :, f_, :].rearrange("c p n -> p c n"),
                                    stb[k][:, 0:512].rearrange("p (c n) -> p c n", n=128)), reads=[stbB[k]])
                return (fa, fb)

            for wi, wv_ in enumerate((w1v, w3v)):
                for kc in range(KC):
                    for c0 in range(0, FFN, 512):
                        pieces.append(mk13(wi, wv_, kc, c0, min(512, FFN - c0)))
            for f_ in range(NF):
                for c0 in (0, 512):
                    pieces.append(mk2(f_, c0))
            return pieces

        def phase_ffn(l, next_l=None):
            w1v = w1_d[l].rearrange("(kc p) n -> p kc n", p=128)
            w3v = w3_d[l].rearrange("(kc p) n -> p kc n", p=128)
            w2v = w2_d[l].rearrange("(f p) n -> p f n", p=128)
            with contextlib.ExitStack() as ph:
                hT = sb("ff_hT", [128, KC, TT], BF16, ph)
                hTB = Buf()
                g = sb("ff_g", [128, NF, TT], BF16, ph)
                gB = [Buf() for _ in range(NF)]
                sqs = [sb("ff_sq%d" % i, [128, TT], BF16, ph) for i in range(2)]
                sqB = [Buf(), Buf()]
                rstd = sb("ff_rstd", [128, TT], F32, ph)
                rstdB = Buf()
                tmpf = [sb("ff_tmpf0", [128, TT], F32, ph)]
                tmpfB = [Buf()]
                sT = [sb("ff_sT0", [128, TT], BF16, ph)]
                sTB = [Buf()]
                w13 = [sb("ff_w13_%d" % i, [128, KC, 256], BF16, ph) for i in range(3)]
                w13B = [Buf() for _ in range(3)]
                w2t = [sb("ff_w2_%d" % i, [128, NF, 128], BF16, ph) for i in range(2)]
                w2B = [Buf() for _ in range(2)]
                pss = ps("ff_pss", ph)
                pssB = Buf()
                pu1 = [ps("ff_pu1_%d" % i, ph) for i in range(2)]
                pu1B = [Buf(), Buf()]
                pu3 = [ps("ff_pu3_%d" % i, ph) for i in range(2)]
                pu3B = [Buf(), Buf()]
                py = [ps("ff_py%d" % i, ph) for i in range(2)]
                pyB = [Buf(), Buf()]
                st = [0]
                nw2 = 0
                ny = 0
                pcs = None
                if next_l is not None:
                    pcf = [sb("ff_pcf%d" % i, [128, 512], F32, ph) for i in range(3)]
                    pcfB = [Buf() for _ in range(3)]
                    pcb = [sb("ff_pcb%d" % i, [128, 512], BF16, ph) for i in range(3)]
                    pcbB = [Buf() for _ in range(3)]
                    pcs = PiecePipe(precast_pieces(next_l, pcf, pcfB, pcb, pcbB, ("pool",)), 1)
                pc_per = -(-len(pcs) // (NTT * (NF + KC))) if pcs is not None else 0

                def do_pc():
                    if pcs is not None:
                        pcs.step(pc_per)

                def n_sq(tn, c):
                    k = c % 2
                    P.op("act", ACT(sqs[k][:], X[:, c, tn * TT:(tn + 1) * TT], AF.Square), reads=[XB[c][tn]], writes=[sqB[k]])

                def n_mm(tn, c):
                    k = c % 2
                    P.op("pe", MM(pss[:], ones_b, sqs[k][:], c == 0, c == KC - 1), reads=[sqB[k], B_const], writes=[pssB])

                def n_rstd():
                    P.op("act", ACT(rstd[:], pss[:], AF.Ln, bias=EPS, scale=1.0 / D), reads=[pssB], writes=[rstdB])
                    P.op("act", ACT(rstd[:], rstd[:], AF.Exp, scale=-0.5), reads=[rstdB], writes=[rstdB])

                def n_h(tn, c):
                    P.op("dve", STT(tmpf[0][:], X[:, c, tn * TT:(tn + 1) * TT], cols[:, GSF + c:GSF + c + 1], rstd[:],
                                    ALU.mult, ALU.mult), reads=[XB[c][tn], B_cols, rstdB], writes=[tmpfB[0]])
                    P.op("act", ACT(hT[:, c, :], tmpf[0][:], AF.Identity, bias=cols[:, SHF + c:SHF + c + 1]),
                         reads=[tmpfB[0], B_cols], writes=[hTB])

                for tt in range(NTT):
                    tsl = slice(tt * TT, (tt + 1) * TT)
                    if tt == 0:
                        emit_rstd(ph, tt, rstd[:], sqs, sqB, pss, pssB, rstdB)
                        emit_hT(tt, hT, hTB, rstd[:], rstdB, tmpf, tmpfB, GSF, SHF)

                    def load13(f):
                        k = f % 3
                        P.dma("sp", DMA(w13[k][:].rearrange("p kc n -> p (kc n)"),
                                        w13s[f].rearrange("p kc w n -> p (kc w n)")), writes=[w13B[k]])

                    load13(0)
                    load13(1)
                    for f in range(NF):
                        if f + 2 < NF:
                            load13(f + 2)
                        do_pc()
                        k = f % 2
                        kw = f % 3
                        for kc in range(KC):
                            P.op("pe", MM(pu1[k][:], w13[kw][:, kc, 0:128], hT[:, kc, :], kc == 0, kc == KC - 1),
                                 reads=[w13B[kw], hTB], writes=[pu1B[k]])
                        for kc in range(KC):
                            P.op("pe", MM(pu3[k][:], w13[kw][:, kc, 128:256], hT[:, kc, :], kc == 0, kc == KC - 1),
                                 reads=[w13B[kw], hTB], writes=[pu3B[k]])
                        P.op("act", ACT(sT[0][:], pu1[k][:], AF.Silu), reads=[pu1B[k]], writes=[sTB[0]])
                        P.op("dve", TT_(g[:, f, :], pu3[k][:], sT[0][:], ALU.mult), reads=[pu3B[k], sTB[0]], writes=[gB[f]])

                    def load2(co):
                        nonlocal nw2
                        k = nw2 % 2
                        nw2 += 1
                        P.dma("sp", DMA(w2t[k][:].rearrange("p f n -> p (f n)"),
                                        w2s[co].rearrange("p f n -> p (f n)")), writes=[w2B[k]])
                        return k

                    nxt = load2(0)
                    for co in range(KC):
                        cur = nxt
                        if co + 1 < KC:
                            nxt = load2(co + 1)
                        do_pc()
                        pre = tt + 1 < NTT
                        if pre and co < 4:
                            n_sq(tt + 1, 2 * co)
                            n_sq(tt + 1, 2 * co + 1)
                        if pre and co == 4:
                            n_rstd()
                        if pre and co >= 4:
                            n_h(tt + 1, 2 * (co - 4))
                            n_h(tt + 1, 2 * (co - 4) + 1)
                        p_, pB = py[ny % 2], pyB[ny % 2]
                        ny += 1
                        for f in range(NF):
                            wk = cur
                            P.op("pe", MM(p_[:], w2t[wk][:, f, :], g[:, f, :], f == 0, f == NF - 1),
                                 reads=[w2B[wk], gB[f]], writes=[pB])
                        if pre and co < 4:
                            n_mm(tt + 1, 2 * co)
                            n_mm(tt + 1, 2 * co + 1)
                        xs = X[:, co, tsl]
                        P.op("dve", STT(xs, p_[:], cols[:, GF + co:GF + co + 1], xs, ALU.mult, ALU.add),
                             reads=[pB, B_cols, XB[co][tt]], writes=[XB[co][tt]])
                if pcs is not None:
                    pcs.drain()
                P.flush()

        phase_precast(layers[0])
        for li_, l in enumerate(layers):
            next_l = layers[li_ + 1] if li_ + 1 < len(layers) else None
            phase_ada(l)
            with contextlib.ExitStack() as phg:
                graw = sb("graw", [128, 512], F32, phg)
                phase_mixin(l)
                phase_gates(l)
            phase_attn(l)
            phase_out(l)
            phase_ffn(l, next_l)

        for c in range(KC):
            P.dma("sp", DMA(outT_d[c * 128:(c + 1) * 128, :], X[:, c, :]), reads=[XB[c][t] for t in range(NTT)])
        P.flush(final=True)
        print("ops", P.total_ops, "waits", P.total_waits, flush=True)
    return nc


def _consts():
    ident = np.eye(128, dtype=np.float32)
    ones = np.ones((128, 128), np.float32)
    bones = np.zeros((128, 128), np.float32)
    bones[:64, :64] = 1.0
    bones[64:, 64:] = 1.0
    s = np.arange(128)[:, None]
    t = np.arange(128)[None, :]
    cm8 = np.where(s <= t, 0.0, NEG * 8).astype(np.float32)
    slopes = 2.0 ** (-8.0 * np.arange(1, 5) / 4)
    dabs = []
    for h in range(4):
        allowed = (s // 64) <= (t // 64)
        dabs.append(np.where(allowed, -slopes[h] * np.abs(t - s) * 8.0, NEG * 8).astype(np.float32))
    U = (s <= t).astype(np.float32)
    cstf = np.concatenate([ident, ones, U], axis=1).astype(np.float32)
    cstb = np.concatenate([ident, ones, bones, cm8] + dabs, axis=1).astype(np.float32)
    datab = np.zeros((128, 4 * 35), np.float32)
    p = np.arange(128)
    for h in range(4):
        for dd in range(35):
            d = dd - 3
            datab[:, h * 35 + dd] = -slopes[h] * 128.0 * d + slopes[h] * p
    sel = np.zeros((68, 4 * 128), np.float32)
    for h in range(4):
        for r in (0, 32, 64):
            sel[r + h, h * 128:(h + 1) * 128] = 1.0
    import ml_dtypes
    tm = (np.arange(S) % TT).astype(np.float32)
    daq = np.zeros((16, S), np.float32)
    for h in range(4):
        v = (-slopes[h] * tm * 8.0).astype(np.float32)
        hi = v.astype(ml_dtypes.bfloat16).astype(np.float32)
        lo = v - hi
        for m in range(2):
            daq[2 * h + m] = hi
            daq[8 + 2 * h + m] = lo
    return cstf, cstb, datab, sel, daq


def _host_inputs(inp):
    f = lambda a: np.ascontiguousarray(np.asarray(a, dtype=np.float32))
    cstf, cstb, datab, sel, daq = _consts()
    col = lambda v: f(np.asarray(v).reshape(-1, 128).T)
    shared = {
        "ada_w": f(inp["ada_w"]),
        "ada_bT": f(np.stack([col(inp["ada_b"][l]) for l in range(DEPTH)])),
        "gmixT": f(np.stack([col(inp["norm_mix_g"][l]) for l in range(DEPTH)])),
        "gffnT": f(np.stack([col(inp["norm_ffn_g"][l]) for l in range(DEPTH)])),
        "ab_w_in": f(inp["ab_w_in"]), "ab_w_out": f(inp["ab_w_out"]),
        "fx_w_in": f(inp["fx_w_in"]), "fx_w_out": f(inp["fx_w_out"]),
        "ffn_w1": f(inp["ffn_w1"]), "ffn_w3": f(inp["ffn_w3"]), "ffn_w2": f(inp["ffn_w2"]),
        "cstf": cstf, "cstb": cstb, "datab": datab, "sel": sel, "daqaug": daq,
    }
    cw = np.asarray(inp["ml_conv_w"])
    shared["convwT"] = f(np.stack([cw[j].reshape(4, 8, 128).transpose(2, 1, 0) for j in range(2)]))
    shared["convbT"] = f(np.stack([col(inp["ml_conv_b"][j]) for j in range(2)]))
    gbe = np.stack([np.tile(np.concatenate([np.asarray(inp["ml_b_i"][j]), np.asarray(inp["ml_b_f"][j])])[None, :], (128, 32))
                    for j in range(2)])
    shared["gb_even"] = f(gbe)
    gbo = np.stack([np.tile(np.asarray(inp["fx_b_f"][j])[None, :], (128, 32)) for j in range(2)])
    shared["gb_odd"] = f(gbo)
    qkg = np.zeros((DEPTH, 128, 2), np.float32)
    for l in range(DEPTH):
        j = l // 2
        if l % 2 == 0:
            qkg[l, :, 0] = np.tile(np.asarray(inp["da_q_g"][j]), 2)
            qkg[l, :, 1] = np.tile(np.asarray(inp["da_k_g"][j]), 2)
        else:
            qkg[l, :, 0] = np.tile(np.asarray(inp["fx_q_g"][j]), 2)
            qkg[l, :, 1] = np.tile(np.asarray(inp["fx_k_g"][j]), 2)
    shared["qkg"] = qkg
    shared["subg"] = f(np.stack([np.asarray(inp["da_subln_g"][j]).reshape(128, 1) for j in range(2)]))
    shared["lpT"] = f(np.stack([np.asarray(inp["da_lambda"][j]).T for j in range(2)]))
    x = np.asarray(inp["x"], dtype=np.float32)
    c = np.asarray(inp["c"], dtype=np.float32)
    maps = []
    for b in range(x.shape[0]):
        m = dict(shared)
        m["xT"] = np.ascontiguousarray(x[b].T)
        m["cT"] = np.ascontiguousarray(c[b].reshape(KC, 128).T)
        maps.append(m)
    return maps


_NC_CACHE = {}


def kernel(**inputs):
    maps = _host_inputs(inputs)
    key = "full"
    if key not in _NC_CACHE:
        _NC_CACHE[key] = build(list(range(DEPTH)))
    nc = _NC_CACHE[key]
    res = run_bass_kernel_spmd(nc, maps, core_ids=list(range(8)))
    out = np.stack([np.ascontiguousarray(r["outT"].T) for r in res.results], axis=0)
    return out.astype(np.float32)
```

```python
import contextlib
import math
import numpy as np
import concourse.bass as bass
import concourse.mybir as mybir
from concourse.bass_utils import run_bass_kernel_spmd

F32 = mybir.dt.float32
BF16 = mybir.dt.bfloat16
ALU = mybir.AluOpType
AF = mybir.ActivationFunctionType

S = 4096
D = 1024
DEPTH = 4
TT = 512
NTT = S // TT
KC = 8
FFN = 2816
NF = FFN // 128
AB_IN = 3592
FX_IN = 3088
EPS = 1e-6
NEG = -30000.0

ENGS = ("pe", "act", "dve", "pool", "sp")
NDMA_SEM = 12


class Buf:
    __slots__ = ("name", "w", "r", "rd")

    def __init__(self, name=""):
        self.name = name
        self.w = None
        self.r = {}
        self.rd = []


class Op:
    __slots__ = ("eng", "fn", "deps", "idx", "ms", "cnt", "dsem", "dval", "isdma")

    def __init__(self, eng, fn, isdma):
        self.eng = eng
        self.fn = fn
        self.deps = []
        self.idx = -1
        self.ms = False
        self.cnt = 0
        self.dsem = None
        self.dval = 0
        self.isdma = isdma


class Prog:
    def __init__(self, nc, es):
        self.nc = nc
        self.esem = {e: es.enter_context(nc.semaphore("s_" + e)) for e in ENGS}
        self.dsem = {}
        for e in ("sp", "pool", "act"):
            for k in range(NDMA_SEM):
                self.dsem[(e, k)] = es.enter_context(nc.semaphore("d_%s%d" % (e, k)))
        self.base = {e: 0 for e in ENGS}
        self.dma_rr = {e: 0 for e in ENGS}
        self.dma_val = {}
        self.dma_last = {}
        self.streams = {e: [] for e in ENGS}
        self.touched = set()
        self.barrier = []
        self.total_ops = 0
        self.total_waits = 0

    def _add(self, eng, fn, reads, writes, isdma):
        op = Op(eng, fn, isdma)
        deps = op.deps
        for b in reads:
            if b.w is not None:
                deps.append(b.w)
        for b in writes:
            if b.w is not None:
                deps.append(b.w)
            deps.extend(b.r.values())
            deps.extend(b.rd)
        for b in reads:
            if isdma:
                b.rd.append(op)
            else:
                b.r[eng] = op
            self.touched.add(b)
        for b in writes:
            b.w = op
            b.r = {}
            b.rd = []
            self.touched.add(b)
        op.idx = len(self.streams[eng])
        self.streams[eng].append(op)
        return op

    def op(self, eng, fn, reads=(), writes=()):
        return self._add(eng, fn, reads, writes, False)

    def dma(self, eng, fn, reads=(), writes=()):
        op = self._add(eng, fn, reads, writes, True)
        k = self.dma_rr[eng]
        self.dma_rr[eng] = (k + 1) % NDMA_SEM
        key = (eng, k)
        prev = self.dma_last.get(key)
        if prev is not None:
            op.deps.append(prev)
        v = self.dma_val.get(key, 0) + 16
        self.dma_val[key] = v
        self.dma_last[key] = op
        op.dsem = key
        op.dval = v
        return op

    def flush(self, final=False):
        nc = self.nc
        waits = {e: [] for e in ENGS}
        for e in ENGS:
            maxidx = {}
            dwaited = {}
            for op in self.streams[e]:
                best = {}
                for d in op.deps:
                    if d.isdma:
                        if dwaited.get(d.dsem, 0) < d.dval:
                            cur = best.get(("d", d.dsem))
                            if cur is None or cur.dval < d.dval:
                                best[("d", d.dsem)] = d
                    else:
                        if d.eng == e and e == "pe":
                            continue
                        if maxidx.get(d.eng, -1) < d.idx:
                            cur = best.get(("e", d.eng))
                            if cur is None or cur.idx < d.idx:
                                best[("e", d.eng)] = d
                wl = []
                for key, d in best.items():
                    if key[0] == "d":
                        dwaited[d.dsem] = d.dval
                    else:
                        maxidx[d.eng] = d.idx
                        d.ms = True
                    wl.append(d)
                waits[e].append(wl)
        for e in ENGS:
            if self.streams[e]:
                last = self.streams[e][-1]
                if not last.isdma:
                    last.ms = True
        for e in ENGS:
            c = self.base[e]
            for op in self.streams[e]:
                if op.ms:
                    c += 1
                    op.cnt = c
            self.base[e] = c
        esem, dsem = self.esem, self.dsem
        barrier = self.barrier
        new_barrier = [("e", e, self.base[e]) for e in ENGS if self.base[e] > 0]
        new_barrier += [("d", key, v) for key, v in self.dma_val.items()]

        def emit(e, engobj):
            first = True
            for op, wl in zip(self.streams[e], waits[e]):
                if first:
                    first = False
                    for kind, key, v in barrier:
                        if kind == "e":
                            if key != e:
                                engobj.wait_ge(esem[key], v)
                        else:
                            engobj.wait_ge(dsem[key], v)
                for d in wl:
                    if d.isdma:
                        engobj.wait_ge(dsem[d.dsem], d.dval)
                    else:
                        engobj.wait_ge(esem[d.eng], d.cnt)
                ins = op.fn(engobj)
                if op.isdma:
                    ins.then_inc(dsem[op.dsem], 16)
                elif op.ms:
                    ins.then_inc(esem[e], 1)
            if final and e == "sp":
                for kind, key, v in new_barrier:
                    if kind == "e":
                        if key != e:
                            engobj.wait_ge(esem[key], v)
                    else:
                        engobj.wait_ge(dsem[key], v)

        with nc.Block() as block:
            @block.tensor
            def _(eng):
                emit("pe", eng)

            @block.scalar
            def _(eng):
                emit("act", eng)

            @block.vector
            def _(eng):
                emit("dve", eng)

            @block.gpsimd
            def _(eng):
                emit("pool", eng)

            @block.sync
            def _(eng):
                emit("sp", eng)

        for e in ENGS:
            self.total_ops += len(self.streams[e])
            self.total_waits += sum(len(w) for w in waits[e])
        self.barrier = new_barrier
        self.streams = {e: [] for e in ENGS}
        self.dma_last = {}
        for b in self.touched:
            b.w = None
            b.r = {}
            b.rd = []
        self.touched = set()


def MM(out, lhsT, rhs, start, stop):
    return lambda e: e.matmul(out, lhsT=lhsT, rhs=rhs, start=start, stop=stop, skip_group_check=True)


def ACT(out, in_, func, bias=None, scale=None):
    kw = {}
    if bias is not None:
        kw["bias"] = bias
    if scale is not None:
        kw["scale"] = scale
    return lambda e: e.activation(out=out, in_=in_, func=func, **kw)


def TT_(out, in0, in1, op):
    return lambda e: e.tensor_tensor(out=out, in0=in0, in1=in1, op=op)


def TS(out, in0, s1, s2, op0, op1=None):
    if op1 is None:
        return lambda e: e.tensor_scalar(out=out, in0=in0, scalar1=s1, scalar2=None, op0=op0)
    return lambda e: e.tensor_scalar(out=out, in0=in0, scalar1=s1, scalar2=s2, op0=op0, op1=op1)


def STT(out, in0, scalar, in1, op0, op1):
    return lambda e: e.scalar_tensor_tensor(out=out, in0=in0, scalar=scalar, in1=in1, op0=op0, op1=op1)


def CP(out, in_):
    return lambda e: e.tensor_copy(out=out, in_=in_)


def MS(ap, v):
    return lambda e: e.memset(ap, v)


def RCP(out, in_):
    return lambda e: e.reciprocal(out=out, in_=in_)


def DMA(out, in_):
    return lambda e: e.dma_start(out=out, in_=in_)


def SCAN(out, d0, d1, init, op0, op1):
    return lambda e: e.tensor_tensor_scan(out=out, data0=d0, data1=d1, initial=init, op0=op0, op1=op1)


def lambda_init(l):
    return 0.8 - 0.6 * math.exp(-0.3 * l)


def build(layers, x_in_name="xT", dbg=False):
    nc = bass.Bass("TRN2", target_bir_lowering=False)

    def din(name, shape):
        return nc.dram_tensor(name, list(shape), F32, kind="ExternalInput").ap()

    xT_d = din("xT", [D, S])
    cT_d = din("cT", [128, KC])
    ada_w_d = din("ada_w", [DEPTH, D, 6 * D])
    ada_bT_d = din("ada_bT", [DEPTH, 128, 48])
    gmixT_d = din("gmixT", [DEPTH, 128, KC])
    gffnT_d = din("gffnT", [DEPTH, 128, KC])
    ab_w_in_d = din("ab_w_in", [2, D, AB_IN])
    ab_w_out_d = din("ab_w_out", [2, D, D])
    fx_w_in_d = din("fx_w_in", [2, D, FX_IN])
    fx_w_out_d = din("fx_w_out", [2, D, D])
    w1_d = din("ffn_w1", [DEPTH, D, FFN])
    w3_d = din("ffn_w3", [DEPTH, D, FFN])
    w2_d = din("ffn_w2", [DEPTH, FFN, D])
    convw_d = din("convwT", [2, 128, 8, 4])
    convb_d = din("convbT", [2, 128, 8])
    gbe_d = din("gb_even", [2, 128, 256])
    gbo_d = din("gb_odd", [2, 128, 512])
    qkg_d = din("qkg", [DEPTH, 128, 2])
    subg_d = din("subg", [2, 128, 1])
    lpT_d = din("lpT", [2, 64, 4])
    cstf_d = din("cstf", [128, 3 * 128])
    cstb_d = din("cstb", [128, 8 * 128])
    datab_d = din("datab", [128, 4 * 35])
    sel_d = din("sel", [68, 4 * 128])
    daq_d = din("daqaug", [16, S])
    outT_d = nc.dram_tensor("outT", [D, S], F32, kind="ExternalOutput").ap()

    def dscr(name, shape, dt=BF16):
        kind = "ExternalOutput" if dbg else "Internal"
        return nc.dram_tensor(name, list(shape), dt, kind=kind).ap()

    qT_o = dscr("qT_o", [16, 68, S])
    kT_o = dscr("kT_o", [16, 68, S])
    V_o = dscr("V_o", [S, 16, 128])
    qT_da = dscr("qT_da", [8, 66, S])
    kT_da = dscr("kT_da", [8, 66, S])
    qT_ml = dscr("qT_ml", [4, 128, S])
    kT_ml = dscr("kT_ml", [4, 128, S])
    V_e = dscr("V_e", [S, 8, 128])
    osig = dscr("osig", [4, 128, S])
    yT_s = dscr("yT_s", [D, S])
    adaS = nc.dram_tensor("adaS", [12, 128, KC, 512], BF16, kind="Internal").ap()
    winA = nc.dram_tensor("winA", [20, 128, KC, 128], BF16, kind="Internal").ap()
    winB = nc.dram_tensor("winB", [2, 128, KC, 512], BF16, kind="Internal").ap()
    winG = nc.dram_tensor("winG", [128, KC, 16], BF16, kind="Internal").ap()
    woutS = nc.dram_tensor("woutS", [128, KC, D], BF16, kind="Internal").ap()
    w13s = nc.dram_tensor("w13s", [NF, 128, KC, 2, 128], BF16, kind="Internal").ap()
    w2s = nc.dram_tensor("w2s", [KC, 128, NF, 128], BF16, kind="Internal").ap()

    with contextlib.ExitStack() as es:
        P = Prog(nc, es)

        uid = [0]

        def sb(name, shape, dt, st=es):
            uid[0] += 1
            return st.enter_context(nc.sbuf_tensor("%s_%d" % (name, uid[0]), list(shape), dt))

        def ps(name, st, shape=(128, 512), dt=F32):
            uid[0] += 1
            return st.enter_context(nc.psum_tensor("%s_%d" % (name, uid[0]), list(shape), dt))

        X = sb("X", [128, KC, S], F32)
        XB = [[Buf("X%d_%d" % (c, t)) for t in range(NTT)] for c in range(KC)]
        cst = sb("cst_sb", [128, 3 * 128], F32)
        cstb = sb("cstb_sb", [128, 8 * 128], BF16)
        ident_b = cstb[:, 0:128]
        ones_b = cstb[:, 128:256]
        bones_b = cstb[:, 256:384]
        cm8_b = cstb[:, 384:512]
        dab8_b = [cstb[:, 512 + 128 * h: 640 + 128 * h] for h in range(4)]
        ident_f = cst[:, 0:128]
        ones_f = cst[:, 128:256]
        U_f = cst[:, 256:384]
        datab = sb("datab_sb", [128, 4 * 35], F32)
        selb = sb("selb", [68, 4 * 128], BF16)
        modT = sb("modT", [128, 48], F32)
        cols = sb("cols", [128, 64], F32)
        B_const = Buf("const")
        B_mod = Buf("mod")
        B_cols = Buf("cols")
        GSM, SHM, GM, GSF, SHF, GF = 0, 8, 16, 24, 32, 40
        QG, KG, SUBG, NLAM = 48, 49, 50, 51
        kbias = sb("kbias", [128, 16, 32], F32)
        B_kbias = Buf("kbias")
        graw = None
        grawB = Buf("graw")

        with contextlib.ExitStack() as ph:
            stage = sb("su_stage", [128, 8 * 128], F32, ph)
            stage2 = sb("su_stage2", [68, 4 * 128], F32, ph)
            Bs2 = Buf()
            rowsf = sb("su_rowsf", [16, S], F32, ph)
            rowsb = sb("su_rowsb", [16, S], BF16, ph)
            onesr = sb("su_onesr", [16, S], BF16, ph)
            Bs, Brf, Brb, Bor = Buf(), Buf(), Buf(), Buf()
            P.dma("sp", DMA(cst[:], cstf_d), writes=[B_const])
            P.dma("sp", DMA(stage[:], cstb_d), writes=[Bs])
            P.op("dve", CP(cstb[:], stage[:]), reads=[Bs], writes=[B_const])
            P.dma("sp", DMA(datab[:], datab_d), writes=[B_const])
            P.dma("sp", DMA(stage2[:], sel_d), writes=[Bs2])
            P.op("dve", CP(selb[:], stage2[:]), reads=[Bs2], writes=[B_const])
            for c in range(KC):
                P.dma("sp", DMA(X[:, c, :], xT_d[c * 128:(c + 1) * 128, :]),
                      writes=[XB[c][t] for t in range(NTT)])
            P.op("pool", MS(onesr[:], 1.0), writes=[Bor])
            P.dma("sp", DMA(kT_o[:, 64, :], onesr[:]), reads=[Bor])
            for r_ in (65, 66, 67):
                P.dma("sp", DMA(qT_o[:, r_, :], onesr[:]), reads=[Bor])
            P.dma("sp", DMA(kT_da[:, 64, :], onesr[0:8, :]), reads=[Bor])
            P.dma("sp", DMA(kT_da[:, 65, :], onesr[0:8, :]), reads=[Bor])
            P.dma("sp", DMA(rowsf[:], daq_d), writes=[Brf])
            P.op("dve", CP(rowsb[:], rowsf[:]), reads=[Brf], writes=[Brb])
            P.dma("sp", DMA(qT_da[:, 64, :], rowsb[0:8, :]), reads=[Brb])
            P.dma("sp", DMA(qT_da[:, 65, :], rowsb[8:16, :]), reads=[Brb])
            P.flush()

        def load_w_cast(ph_stage, stage_bufs, dst_ap, src_ap, dst_buf, n_free, state, engs=("pool",)):
            k = state[0] % len(stage_bufs)
            eng = engs[state[0] % len(engs)]
            state[0] += 1
            st_tile, st_buf = ph_stage[k], stage_bufs[k]
            P.dma("sp", DMA(st_tile[:, 0:n_free], src_ap), writes=[st_buf])
            if eng == "act":
                P.op("act", ACT(dst_ap, st_tile[:, 0:n_free], AF.Copy), reads=[st_buf], writes=[dst_buf])
            else:
                P.op(eng, CP(dst_ap, st_tile[:, 0:n_free]), reads=[st_buf], writes=[dst_buf])

        class PiecePipe:
            def __init__(self, pieces, ahead):
                self.p = pieces
                self.ia = 0
                self.ib = 0
                self.ahead = ahead

            def step(self, n=1):
                for _ in range(n):
                    while self.ia < len(self.p) and self.ia <= self.ib + self.ahead:
                        self.p[self.ia][0]()
                        self.ia += 1
                    if self.ib < len(self.p):
                        self.p[self.ib][1]()
                        self.ib += 1

            def drain(self):
                while self.ib < len(self.p):
                    self.step()

            def __len__(self):
                return len(self.p)

        def fm_groups(l):
            if l % 2 == 0:
                return [(0, 1024), (1536, 1024), (3072, 512)]
            return [(0, 1024), (1024, 1024)]

        def tm_groups(l):
            if l % 2 == 0:
                return [1024, 2560], (3584, 8)
            return [2048, 2560], (3072, 16)

        def precast_pieces(l, stf, stfB, stb, stbB, engs):
            even = (l % 2 == 0)
            j = l // 2
            win = (ab_w_in_d if even else fx_w_in_d)[j].rearrange("(kc p) n -> p kc n", p=128)
            wout = (ab_w_out_d if even else fx_w_out_d)[j].rearrange("(kc p) n -> p kc n", p=128)
            wada = ada_w_d[l].rearrange("(kc p) n -> p kc n", p=128)
            NS = len(stf)
            ctr = [0]
            pieces = []

            def mk(src, ncol, dst_fn, view=None):
                slot = [0]

                def fa():
                    k = ctr[0] % NS
                    eng = engs[ctr[0] % len(engs)]
                    ctr[0] += 1
                    slot[0] = k
                    P.dma("sp", DMA(stf[k][:, 0:ncol], src), writes=[stfB[k]])
                    if eng == "act":
                        P.op("act", ACT(stb[k][:, 0:ncol], stf[k][:, 0:ncol], AF.Copy), reads=[stfB[k]], writes=[stbB[k]])
                    else:
                        P.op(eng, CP(stb[k][:, 0:ncol], stf[k][:, 0:ncol]), reads=[stfB[k]], writes=[stbB[k]])

                def fb():
                    k = slot[0]
                    srcv = stb[k][:, 0:ncol]
                    if view is not None:
                        srcv = srcv.rearrange(view, n=128)
                    P.dma("sp", DMA(dst_fn(), srcv), reads=[stbB[k]])
                return (fa, fb)

            ch = 0
            for (c0, n) in fm_groups(l):
                for cc0 in range(0, n, 512):
                    for kc in range(KC):
                        def dst(ch_=ch + cc0 // 128, kc_=kc):
                            return winA[ch_:ch_ + 4, :, kc_, :].rearrange("c p n -> p c n")
                        pieces.append(mk(win[:, kc, c0 + cc0:c0 + cc0 + 512], 512, dst, "p (c n) -> p c n"))
                ch += n // 128
            tmc, (g0, ng) = tm_groups(l)
            for gi, c0 in enumerate(tmc):
                for kc in range(KC):
                    pieces.append(mk(win[:, kc, c0:c0 + 512], 512, lambda gi_=gi, kc_=kc: winB[gi_, :, kc_, :]))
            for kc in range(KC):
                pieces.append(mk(win[:, kc, g0:g0 + ng], ng, lambda kc_=kc, ng_=ng: winG[:, kc_, 0:ng_]))
            for kc in range(KC):
                for c0 in (0, 512):
                    pieces.append(mk(wout[:, kc, c0:c0 + 512], 512, lambda kc_=kc, c0_=c0: woutS[:, kc_, c0_:c0_ + 512]))
            for blk in range(12):
                for kc in range(KC):
                    pieces.append(mk(wada[:, kc, blk * 512:(blk + 1) * 512], 512, lambda b_=blk, kc_=kc: adaS[b_, :, kc_, :]))
            return pieces

        def phase_precast(l):
            with contextlib.ExitStack() as ph:
                NS = 10
                stf = [sb("pc_f%d" % i, [128, 512], F32, ph) for i in range(NS)]
                stfB = [Buf() for _ in range(NS)]
                stb = [sb("pc_b%d" % i, [128, 512], BF16, ph) for i in range(NS)]
                stbB = [Buf() for _ in range(NS)]
                pp_ = PiecePipe(precast_pieces(l, stf, stfB, stb, stbB, ("pool", "dve", "act")), NS - 2)
                pp_.drain()
                P.flush()

        def phase_ada(l):
            with contextlib.ExitStack() as ph:
                cT = sb("ad_cT", [128, KC], F32, ph)
                scb = sb("ad_scb", [128, KC], BF16, ph)
                abT = sb("ad_abT", [128, 48], F32, ph)
                gm = sb("ad_gm", [128, 2 * KC], F32, ph)
                wb = [sb("ad_wb%d" % i, [128, KC, 512], BF16, ph) for i in range(3)]
                wbB = [Buf() for _ in range(3)]
                pm = ps("ad_pm", ph, (128, 48))
                BcT, Bsc, Bab, Bgm, Bpm = Buf(), Buf(), Buf(), Buf(), Buf()
                st = [0]
                P.dma("sp", DMA(cT[:], cT_d), writes=[BcT])
                P.dma("sp", DMA(abT[:], ada_bT_d[l]), writes=[Bab])
                P.dma("sp", DMA(gm[:, 0:KC], gmixT_d[l]), writes=[Bgm])
                P.dma("sp", DMA(gm[:, KC:2 * KC], gffnT_d[l]), writes=[Bgm])
                P.dma("sp", DMA(cols[:, QG:QG + 2], qkg_d[l]), writes=[B_cols])
                P.op("act", ACT(scb[:], cT[:], AF.Silu), reads=[BcT], writes=[Bsc])
                wv = ada_w_d[l].rearrange("(kc p) n -> p kc n", p=128)
                def ld_ada(blk):
                    P.dma("sp", DMA(wb[blk % 3][:].rearrange("p kc n -> p (kc n)"),
                                    adaS[blk].rearrange("p kc n -> p (kc n)")), writes=[wbB[blk % 3]])
                ld_ada(0)
                ld_ada(1)
                for blk in range(12):
                    b = blk % 3
                    if blk + 2 < 12:
                        ld_ada(blk + 2)
                    for cc in range(4):
                        j = blk * 4 + cc
                        for kc in range(KC):
                            P.op("pe", MM(pm[:, j:j + 1], wb[b][:, kc, cc * 128:(cc + 1) * 128],
                                          scb[:, kc:kc + 1], kc == 0, kc == KC - 1),
                                 reads=[wbB[b], Bsc], writes=[Bpm])
                P.op("dve", TT_(modT[:], pm[:], abT[:], ALU.add), reads=[Bpm, Bab], writes=[B_mod])
                for (dst, g0, sc0, sh0, ga0) in ((GSM, 0, 8, 0, 16), (GSF, KC, 32, 24, 40)):
                    P.op("dve", STT(cols[:, dst:dst + 8], modT[:, sc0:sc0 + 8], 1.0, gm[:, g0:g0 + 8],
                                    ALU.add, ALU.mult), reads=[B_mod, Bgm], writes=[B_cols])
                    P.op("dve", CP(cols[:, dst + 8:dst + 16], modT[:, sh0:sh0 + 8]), reads=[B_mod], writes=[B_cols])
                    P.op("dve", CP(cols[:, dst + 16:dst + 24], modT[:, ga0:ga0 + 8]), reads=[B_mod], writes=[B_cols])
                if l % 2 == 0:
                    j = l // 2
                    lp = sb("ad_lp", [64, 4], F32, ph)
                    pr = sb("ad_pr", [64, 2], F32, ph)
                    ee = sb("ad_ee", [128, 2], F32, ph)
                    pl = ps("ad_pl", ph, (128, 2))
                    Blp, Bpr, Bee, Bpl = Buf(), Buf(), Buf(), Buf()
                    P.dma("sp", DMA(lp[:], lpT_d[j]), writes=[Blp])
                    P.dma("sp", DMA(cols[:, SUBG:SUBG + 1], subg_d[j]), writes=[B_cols])
                    P.op("dve", TT_(pr[:, 0:1], lp[:, 0:1], lp[:, 1:2], ALU.mult), reads=[Blp], writes=[Bpr])
                    P.op("dve", TT_(pr[:, 1:2], lp[:, 2:3], lp[:, 3:4], ALU.mult), reads=[Blp], writes=[Bpr])
                    P.op("pe", MM(pl[:], ones_f[0:64, :], pr[:], True, True), reads=[Bpr, B_const], writes=[Bpl])
                    P.op("act", ACT(ee[:], pl[:], AF.Exp), reads=[Bpl], writes=[Bee])
                    P.op("dve", TT_(cols[:, NLAM:NLAM + 1], ee[:, 1:2], ee[:, 0:1], ALU.subtract),
                         reads=[Bee], writes=[B_cols])
                    P.op("dve", TS(cols[:, NLAM:NLAM + 1], cols[:, NLAM:NLAM + 1], -lambda_init(l), None, ALU.add),
                         reads=[B_cols], writes=[B_cols])
                P.flush()

        def emit_rstd(ph, tt, rstd_ap, sqs, sqB, pss, pssB, rstdB):
            for c in range(KC):
                k = c % len(sqs)
                P.op("act", ACT(sqs[k][:], X[:, c, tt * TT:(tt + 1) * TT], AF.Square),
                     reads=[XB[c][tt]], writes=[sqB[k]])
                P.op("pe", MM(pss[:], ones_b, sqs[k][:], c == 0, c == KC - 1),
                     reads=[sqB[k], B_const], writes=[pssB])
            P.op("act", ACT(rstd_ap, pss[:], AF.Ln, bias=EPS, scale=1.0 / D), reads=[pssB], writes=[rstdB])
            P.op("act", ACT(rstd_ap, rstd_ap, AF.Exp, scale=-0.5), reads=[rstdB], writes=[rstdB])

        def emit_hT(tt, hT, hTB, rstd_ap, rstdB, tmpf, tmpfB, gs0, sh0):
            for c in range(KC):
                k = c % len(tmpf)
                P.op("dve", STT(tmpf[k][:], X[:, c, tt * TT:(tt + 1) * TT], cols[:, gs0 + c:gs0 + c + 1], rstd_ap,
                                ALU.mult, ALU.mult), reads=[XB[c][tt], B_cols, rstdB], writes=[tmpfB[k]])
                P.op("act", ACT(hT[:, c, :], tmpf[k][:], AF.Identity, bias=cols[:, sh0 + c:sh0 + c + 1]),
                     reads=[tmpfB[k], B_cols], writes=[hTB])

        def phase_mixin(l):
            even = (l % 2 == 0)
            j = l // 2
            with contextlib.ExitStack() as ph:
                rstd = [sb("mi_rstd%d" % i, [128, TT], F32, ph) for i in range(2)]
                rstdB = [Buf(), Buf()]
                hT = [sb("mi_hT%d" % i, [128, KC, TT], BF16, ph) for i in range(2)]
                hTB = [Buf(), Buf()]
                sqs = [sb("mi_sq%d" % i, [128, TT], BF16, ph) for i in range(2)]
                sqB = [Buf(), Buf()]
                tmpf = [sb("mi_tmpf%d" % i, [128, TT], F32, ph) for i in range(2)]
                tmpfB = [Buf(), Buf()]
                qsq = [sb("mi_qsq%d" % i, [128, TT], BF16, ph) for i in range(2)]
                qsqB = [Buf(), Buf()]
                qrs = [sb("mi_qrs%d" % i, [128, TT], F32, ph) for i in range(2)]
                qrsB = [Buf(), Buf()]
                wa = [sb("mi_wa%d" % i, [128, KC, 128], BF16, ph) for i in range(4)]
                waB = [Buf() for _ in range(4)]
                wbt = [sb("mi_wb%d" % i, [128, KC, 512], BF16, ph) for i in range(2)]
                wbtB = [Buf(), Buf()]
                wgt = sb("mi_wgt", [128, KC, 16], BF16, ph)
                wgtB = Buf()
                stg = [sb("mi_stg%d" % i, [128, TT], BF16, ph) for i in range(3)]
                stgB = [Buf() for _ in range(3)]
                vst = [sb("mi_vst%d" % i, [128, 8, 128], BF16, ph) for i in range(2)]
                vstB = [Buf(), Buf()]
                pss = ps("mi_pss", ph)
                pssB = Buf()
                pz = [ps("mi_pz%d" % i, ph) for i in range(3)]
                pzB = [Buf() for _ in range(3)]
                pn = [ps("mi_pn%d" % i, ph) for i in range(2)]
                pnB = [Buf(), Buf()]
                pg = ps("mi_pg", ph)
                pgB = Buf()
                cnt = {"z": 0, "stg": 0, "vst": 0, "q": 0, "wa": 0}
                deferred = [None]
                if even:
                    raw = sb("mi_raw", [128, TT + 3], F32, ph)
                    rawB = Buf()
                    halo = sb("mi_halo", [128, 8, 3], F32, ph)
                    haloB = Buf()
                    acc = sb("mi_acc", [128, TT], F32, ph)
                    accB = Buf()
                    cw = sb("mi_cw", [128, 8, 4], F32, ph)
                    cb = sb("mi_cb", [128, 8], F32, ph)
                    BcwB = Buf()
                    P.dma("sp", DMA(cw[:], convw_d[j]), writes=[BcwB])
                    P.dma("sp", DMA(cb[:], convb_d[j]), writes=[BcwB])
                else:
                    for i in range(2):
                        P.op("pool", MS(vst[i][:], 1.0), writes=[vstB[i]])
                ng = 8 if even else 16
                P.dma("sp", DMA(wgt[:, :, 0:ng], winG[:, :, 0:ng]), writes=[wgtB])
                chunks = []
                if even:
                    for cc in range(8):
                        chunks.append(("qk", cc, ("da", cc)))
                    for cc in range(8):
                        chunks.append(("conv", 8 + cc, cc))
                    for cc in range(4):
                        chunks.append(("sig", 16 + cc, cc))
                    vaux = [0, 4]
                else:
                    for cc in range(8):
                        chunks.append(("qk", cc, ("fq", cc)))
                    for cc in range(8):
                        chunks.append(("qk", 8 + cc, ("fk", cc)))
                    vaux = [0, 8]
                NCH = len(chunks)
                total = NTT * NCH

                def load_wa(gi):
                    ch = chunks[gi % NCH][1]
                    k = gi % 4
                    P.dma("sp", DMA(wa[k][:].rearrange("p kc n -> p (kc n)"), winA[ch].rearrange("p kc n -> p (kc n)")),
                          writes=[waB[k]])

                def load_wb(gi):
                    k = gi % 2
                    P.dma("sp", DMA(wbt[k][:].rearrange("p kc n -> p (kc n)"), winB[gi % 2].rearrange("p kc n -> p (kc n)")),
                          writes=[wbtB[k]])

                def norm_tile(tt):
                    k = tt % 2
                    emit_rstd(ph, tt, rstd[k][:], sqs, sqB, pss, pssB, rstdB[k])
                    emit_hT(tt, hT[k], hTB[k], rstd[k][:], rstdB[k], tmpf, tmpfB, GSM, SHM)

                load_wa(0)
                load_wa(1)
                load_wa(2)
                norm_tile(0)
                for tt in range(NTT):
                    tsl = slice(tt * TT, (tt + 1) * TT)
                    h_, h_B = hT[tt % 2], hTB[tt % 2]
                    load_wb(2 * tt)
                    load_wb(2 * tt + 1)
                    for ci, (kind, ch, aux) in enumerate(chunks):
                        gi = tt * NCH + ci
                        if gi + 3 < total:
                            load_wa(gi + 3)
                        if ci == NCH // 2 and tt + 1 < NTT:
                            norm_tile(tt + 1)
                        wt, wtB = wa[gi % 4], waB[gi % 4]
                        z = pz[cnt["z"] % 3]
                        zB = pzB[cnt["z"] % 3]
                        cnt["z"] += 1
                        for kc in range(KC):
                            P.op("pe", MM(z[:], wt[:, kc, :], h_[:, kc, :], kc == 0, kc == KC - 1),
                                 reads=[wtB, h_B], writes=[zB])
                        sg = stg[cnt["stg"] % 3]
                        sgB = stgB[cnt["stg"] % 3]
                        cnt["stg"] += 1
                        if deferred[0] is not None:
                            deferred[0]()
                            deferred[0] = None
                        if kind == "qk":
                            qi = cnt["q"] % 2
                            cnt["q"] += 1
                            P.op("act", ACT(qsq[qi][:], z[:], AF.Square), reads=[zB], writes=[qsqB[qi]])

                            def post(qi=qi, z=z, zB=zB, sg=sg, sgB=sgB, aux=aux, tsl=tsl):
                                P.op("pe", MM(pn[qi][:], bones_b, qsq[qi][:], True, True),
                                     reads=[qsqB[qi], B_const], writes=[pnB[qi]])
                                P.op("act", ACT(qrs[qi][:], pn[qi][:], AF.Ln, bias=EPS, scale=1.0 / 64), reads=[pnB[qi]], writes=[qrsB[qi]])
                                P.op("act", ACT(qrs[qi][:], qrs[qi][:], AF.Exp, scale=-0.5), reads=[qrsB[qi]], writes=[qrsB[qi]])
                                typ, cc = aux
                                if typ == "da":
                                    isq = cc < 4
                                    dst = qT_da if isq else kT_da
                                    u0 = 2 * (cc % 4)
                                else:
                                    isq = (typ == "fq")
                                    dst = qT_o if isq else kT_o
                                    u0 = 2 * cc
                                gcol = cols[:, QG:QG + 1] if isq else cols[:, KG:KG + 1]
                                P.op("dve", STT(sg[:], z[:], gcol, qrs[qi][:], ALU.mult, ALU.mult),
                                     reads=[zB, B_cols, qrsB[qi]], writes=[sgB])
                                P.dma("sp", DMA(dst[u0, 0:64, tsl], sg[0:64, :]), reads=[sgB])
                                P.dma("sp", DMA(dst[u0 + 1, 0:64, tsl], sg[64:128, :]), reads=[sgB])
                            deferred[0] = post
                        elif kind == "conv":
                            cc = aux
                            if tt == 0:
                                P.op("pool", MS(raw[:, 0:3], 0.0), writes=[rawB])
                            else:
                                P.op("pool", CP(raw[:, 0:3], halo[:, cc, :]), reads=[haloB], writes=[rawB])
                            P.op("act", ACT(raw[:, 3:TT + 3], z[:], AF.Copy), reads=[zB], writes=[rawB])
                            P.op("pool", CP(halo[:, cc, :], raw[:, TT:TT + 3]), reads=[rawB], writes=[haloB])
                            P.op("dve", TS(acc[:], raw[:, 0:TT], cw[:, cc, 0:1], cb[:, cc:cc + 1], ALU.mult, ALU.add),
                                 reads=[rawB, BcwB], writes=[accB])
                            for tap in range(1, 4):
                                P.op("dve", STT(acc[:], raw[:, tap:tap + TT], cw[:, cc, tap:tap + 1], acc[:],
                                                ALU.mult, ALU.add), reads=[rawB, BcwB, accB], writes=[accB])
                            P.op("act", ACT(sg[:], acc[:], AF.Silu), reads=[accB], writes=[sgB])
                            dst = qT_ml if cc < 4 else kT_ml
                            P.dma("sp", DMA(dst[cc % 4, :, tsl], sg[:]), reads=[sgB])
                        else:
                            cc = aux
                            P.op("act", ACT(sg[:], z[:], AF.Sigmoid), reads=[zB], writes=[sgB])
                            P.dma("sp", DMA(osig[cc, :, tsl], sg[:]), reads=[sgB])
                    for gi2 in range(2):
                        wt, wtB = wbt[gi2], wbtB[gi2]
                        for sub in range(4):
                            z = pz[cnt["z"] % 3]
                            zB = pzB[cnt["z"] % 3]
                            cnt["z"] += 1
                            for kc in range(KC):
                                P.op("pe", MM(z[:], h_[:, kc, sub * 128:(sub + 1) * 128], wt[:, kc, :],
                                              kc == 0, kc == KC - 1), reads=[wtB, h_B], writes=[zB])
                            if deferred[0] is not None:
                                deferred[0]()
                                deferred[0] = None
                            vs = vst[cnt["vst"] % 2]
                            vsB = vstB[cnt["vst"] % 2]
                            cnt["vst"] += 1
                            t0 = tt * TT + sub * 128
                            if even:
                                P.op("act", ACT(vs[:, 0:4, :], z[:].rearrange("p (h d) -> p h d", d=128), AF.Copy),
                                     reads=[zB], writes=[vsB])
                                P.dma("sp", DMA(V_e[t0:t0 + 128, vaux[gi2]:vaux[gi2] + 4, :], vs[:, 0:4, :]), reads=[vsB])
                            else:
                                P.op("act", ACT(vs[:, :, 0:64], z[:].rearrange("p (h d) -> p h d", d=64), AF.Copy),
                                     reads=[zB], writes=[vsB])
                                P.dma("sp", DMA(V_o[t0:t0 + 128, vaux[gi2]:vaux[gi2] + 8, :], vs[:]), reads=[vsB])
                    for sub in range(4):
                        ti = tt * 4 + sub
                        for kc in range(KC):
                            P.op("pe", MM(pg[:, ti * ng:(ti + 1) * ng], h_[:, kc, sub * 128:(sub + 1) * 128],
                                          wgt[:, kc, 0:ng], kc == 0, kc == KC - 1),
                                 reads=[wgtB, h_B], writes=[pgB])
                P.op("dve", CP(graw[:, 0:32 * ng], pg[:, 0:32 * ng]), reads=[pgB], writes=[grawB])
                P.flush()

        def phase_gates(l):
            even = (l % 2 == 0)
            j = l // 2
            with contextlib.ExitStack() as ph:
                ng = 8 if even else 16
                nh = 4 if even else 16
                gb = sb("mi_gb", [128, 512], F32, ph)
                gbB = Buf()
                graw2 = sb("mi_graw2", [128, 32 * ng], F32, ph)
                graw2B = Buf()
                spt = sb("mi_spt", [128, 16, 32], F32, ph)
                sptB = Buf()
                tot = sb("mi_tot", [128, 16, 32], F32, ph)
                totB = Buf()
                inc = sb("mi_inc", [128, 16, 32], F32, ph)
                incB = Buf()
                fpos = sb("mi_fpos", [128, 16, 32], F32, ph)
                fposB = Buf()
                onesc = sb("mi_onesc", [128, 32], F32, ph)
                onescB = Buf()
                pc = ps("mi_pc", ph)
                pcB = Buf()
                pt = ps("mi_pt", ph)
                ptB = Buf()
                P.dma("sp", DMA(gb[:, 0:32 * ng], (gbe_d if even else gbo_d)[j]), writes=[gbB])
                P.op("pool", MS(onesc[:], 1.0), writes=[onescB])
                P.op("dve", TT_(graw2[:, 0:32 * ng], graw[:, 0:32 * ng], gb[:, 0:32 * ng], ALU.add),
                     reads=[grawB, gbB], writes=[graw2B])
                gv = graw2[:].rearrange("p (t g) -> p g t", g=ng)
                f0 = 4 if even else 0
                P.op("act", ACT(spt[:, 0:nh, :], gv[:, f0:f0 + nh, :], AF.Exp, scale=-1.0), reads=[graw2B], writes=[sptB])
                P.op("act", ACT(spt[:, 0:nh, :], spt[:, 0:nh, :], AF.Ln, bias=1.0), reads=[sptB], writes=[sptB])
                spf = spt[:, 0:nh, :].rearrange("p h t -> p (h t)")
                P.op("pe", MM(pc[:, 0:nh * 32], U_f, spf, True, True), reads=[sptB, B_const], writes=[pcB])
                P.op("pe", MM(pt[:, 0:nh * 32], ones_f, spf, True, True), reads=[sptB, B_const], writes=[ptB])
                P.op("dve", CP(tot[:, 0:nh, :].rearrange("p h t -> p (h t)"), pt[:, 0:nh * 32]), reads=[ptB], writes=[totB])
                for h in range(nh):
                    P.op("dve", SCAN(inc[:, h, :], onesc[:], tot[:, h, :], 0.0, ALU.mult, ALU.add),
                         reads=[totB, onescB], writes=[incB])
                P.op("dve", TT_(inc[:, 0:nh, :], inc[:, 0:nh, :], tot[:, 0:nh, :], ALU.subtract), reads=[incB, totB], writes=[incB])
                P.op("dve", TT_(fpos[:, 0:nh, :].rearrange("p h t -> p (h t)"), pc[:, 0:nh * 32],
                                inc[:, 0:nh, :].rearrange("p h t -> p (h t)"), ALU.add), reads=[pcB, incB], writes=[fposB])
                if even:
                    P.op("dve", STT(kbias[:, 0:4, :], gv[:, 0:4, :], math.log(128 ** -0.5), fpos[:, 0:4, :], ALU.add, ALU.add),
                         reads=[graw2B, fposB], writes=[B_kbias])
                else:
                    P.op("dve", CP(kbias[:, :, :], fpos[:, :, :]), reads=[fposB], writes=[B_kbias])
                prow = [ps("mi_prow%d" % i, ph, (16, 2048)) for i in range(1)]
                prowB = Buf()
                frow = sb("mi_frow", [16, S], F32, ph)
                frowB = Buf()
                fposT = sb("mi_fposT", [128, 32, 16], F32, ph)
                fposTB = Buf()
                P.op("dve", CP(fposT[:, :, 0:nh].rearrange("p t h -> p h t"), fpos[:, 0:nh, :]), reads=[fposB], writes=[fposTB])
                for half in range(2):
                    for t8 in range(16):
                        ti = half * 16 + t8
                        P.op("pe", MM(prow[0][0:nh, t8 * 128:(t8 + 1) * 128], fposT[:, ti, 0:nh], ident_f, True, True),
                             reads=[fposTB, B_const], writes=[prowB])
                    P.op("act", ACT(frow[0:nh, half * 2048:(half + 1) * 2048], prow[0][0:nh, :], AF.Copy,
                                    scale=(-1.0 if even else -8.0)), reads=[prowB], writes=[frowB])
                if even:
                    fsp = sb("mi_fsp", [68, S], BF16, ph)
                    fspB = Buf()
                    r1 = sb("mi_r1", [4, S], F32, ph)
                    r1B = Buf()
                    hb = sb("mi_hb", [4, S], BF16, ph)
                    hbB = Buf()
                    P.op("pool", MS(fsp[:], 0.0), writes=[fspB])
                    P.op("act", ACT(fsp[0:4, :], frow[0:4, :], AF.Copy), reads=[frowB, fspB], writes=[fspB])
                    P.op("dve", TT_(r1[:], frow[0:4, :], fsp[0:4, :], ALU.subtract), reads=[frowB, fspB], writes=[r1B])
                    P.op("act", ACT(hb[:], r1[:], AF.Copy), reads=[r1B], writes=[hbB])
                    P.op("act", ACT(fsp[32:36, :], hb[:], AF.Copy), reads=[hbB, fspB], writes=[fspB])
                    P.op("dve", TT_(r1[:], r1[:], hb[:], ALU.subtract), reads=[r1B, hbB], writes=[r1B])
                    P.op("act", ACT(fsp[64:68, :], r1[:], AF.Copy), reads=[r1B, fspB], writes=[fspB])
                    P.dma("sp", DMA(fsplit_d[:, :], fsp[:]), reads=[fspB])
                else:
                    frb = sb("mi_frb", [16, S], BF16, ph)
                    frbB = Buf()
                    P.op("act", ACT(frb[:], frow[:], AF.Copy), reads=[frowB], writes=[frbB])
                    P.dma("sp", DMA(qT_o[:, 64, :], frb[:]), reads=[frbB])
                    kh = [sb("mi_kh%d" % i, [16, S], BF16, ph) for i in range(3)]
                    khB = [Buf() for _ in range(3)]
                    P.op("dve", TS(frow[:], frow[:], -1.0, None, ALU.mult), reads=[frowB, frbB], writes=[frowB])
                    for i in range(3):
                        P.op("act", ACT(kh[i][:], frow[:], AF.Copy), reads=[frowB], writes=[khB[i]])
                        if i < 2:
                            P.op("dve", TT_(frow[:], frow[:], kh[i][:], ALU.subtract), reads=[frowB, khB[i]], writes=[frowB])
                        P.dma("sp", DMA(kT_o[:, 65 + i, :], kh[i][:]), reads=[khB[i]])
                P.flush()

        fsplit_d = dscr("fsplit", [68, S])

        def phase_attn(l):
            even = (l % 2 == 0)
            with contextlib.ExitStack() as ph:
                kT = [sb("at_kT%d" % i, [128, S], BF16, ph) for i in range(2)]
                kTB = [Buf(), Buf()]
                Vt = [sb("at_V%d" % i, [128, 32, 128], BF16, ph) for i in range(2)]
                VB = [Buf(), Buf()]
                qT = [sb("at_qT%d" % i, [128, TT], BF16, ph) for i in range(2)]
                qTB = [Buf(), Buf()]
                NP = 4 if even else 6
                pT = [sb("at_pT%d" % i, [128, TT], BF16, ph) for i in range(NP)]
                pTB = [Buf() for _ in range(NP)]
                ysg = [sb("at_ysg%d" % i, [128, TT], BF16, ph) for i in range(2)]
                ysgB = [Buf(), Buf()]
                tA = sb("at_tA", [128, TT], F32, ph)
                tAB = Buf()
                tB_ = sb("at_tB", [128, TT], F32, ph)
                tBB = Buf()
                NSB = 4 if even else 6
                bank = [ps("at_bk%d" % i, ph) for i in range(NSB)]
                bankB = [Buf() for _ in range(NSB)]
                p_o = [ps("at_po%d" % i, ph) for i in range(2)]
                p_oB = [Buf(), Buf()]
                NWC = 2 if even else 4
                wcf = [sb("at_wcf%d" % i, [128, 512], F32, ph) for i in range(NWC)]
                wcfB = [Buf() for _ in range(NWC)]
                wcb = [sb("at_wcb%d" % i, [128, 512], BF16, ph) for i in range(NWC)]
                wcbB = [Buf() for _ in range(NWC)]
                cnt = {"s": 0, "p": 0, "y": 0, "d": 0}
                if even:
                    dT = [sb("at_dT%d" % i, [128, TT], BF16, ph) for i in range(2)]
                    dTB = [Buf() for _ in range(2)]
                    negF = [sb("at_negF%d" % i, [128, TT], F32, ph) for i in range(2)]
                    negFB = [Buf(), Buf()]
                    dtmp = sb("at_dtmp", [128, 128], F32, ph)
                    dtmpB = Buf()
                    r0 = sb("at_r0", [128, TT], F32, ph)
                    r0B = Buf()
                    sqb = sb("at_sqb", [128, TT], BF16, ph)
                    sqbB = Buf()
                    fsp = sb("at_fsp", [68, S], BF16, ph)
                    fspB = Buf()
                    og = [sb("at_og%d" % i, [128, TT], BF16, ph) for i in range(2)]
                    ogB = [Buf(), Buf()]
                    p_z = [ps("at_pz%d" % i, ph) for i in range(2)]
                    p_zB = [Buf(), Buf()]
                    P.dma("sp", DMA(fsp[:], fsplit_d[:, :]), writes=[fspB])
                    items = [("da", h) for h in range(4)] + [("ml", h) for h in range(4)]
                else:
                    items = [("fx", h) for h in range(16)]

                def vview(src, h):
                    return src[:, h, :].rearrange("(t p) d -> p t d", p=128)

                def item_bufs(ii):
                    kind, h = items[ii]
                    if kind == "da":
                        return (0, 1), h % 2
                    return (ii % 2,), ii % 2

                def load_item(ii):
                    kind, h = items[ii]
                    kb, vb = item_bufs(ii)
                    if kind == "fx":
                        P.dma("sp", DMA(kT[kb[0]][0:68, :], kT_o[h]), writes=[kTB[kb[0]]])
                        P.dma("sp", DMA(Vt[vb][:], vview(V_o, h)), writes=[VB[vb]])
                    elif kind == "da":
                        for m in range(2):
                            P.dma("sp", DMA(kT[m][0:66, :], kT_da[2 * h + m]), writes=[kTB[m]])
                        P.dma("sp", DMA(Vt[vb][:], vview(V_e, h)), writes=[VB[vb]])
                    else:
                        P.dma("sp", DMA(kT[kb[0]][:], kT_ml[h]), writes=[kTB[kb[0]]])
                        P.dma("sp", DMA(Vt[vb][:], vview(V_e, 4 + h)), writes=[VB[vb]])

                steps = []
                for ii, (kind, h) in enumerate(items):
                    for jq in range(NTT):
                        for m in range(2 if kind == "da" else 1):
                            steps.append((ii, kind, h, m, jq))

                def load_q(si):
                    ii, kind, h, m, jq = steps[si]
                    t0 = jq * TT
                    qq, qqB = qT[si % 2], qTB[si % 2]
                    if kind == "fx":
                        P.dma("sp", DMA(qq[0:68, :], qT_o[h][:, t0:t0 + TT]), writes=[qqB])
                    elif kind == "da":
                        P.dma("sp", DMA(qq[0:66, :], qT_da[2 * h + m][:, t0:t0 + TT]), writes=[qqB])
                    else:
                        P.dma("sp", DMA(qq[:, :], qT_ml[h][:, t0:t0 + TT]), writes=[qqB])
                        P.dma("sp", DMA(og[si % 2][:], osig[h, :, t0:t0 + TT]), writes=[ogB[si % 2]])

                def step_blocks(si):
                    jq = steps[si][4]
                    blocks = [(kt, 0, TT, False) for kt in range(4 * jq)]
                    for r in range(4):
                        blocks.append((4 * jq + r, 128 * r, 128 * r + 128, True))
                        if r < 3:
                            blocks.append((4 * jq + r, 128 * (r + 1), TT, False))
                    return blocks

                sblocks = [step_blocks(si) for si in range(len(steps))]
                touched = {}
                loaded_items = set()
                loaded_q = set()

                def ensure_loaded(si):
                    ii = steps[si][0]
                    if ii not in loaded_items:
                        load_item(ii)
                        loaded_items.add(ii)
                    if si not in loaded_q:
                        load_q(si)
                        loaded_q.add(si)

                def step_ctx(si):
                    ii, kind, h, m, jq = steps[si]
                    kb, vb = item_bufs(ii)
                    if kind == "da":
                        kk, kkB = kT[m], kTB[m]
                    else:
                        kk, kkB = kT[kb[0]], kTB[kb[0]]
                    return kind, h, m, jq, kk, kkB, Vt[vb], VB[vb], qT[si % 2], qTB[si % 2]

                def issue_scores(si, bi):
                    kind, h, m, jq, kk, kkB, vv, vvB, qq, qqB = step_ctx(si)
                    t0 = jq * TT
                    kt, c0, c1, diag = sblocks[si][bi]
                    nsb = NSB
                    si_ = cnt["s"] % nsb
                    cnt["s"] += 1
                    s_t, s_B = bank[si_], bankB[si_]
                    ks = slice(kt * 128, kt * 128 + 128)
                    if kind == "fx":
                        P.op("pe", MM(s_t[:, c0:c1], kk[0:68, ks], qq[0:68, c0:c1], True, not diag),
                             reads=[kkB, qqB], writes=[s_B])
                        if diag:
                            P.op("pe", MM(s_t[:, c0:c1], ident_b, cm8_b, False, True), reads=[B_const], writes=[s_B])
                        return (s_t, s_B, None, None)
                    if kind == "da":
                        if diag:
                            P.op("pe", MM(s_t[:, c0:c1], kk[0:64, ks], qq[0:64, c0:c1], True, False),
                                 reads=[kkB, qqB], writes=[s_B])
                            P.op("pe", MM(s_t[:, c0:c1], ident_b, dab8_b[h], False, True), reads=[B_const], writes=[s_B])
                        else:
                            P.op("pe", MM(s_t[:, c0:c1], kk[0:66, ks], qq[0:66, c0:c1], True, True),
                                 reads=[kkB, qqB], writes=[s_B])
                        return (s_t, s_B, None, None)
                    P.op("pe", MM(s_t[:, c0:c1], kk[:, ks], qq[:, c0:c1], True, True), reads=[kkB, qqB], writes=[s_B])
                    return (s_t, s_B, None, None)

                def finish_block(si, bi, sc):
                    kind, h, m, jq, kk, kkB, vv, vvB, qq, qqB = step_ctx(si)
                    kt, c0, c1, diag = sblocks[si][bi]
                    s_t, s_B, e_t, e_B = sc
                    pi = cnt["p"] % NP
                    cnt["p"] += 1
                    pp, ppB = pT[pi], pTB[pi]
                    po, poB = p_o[si % 2], p_oB[si % 2]
                    if kind == "fx":
                        P.op("act", ACT(pp[:, c0:c1], s_t[:, c0:c1], AF.Exp, scale=0.125),
                             reads=[s_B], writes=[ppB])
                    elif kind == "da":
                        if diag:
                            P.op("act", ACT(pp[:, c0:c1], s_t[:, c0:c1], AF.Exp, scale=0.125), reads=[s_B], writes=[ppB])
                        else:
                            dd = 4 * jq - kt + 3
                            P.op("act", ACT(pp[:, c0:c1], s_t[:, c0:c1], AF.Exp, bias=datab[:, h * 35 + dd:h * 35 + dd + 1],
                                            scale=0.125), reads=[s_B, B_const], writes=[ppB])
                    else:
                        di = cnt["d"] % 2
                        cnt["d"] += 1
                        nf, nfB = negF[si % 2], negFB[si % 2]
                        if diag:
                            P.op("dve", TT_(dtmp[:], nf[:, c0:c1], cm8_b, ALU.add), reads=[nfB, B_const], writes=[dtmpB])
                            P.op("act", ACT(dT[di][:, c0:c1], dtmp[:], AF.Exp, bias=kbias[:, h, kt:kt + 1]),
                                 reads=[dtmpB, B_kbias], writes=[dTB[di]])
                        else:
                            P.op("act", ACT(dT[di][:, c0:c1], nf[:, c0:c1], AF.Exp, bias=kbias[:, h, kt:kt + 1]),
                                 reads=[nfB, B_kbias], writes=[dTB[di]])
                        P.op("dve", TT_(pp[:, c0:c1], s_t[:, c0:c1], dT[di][:, c0:c1], ALU.mult),
                             reads=[s_B, dTB[di]], writes=[ppB])
                    tch = touched.setdefault(si, [False] * 4)
                    first = not tch[c0 // 128]
                    for qi in range(c0 // 128, c1 // 128):
                        tch[qi] = True
                    P.op("pe", MM(po[:, c0:c1], vv[:, kt, :], pp[:, c0:c1], first, diag),
                         reads=[vvB, ppB], writes=[poB])
                    if kind != "fx":
                        P.op("pe", MM(p_z[si % 2][:, c0:c1], ones_b, pp[:, c0:c1], first, diag),
                             reads=[B_const, ppB], writes=[p_zB[si % 2]])

                def finalize(si):
                    kind, h, m, jq, kk, kkB, vv, vvB, qq, qqB = step_ctx(si)
                    t0 = jq * TT
                    tsl = slice(t0, t0 + TT)
                    po, poB = p_o[si % 2], p_oB[si % 2]
                    if kind == "fx":
                        yi = cnt["y"] % 2
                        cnt["y"] += 1
                        P.op("dve", RCP(tA[0:64, :], po[64:128, :]), reads=[poB], writes=[tAB])
                        P.op("dve", TT_(ysg[yi][0:64, :], po[0:64, :], tA[0:64, :], ALU.mult),
                             reads=[poB, tAB], writes=[ysgB[yi]])
                        P.dma("sp", DMA(yT_s[h * 64:(h + 1) * 64, tsl], ysg[yi][0:64, :]), reads=[ysgB[yi]])
                        return
                    pz, pzB = p_z[si % 2], p_zB[si % 2]
                    if kind == "da":
                        P.op("dve", RCP(tA[:], pz[:]), reads=[pzB], writes=[tAB])
                        if m == 0:
                            P.op("dve", TT_(r0[:], po[:], tA[:], ALU.mult), reads=[poB, tAB], writes=[r0B])
                        else:
                            yi = cnt["y"] % 2
                            cnt["y"] += 1
                            P.op("dve", TT_(tB_[:], po[:], tA[:], ALU.mult), reads=[poB, tAB], writes=[tBB])
                            P.op("dve", STT(tB_[:], tB_[:], cols[:, NLAM:NLAM + 1], r0[:], ALU.mult, ALU.add),
                                 reads=[tBB, B_cols, r0B], writes=[tBB])
                            P.op("act", ACT(sqb[:], tB_[:], AF.Square), reads=[tBB], writes=[sqbB])
                            P.op("pe", MM(pz[:], ones_b, sqb[:], True, True), reads=[sqbB, B_const], writes=[pzB])
                            P.op("act", ACT(tA[:], pz[:], AF.Ln, bias=EPS, scale=1.0 / 128), reads=[pzB], writes=[tAB])
                            P.op("act", ACT(tA[:], tA[:], AF.Exp, scale=-0.5), reads=[tAB], writes=[tAB])
                            P.op("dve", STT(tB_[:], tB_[:], cols[:, SUBG:SUBG + 1], tA[:], ALU.mult, ALU.mult),
                                 reads=[tBB, B_cols, tAB], writes=[tBB])
                            P.op("act", ACT(ysg[yi][:], tB_[:], AF.Copy, scale=1.0 - lambda_init(l)),
                                 reads=[tBB], writes=[ysgB[yi]])
                            P.dma("sp", DMA(yT_s[h * 128:(h + 1) * 128, tsl], ysg[yi][:]), reads=[ysgB[yi]])
                    else:
                        yi = cnt["y"] % 2
                        cnt["y"] += 1
                        ogt, ogtB = og[si % 2], ogB[si % 2]
                        P.op("act", ACT(tA[:], pz[:], AF.Abs), reads=[pzB], writes=[tAB])
                        P.op("dve", TS(tA[:], tA[:], 1.0, None, ALU.max), reads=[tAB], writes=[tAB])
                        P.op("dve", RCP(tA[:], tA[:]), reads=[tAB], writes=[tAB])
                        P.op("dve", TT_(tB_[:], po[:], tA[:], ALU.mult), reads=[poB, tAB], writes=[tBB])
                        P.op("dve", TT_(ysg[yi][:], tB_[:], ogt[:], ALU.mult), reads=[tBB, ogtB], writes=[ysgB[yi]])
                        P.dma("sp", DMA(yT_s[512 + h * 128:512 + (h + 1) * 128, tsl], ysg[yi][:]), reads=[ysgB[yi]])

                flat = [(si, bi) for si in range(len(steps)) for bi in range(len(sblocks[si]))]
                wpipe = PiecePipe(wcast_pieces(l, wcf, wcfB, wcb, wcbB, ("pool",)), max(1, NWC - 2))
                wper = -(-len(wpipe) // len(steps))
                pend = []
                nxt = 0
                for idx, (si, bi) in enumerate(flat):
                    if bi == 0:
                        ensure_loaded(si)
                        if steps[si][1] == "ml":
                            h_ = steps[si][2]
                            t0_ = steps[si][4] * TT
                            pzz, pzzB = p_z[si % 2], p_zB[si % 2]
                            P.op("pe", MM(pzz[:], selb[:, h_ * 128:(h_ + 1) * 128], fsp[:, t0_:t0_ + TT], True, True),
                                 reads=[B_const, fspB], writes=[pzzB])
                            P.op("act", ACT(negF[si % 2][:], pzz[:], AF.Copy), reads=[pzzB], writes=[negFB[si % 2]])
                        ii = steps[si][0]
                        if ii + 1 < len(items) and (ii + 1) not in loaded_items:
                            kb0, vb0 = item_bufs(ii)
                            kb1, vb1 = item_bufs(ii + 1)
                            if not (set(kb0) & set(kb1)) and vb0 != vb1:
                                load_item(ii + 1)
                                loaded_items.add(ii + 1)
                        if si + 1 < len(steps) and steps[si + 1][0] in loaded_items:
                            ensure_loaded(si + 1)
                        wpipe.step(wper)
                    la = NSB - 1
                    while nxt < len(flat) and nxt <= idx + la:
                        nsi, nbi = flat[nxt]
                        ensure_loaded(nsi)
                        pend.append(issue_scores(nsi, nbi))
                        nxt += 1
                    finish_block(si, bi, pend.pop(0))
                    if bi == len(sblocks[si]) - 1:
                        finalize(si)
                wpipe.drain()
                P.flush()

        def phase_out(l):
            even = (l % 2 == 0)
            j = l // 2
            w_d = (ab_w_out_d if even else fx_w_out_d)[j].rearrange("(kc p) n -> p kc n", p=128)
            with contextlib.ExitStack() as ph:
                wo = sb("ou_wo", [128, KC, D], BF16, ph)
                woB = Buf()
                yt = [sb("ou_yt%d" % i, [128, KC, TT], BF16, ph) for i in range(2)]
                ytB = [Buf(), Buf()]
                pp = [ps("ou_p%d" % i, ph) for i in range(2)]
                ppB = [Buf(), Buf()]
                P.dma("sp", DMA(wo[:].rearrange("p kc n -> p (kc n)"), woutS.rearrange("p kc n -> p (kc n)")), writes=[woB])
                yv = yT_s.rearrange("(c p) t -> p c t", p=128)
                n = 0
                P.dma("sp", DMA(yt[0][:], yv[:, :, 0:TT]), writes=[ytB[0]])
                for tt in range(NTT):
                    if tt + 1 < NTT:
                        P.dma("sp", DMA(yt[(tt + 1) % 2][:], yv[:, :, (tt + 1) * TT:(tt + 2) * TT]), writes=[ytB[(tt + 1) % 2]])
                    y, yB = yt[tt % 2], ytB[tt % 2]
                    for co in range(KC):
                        p_, pB = pp[n % 2], ppB[n % 2]
                        n += 1
                        for kc in range(KC):
                            P.op("pe", MM(p_[:], wo[:, kc, co * 128:(co + 1) * 128], y[:, kc, :], kc == 0, kc == KC - 1),
                                 reads=[woB, yB], writes=[pB])
                        xs = X[:, co, tt * TT:(tt + 1) * TT]
                        P.op("dve", STT(xs, p_[:], cols[:, GM + co:GM + co + 1], xs, ALU.mult, ALU.add),
                             reads=[pB, B_cols, XB[co][tt]], writes=[XB[co][tt]])
                P.flush()

        def wcast_pieces(l, stf, stfB, stb, stbB, engs):
            w1v = w1_d[l].rearrange("(kc p) n -> p kc n", p=128)
            w3v = w3_d[l].rearrange("(kc p) n -> p kc n", p=128)
            w2v = w2_d[l].rearrange("(f p) n -> p f n", p=128)
            NS = len(stf)
            pieces = []
            ctr = [0]

            def mk13(wi, wv_, kc, c0, ncol):
                slot = [0]

                def fa():
                    k = ctr[0] % NS
                    eng = engs[ctr[0] % len(engs)]
                    ctr[0] += 1
                    slot[0] = k
                    P.dma("sp", DMA(stf[k][:, 0:ncol], wv_[:, kc, c0:c0 + ncol]), writes=[stfB[k]])
                    if eng == "act":
                        P.op("act", ACT(stb[k][:, 0:ncol], stf[k][:, 0:ncol], AF.Copy), reads=[stfB[k]], writes=[stbB[k]])
                    else:
                        P.op(eng, CP(stb[k][:, 0:ncol], stf[k][:, 0:ncol]), reads=[stfB[k]], writes=[stbB[k]])

                def fb():
                    k = slot[0]
                    nfc = ncol // 128
                    f0 = c0 // 128
                    P.dma("sp", DMA(w13s[f0:f0 + nfc, :, kc, wi, :].rearrange("f p n -> p f n"),
                                    stb[k][:, 0:ncol].rearrange("p (f n) -> p f n", n=128)), reads=[stbB[k]])
                return (fa, fb)

            def mk2(f_, c0):
                slot = [0]

                def fa():
                    k = ctr[0] % NS
                    eng = engs[ctr[0] % len(engs)]
                    ctr[0] += 1
                    slot[0] = k
                    P.dma("sp", DMA(stf[k][:, 0:512], w2v[:, f_, c0:c0 + 512]), writes=[stfB[k]])
                    if eng == "act":
                        P.op("act", ACT(stb[k][:, 0:512], stf[k][:, 0:512], AF.Copy), reads=[stfB[k]], writes=[stbB[k]])
                    else:
                        P.op(eng, CP(stb[k][:, 0:512], stf[k][:, 0:512]), reads=[stfB[k]], writes=[stbB[k]])

                def fb():
                    k = slot[0]
                    co0 = c0 // 128
                    P.dma("sp", DMA(w2s[co0:co0 + 4, :, f_, :].rearrange("c p n -> p c n"),
                                    stb[k][:, 0:512].rearrange("p (c n) -> p c n", n=128)), reads=[stbB[k]])
                return (fa, fb)

            for wi, wv_ in enumerate((w1v, w3v)):
                for kc in range(KC):
                    for c0 in range(0, FFN, 512):
                        pieces.append(mk13(wi, wv_, kc, c0, min(512, FFN - c0)))
            for f_ in range(NF):
                for c0 in (0, 512):
                    pieces.append(mk2(f_, c0))
            return pieces

        def phase_ffn(l, next_l=None):
            w1v = w1_d[l].rearrange("(kc p) n -> p kc n", p=128)
            w3v = w3_d[l].rearrange("(kc p) n -> p kc n", p=128)
            w2v = w2_d[l].rearrange("(f p) n -> p f n", p=128)
            with contextlib.ExitStack() as ph:
                hT = sb("ff_hT", [128, KC, TT], BF16, ph)
                hTB = Buf()
                g = sb("ff_g", [128, NF, TT], BF16, ph)
                gB = [Buf() for _ in range(NF)]
                sqs = [sb("ff_sq%d" % i, [128, TT], BF16, ph) for i in range(2)]
                sqB = [Buf(), Buf()]
                rstd = sb("ff_rstd", [128, TT], F32, ph)
                rstdB = Buf()
                tmpf = [sb("ff_tmpf0", [128, TT], F32, ph)]
                tmpfB = [Buf()]
                sT = [sb("ff_sT0", [128, TT], BF16, ph)]
                sTB = [Buf()]
                w13 = [sb("ff_w13_%d" % i, [128, KC, 256], BF16, ph) for i in range(3)]
                w13B = [Buf() for _ in range(3)]
                w2t = [sb("ff_w2_%d" % i, [128, NF, 128], BF16, ph) for i in range(2)]
                w2B = [Buf() for _ in range(2)]
                pss = ps("ff_pss", ph)
                pssB = Buf()
                pu1 = [ps("ff_pu1_%d" % i, ph) for i in range(2)]
                pu1B = [Buf(), Buf()]
                pu3 = [ps("ff_pu3_%d" % i, ph) for i in range(2)]
                pu3B = [Buf(), Buf()]
                py = [ps("ff_py%d" % i, ph) for i in range(2)]
                pyB = [Buf(), Buf()]
                st = [0]
                nw2 = 0
                ny = 0
                pcs = None
                if next_l is not None:
                    pcf = [sb("ff_pcf%d" % i, [128, 512], F32, ph) for i in range(3)]
                    pcfB = [Buf() for _ in range(3)]
                    pcb = [sb("ff_pcb%d" % i, [128, 512], BF16, ph) for i in range(3)]
                    pcbB = [Buf() for _ in range(3)]
                    pcs = PiecePipe(precast_pieces(next_l, pcf, pcfB, pcb, pcbB, ("pool",)), 1)
                pc_per = -(-len(pcs) // (NTT * (NF + KC))) if pcs is not None else 0

                def do_pc():
                    if pcs is not None:
                        pcs.step(pc_per)

                def n_sq(tn, c):
                    k = c % 2
                    P.op("act", ACT(sqs[k][:], X[:, c, tn * TT:(tn + 1) * TT], AF.Square), reads=[XB[c][tn]], writes=[sqB[k]])

                def n_mm(tn, c):
                    k = c % 2
                    P.op("pe", MM(pss[:], ones_b, sqs[k][:], c == 0, c == KC - 1), reads=[sqB[k], B_const], writes=[pssB])

                def n_rstd():
                    P.op("act", ACT(rstd[:], pss[:], AF.Ln, bias=EPS, scale=1.0 / D), reads=[pssB], writes=[rstdB])
                    P.op("act", ACT(rstd[:], rstd[:], AF.Exp, scale=-0.5), reads=[rstdB], writes=[rstdB])

                def n_h(tn, c):
                    P.op("dve", STT(tmpf[0][:], X[:, c, tn * TT:(tn + 1) * TT], cols[:, GSF + c:GSF + c + 1], rstd[:],
                                    ALU.mult, ALU.mult), reads=[XB[c][tn], B_cols, rstdB], writes=[tmpfB[0]])
                    P.op("act", ACT(hT[:, c, :], tmpf[0][:], AF.Identity, bias=cols[:, SHF + c:SHF + c + 1]),
                         reads=[tmpfB[0], B_cols], writes=[hTB])

                for tt in range(NTT):
                    tsl = slice(tt * TT, (tt + 1) * TT)
                    if tt == 0:
                        emit_rstd(ph, tt, rstd[:], sqs, sqB, pss, pssB, rstdB)
                        emit_hT(tt, hT, hTB, rstd[:], rstdB, tmpf, tmpfB, GSF, SHF)

                    def load13(f):
                        k = f % 3
                        P.dma("sp", DMA(w13[k][:].rearrange("p kc n -> p (kc n)"),
                                        w13s[f].rearrange("p kc w n -> p (kc w n)")), writes=[w13B[k]])

                    load13(0)
                    load13(1)
                    for f in range(NF):
                        if f + 2 < NF:
                            load13(f + 2)
                        do_pc()
                        k = f % 2
                        kw = f % 3
                        for kc in range(KC):
                            P.op("pe", MM(pu1[k][:], w13[kw][:, kc, 0:128], hT[:, kc, :], kc == 0, kc == KC - 1),
                                 reads=[w13B[kw], hTB], writes=[pu1B[k]])
                        for kc in range(KC):
                            P.op("pe", MM(pu3[k][:], w13[kw][:, kc, 128:256], hT[:, kc, :], kc == 0, kc == KC - 1),
                                 reads=[w13B[kw], hTB], writes=[pu3B[k]])
                        P.op("act", ACT(sT[0][:], pu1[k][:], AF.Silu), reads=[pu1B[k]], writes=[sTB[0]])
                        P.op("dve", TT_(g[:, f, :], pu3[k][:], sT[0][:], ALU.mult), reads=[pu3B[k], sTB[0]], writes=[gB[f]])

                    def load2(co):
                        nonlocal nw2
                        k = nw2 % 2
                        nw2 += 1
                        P.dma("sp", DMA(w2t[k][:].rearrange("p f n -> p (f n)"),
                                        w2s[co].rearrange("p f n -> p (f n)")), writes=[w2B[k]])
                        return k

                    nxt = load2(0)
                    for co in range(KC):
                        cur = nxt
                        if co + 1 < KC:
                            nxt = load2(co + 1)
                        do_pc()
                        pre = tt + 1 < NTT
                        if pre and co < 4:
                            n_sq(tt + 1, 2 * co)
                            n_sq(tt + 1, 2 * co + 1)
                        if pre and co == 4:
                            n_rstd()
                        if pre and co >= 4:
                            n_h(tt + 1, 2 * (co - 4))
                            n_h(tt + 1, 2 * (co - 4) + 1)
                        p_, pB = py[ny % 2], pyB[ny % 2]
                        ny += 1
                        for f in range(NF):
                            wk = cur
                            P.op("pe", MM(p_[:], w2t[wk][:, f, :], g[:, f, :], f == 0, f == NF - 1),
                                 reads=[w2B[wk], gB[f]], writes=[pB])
                        if pre and co < 4:
                            n_mm(tt + 1, 2 * co)
                            n_mm(tt + 1, 2 * co + 1)
                        xs = X[:, co, tsl]
                        P.op("dve", STT(xs, p_[:], cols[:, GF + co:GF + co + 1], xs, ALU.mult, ALU.add),
                             reads=[pB, B_cols, XB[co][tt]], writes=[XB[co][tt]])
                if pcs is not None:
                    pcs.drain()
                P.flush()

        phase_precast(layers[0])
        for li_, l in enumerate(layers):
            next_l = layers[li_ + 1] if li_ + 1 < len(layers) else None
            phase_ada(l)
            with contextlib.ExitStack() as phg:
                graw = sb("graw", [128, 512], F32, phg)
                phase_mixin(l)
                phase_gates(l)
            phase_attn(l)
            phase_out(l)
            phase_ffn(l, next_l)

        for c in range(KC):
            P.dma("sp", DMA(outT_d[c * 128:(c + 1) * 128, :], X[:, c, :]), reads=[XB[c][t] for t in range(NTT)])
        P.flush(final=True)
        print("ops", P.total_ops, "waits", P.total_waits, flush=True)
    return nc


def _consts():
    ident = np.eye(128, dtype=np.float32)
    ones = np.ones((128, 128), np.float32)
    bones = np.zeros((128, 128), np.float32)
    bones[:64, :64] = 1.0
    bones[64:, 64:] = 1.0
    s = np.arange(128)[:, None]
    t = np.arange(128)[None, :]
    cm8 = np.where(s <= t, 0.0, NEG * 8).astype(np.float32)
    slopes = 2.0 ** (-8.0 * np.arange(1, 5) / 4)
    dabs = []
    for h in range(4):
        allowed = (s // 64) <= (t // 64)
        dabs.append(np.where(allowed, -slopes[h] * np.abs(t - s) * 8.0, NEG * 8).astype(np.float32))
    U = (s <= t).astype(np.float32)
    cstf = np.concatenate([ident, ones, U], axis=1).astype(np.float32)
    cstb = np.concatenate([ident, ones, bones, cm8] + dabs, axis=1).astype(np.float32)
    datab = np.zeros((128, 4 * 35), np.float32)
    p = np.arange(128)
    for h in range(4):
        for dd in range(35):
            d = dd - 3
            datab[:, h * 35 + dd] = -slopes[h] * 128.0 * d + slopes[h] * p
    sel = np.zeros((68, 4 * 128), np.float32)
    for h in range(4):
        for r in (0, 32, 64):
            sel[r + h, h * 128:(h + 1) * 128] = 1.0
    import ml_dtypes
    tm = (np.arange(S) % TT).astype(np.float32)
    daq = np.zeros((16, S), np.float32)
    for h in range(4):
        v = (-slopes[h] * tm * 8.0).astype(np.float32)
        hi = v.astype(ml_dtypes.bfloat16).astype(np.float32)
        lo = v - hi
        for m in range(2):
            daq[2 * h + m] = hi
            daq[8 + 2 * h + m] = lo
    return cstf, cstb, datab, sel, daq


def _host_inputs(inp):
    f = lambda a: np.ascontiguousarray(np.asarray(a, dtype=np.float32))
    cstf, cstb, datab, sel, daq = _consts()
    col = lambda v: f(np.asarray(v).reshape(-1, 128).T)
    shared = {
        "ada_w": f(inp["ada_w"]),
        "ada_bT": f(np.stack([col(inp["ada_b"][l]) for l in range(DEPTH)])),
        "gmixT": f(np.stack([col(inp["norm_mix_g"][l]) for l in range(DEPTH)])),
        "gffnT": f(np.stack([col(inp["norm_ffn_g"][l]) for l in range(DEPTH)])),
        "ab_w_in": f(inp["ab_w_in"]), "ab_w_out": f(inp["ab_w_out"]),
        "fx_w_in": f(inp["fx_w_in"]), "fx_w_out": f(inp["fx_w_out"]),
        "ffn_w1": f(inp["ffn_w1"]), "ffn_w3": f(inp["ffn_w3"]), "ffn_w2": f(inp["ffn_w2"]),
        "cstf": cstf, "cstb": cstb, "datab": datab, "sel": sel, "daqaug": daq,
    }
    cw = np.asarray(inp["ml_conv_w"])
    shared["convwT"] = f(np.stack([cw[j].reshape(4, 8, 128).transpose(2, 1, 0) for j in range(2)]))
    shared["convbT"] = f(np.stack([col(inp["ml_conv_b"][j]) for j in range(2)]))
    gbe = np.stack([np.tile(np.concatenate([np.asarray(inp["ml_b_i"][j]), np.asarray(inp["ml_b_f"][j])])[None, :], (128, 32))
                    for j in range(2)])
    shared["gb_even"] = f(gbe)
    gbo = np.stack([np.tile(np.asarray(inp["fx_b_f"][j])[None, :], (128, 32)) for j in range(2)])
    shared["gb_odd"] = f(gbo)
    qkg = np.zeros((DEPTH, 128, 2), np.float32)
    for l in range(DEPTH):
        j = l // 2
        if l % 2 == 0:
            qkg[l, :, 0] = np.tile(np.asarray(inp["da_q_g"][j]), 2)
            qkg[l, :, 1] = np.tile(np.asarray(inp["da_k_g"][j]), 2)
        else:
            qkg[l, :, 0] = np.tile(np.asarray(inp["fx_q_g"][j]), 2)
            qkg[l, :, 1] = np.tile(np.asarray(inp["fx_k_g"][j]), 2)
    shared["qkg"] = qkg
    shared["subg"] = f(np.stack([np.asarray(inp["da_subln_g"][j]).reshape(128, 1) for j in range(2)]))
    shared["lpT"] = f(np.stack([np.asarray(inp["da_lambda"][j]).T for j in range(2)]))
    x = np.asarray(inp["x"], dtype=np.float32)
    c = np.asarray(inp["c"], dtype=np.float32)
    maps = []
    for b in range(x.shape[0]):
        m = dict(shared)
        m["xT"] = np.ascontiguousarray(x[b].T)
        m["cT"] = np.ascontiguousarray(c[b].reshape(KC, 128).T)
        maps.append(m)
    return maps


_NC_CACHE = {}


def kernel(**inputs):
    maps = _host_inputs(inputs)
    key = "full"
    if key not in _NC_CACHE:
        _NC_CACHE[key] = build(list(range(DEPTH)))
    nc = _NC_CACHE[key]
    res = run_bass_kernel_spmd(nc, maps, core_ids=list(range(8)))
    out = np.stack([np.ascontiguousarray(r["outT"].T) for r in res.results], axis=0)
    return out.astype(np.float32)
```
